# Optimizing a Trainium2 kernel written in Bass

```python
import jax, jax.numpy as jnp
from jax import lax
import numpy as np

D_MODEL = 1024
BATCH = 8
SEQ = 4096
DEPTH = 1

N_ATTN_HEADS = 8
HEAD_DIM = 64
ATTN_WIDTH = N_ATTN_HEADS * HEAD_DIM
SGU_WIDTH = D_MODEL - ATTN_WIDTH
N_SGU_GROUPS = 4
SGU_GROUP_DIM = SGU_WIDTH // N_SGU_GROUPS
IN_WIDTH = 3 * ATTN_WIDTH + 2 * SGU_WIDTH
MOBA_BLOCK = 256
MOBA_TOPK = 3
Q_CHUNK = 32
SGU_CHUNK = 128
D_FF = -(-8 * D_MODEL // (3 * 256)) * 256
N_MOD = 6
EPS = 1e-6
NEG = -1e30

kernel_name = "hymba_moba_gmlp_swiglu_adaln"


def rms_norm(x, g):
    xf = x.astype(jnp.float32)
    y = xf * lax.rsqrt(jnp.mean(xf * xf, axis=-1, keepdims=True) + EPS)
    return (y * g.astype(jnp.float32)).astype(x.dtype)


def layer_norm(x, g):
    xf = x.astype(jnp.float32)
    mu = jnp.mean(xf, axis=-1, keepdims=True)
    d = xf - mu
    y = d * lax.rsqrt(jnp.mean(d * d, axis=-1, keepdims=True) + EPS)
    return (y * g.astype(jnp.float32)).astype(x.dtype)


def modulate(h, shift, scale):
    return h * (1 + scale[:, None, :]) + shift[:, None, :]


def gather_blocks(blocks, idx):
    return jax.vmap(jax.vmap(lambda blk, i: blk[i]))(blocks, idx)


def moba_attention(q, k, v):
    B, H, S, Dh = q.shape
    S_pad = -(-S // MOBA_BLOCK) * MOBA_BLOCK
    pad = S_pad - S
    if pad:
        cfg = ((0, 0), (0, 0), (0, pad), (0, 0))
        q, k, v = jnp.pad(q, cfg), jnp.pad(k, cfg), jnp.pad(v, cfg)
    nb = S_pad // MOBA_BLOCK
    k_sel = max(1, min(MOBA_TOPK, nb - 1))
    kb = k.reshape(B, H, nb, MOBA_BLOCK, Dh)
    vb = v.reshape(B, H, nb, MOBA_BLOCK, Dh)
    k_mean = jnp.mean(kb.astype(jnp.float32), axis=3)
    q_blk = jnp.arange(S_pad) // MOBA_BLOCK
    gate = jnp.einsum('bhtd,bhnd->bhtn', q.astype(jnp.float32), k_mean)
    fully_past = jnp.arange(nb)[None, :] < q_blk[:, None]
    gate = jnp.where(fully_past, gate, -jnp.inf)
    _, sel = lax.top_k(gate, k_sel)
    sel_valid = sel < q_blk[:, None]
    scale = Dh ** -0.5

    def chunk_fn(ci):
        t0 = ci * Q_CHUNK
        qc = lax.dynamic_slice_in_dim(q, t0, Q_CHUNK, axis=2)
        selc = lax.dynamic_slice_in_dim(sel, t0, Q_CHUNK, axis=2)
        validc = lax.dynamic_slice_in_dim(sel_valid, t0, Q_CHUNK, axis=2)
        blk = t0 // MOBA_BLOCK
        k_own = lax.dynamic_index_in_dim(kb, blk, axis=2, keepdims=False)
        v_own = lax.dynamic_index_in_dim(vb, blk, axis=2, keepdims=False)
        k_g = gather_blocks(kb, selc)
        v_g = gather_blocks(vb, selc)
        s_sel = jnp.einsum('bhtd,bhtkld->bhtkl', qc, k_g,
                           preferred_element_type=jnp.float32) * scale
        s_sel = jnp.where(validc[..., None], s_sel, NEG)
        s_own = jnp.einsum('bhtd,bhld->bhtl', qc, k_own,
                           preferred_element_type=jnp.float32) * scale
        t_pos = t0 + jnp.arange(Q_CHUNK)
        k_pos = blk * MOBA_BLOCK + jnp.arange(MOBA_BLOCK)
        s_own = jnp.where(k_pos[None, :] <= t_pos[:, None], s_own, NEG)
        logits = jnp.concatenate(
            [s_sel.reshape(B, H, Q_CHUNK, k_sel * MOBA_BLOCK), s_own], axis=-1)
        p = jax.nn.softmax(logits, axis=-1)
        p_sel = p[..., :k_sel * MOBA_BLOCK].reshape(B, H, Q_CHUNK, k_sel, MOBA_BLOCK).astype(v.dtype)
        p_own = p[..., k_sel * MOBA_BLOCK:].astype(v.dtype)
        return (jnp.einsum('bhtkl,bhtkld->bhtd', p_sel, v_g)
                + jnp.einsum('bhtl,bhld->bhtd', p_own, v_own))

    out = lax.map(chunk_fn, jnp.arange(S_pad // Q_CHUNK))
    out = out.transpose(1, 2, 0, 3, 4).reshape(B, H, S_pad, Dh)
    return out[:, :, :S]


def spatial_gating(u, vg, w_s, b_s, g_norm):
    B, S, _ = u.shape
    u = jax.nn.gelu(u)
    vg = jax.nn.gelu(vg).reshape(B, S // SGU_CHUNK, SGU_CHUNK, N_SGU_GROUPS, SGU_GROUP_DIM)
    vg = layer_norm(vg, g_norm.reshape(N_SGU_GROUPS, SGU_GROUP_DIM))
    causal = jnp.tril(jnp.ones((SGU_CHUNK, SGU_CHUNK), dtype=bool))
    w = jnp.where(causal[None], w_s, jnp.zeros((), w_s.dtype))
    mixed = jnp.einsum('gts,bnsgc->bntgc', w, vg) + b_s.T[None, None, :, :, None]
    return u * mixed.reshape(B, S, SGU_WIDTH)


def setup_inputs(seed: int = 0) -> dict:
    key = jax.random.key(seed)
    ks = jax.random.split(key, 17)
    f32 = jnp.float32

    def nrm(k, shape, scale):
        return jax.random.normal(k, shape, f32) * scale

    def gain(k, shape):
        return 1.0 + 0.02 * jax.random.normal(k, shape, f32)

    return {
        "x": nrm(ks[0], (BATCH, SEQ, D_MODEL), 1.0),
        "c": nrm(ks[1], (BATCH, D_MODEL), 1.0),
        "w_ada": nrm(ks[2], (DEPTH, D_MODEL, N_MOD * D_MODEL), D_MODEL ** -0.5),
        "b_ada": nrm(ks[3], (DEPTH, N_MOD * D_MODEL), 0.02),
        "g_pre_mix": gain(ks[4], (DEPTH, D_MODEL)),
        "g_post_mix": gain(ks[5], (DEPTH, D_MODEL)),
        "w_in": nrm(ks[6], (DEPTH, D_MODEL, IN_WIDTH), D_MODEL ** -0.5),
        "g_sgu_norm": gain(ks[7], (DEPTH, SGU_WIDTH)),
        "w_sgu": nrm(ks[8], (DEPTH, N_SGU_GROUPS, SGU_CHUNK, SGU_CHUNK), SGU_CHUNK ** -0.5),
        "b_sgu": gain(ks[9], (DEPTH, N_SGU_GROUPS, SGU_CHUNK)),
        "g_attn_out": gain(ks[10], (DEPTH, ATTN_WIDTH)),
        "g_sgu_out": gain(ks[11], (DEPTH, SGU_WIDTH)),
        "w_out": nrm(ks[12], (DEPTH, D_MODEL, D_MODEL), D_MODEL ** -0.5),
        "g_pre_ffn": gain(ks[13], (DEPTH, D_MODEL)),
        "g_post_ffn": gain(ks[14], (DEPTH, D_MODEL)),
        "w_gate_up": nrm(ks[15], (DEPTH, D_MODEL, 2 * D_FF), D_MODEL ** -0.5),
        "w_down": nrm(ks[16], (DEPTH, D_FF, D_MODEL), D_FF ** -0.5),
    }


def reference(x, c, w_ada, b_ada, g_pre_mix, g_post_mix, w_in, g_sgu_norm, w_sgu, b_sgu,
              g_attn_out, g_sgu_out, w_out, g_pre_ffn, g_post_ffn, w_gate_up, w_down):
    B, S, D = x.shape
    c_act = jax.nn.silu(c)
    for l in range(DEPTH):
        mod = c_act @ w_ada[l] + b_ada[l]
        shift_m, scale_m, gate_m, shift_f, scale_f, gate_f = jnp.split(mod, N_MOD, axis=-1)

        h = modulate(rms_norm(x, g_pre_mix[l]), shift_m, scale_m)
        proj = h @ w_in[l]
        q, k, v, u, vg = jnp.split(
            proj, [ATTN_WIDTH, 2 * ATTN_WIDTH, 3 * ATTN_WIDTH, 3 * ATTN_WIDTH + SGU_WIDTH], axis=-1)
        to_heads = lambda t: t.reshape(B, S, N_ATTN_HEADS, HEAD_DIM).transpose(0, 2, 1, 3)
        attn = moba_attention(to_heads(q), to_heads(k), to_heads(v))
        attn = attn.transpose(0, 2, 1, 3).reshape(B, S, ATTN_WIDTH)
        sgu = spatial_gating(u, vg, w_sgu[l], b_sgu[l], g_sgu_norm[l])
        mix = jnp.concatenate([rms_norm(attn, g_attn_out[l]), rms_norm(sgu, g_sgu_out[l])], axis=-1)
        mix = mix @ w_out[l]
        x = x + gate_m[:, None, :] * rms_norm(mix, g_post_mix[l])

        h = modulate(rms_norm(x, g_pre_ffn[l]), shift_f, scale_f)
        g, up = jnp.split(h @ w_gate_up[l], 2, axis=-1)
        y = (jax.nn.silu(g) * up) @ w_down[l]
        x = x + gate_f[:, None, :] * rms_norm(y, g_post_ffn[l])
    return x
```

```python
from contextlib import ExitStack
import numpy as np
import concourse.bass as bass
import concourse.mybir as mybir
from concourse.bass_utils import run_bass_kernel_spmd

F32, BF16 = mybir.dt.float32, mybir.dt.bfloat16
AF = mybir.ActivationFunctionType
ALU = mybir.AluOpType
AX = mybir.AxisListType

S, D, NH, HD = 4096, 1024, 8, 64
DFF = 2816
NFC = DFF // 128
EPS = 1e-6
NEGM = -30000.0
GELU_C = 0.7978845608028654
STRICT = False
LOOK = 0
INTERLEAVE = False


class Op:
    __slots__ = ("eng", "fn", "deps", "signal", "cnt", "dma", "sem", "target")


class Tr:
    ENG = ("pe", "act", "dve", "pool", "sp")

    def __init__(self):
        self.streams = {e: [] for e in self.ENG}
        self.last_w = {}
        self.readers = {}

    def _add(self, eng, fn, r, w, dma):
        op = Op()
        op.eng, op.fn, op.dma, op.signal, op.cnt, op.sem, op.target = eng, fn, dma, False, 0, None, 0
        deps = set()
        for k in r:
            p = self.last_w.get(k)
            if p is not None and (STRICT or not (eng == "pe" and p.eng == "pe" and not p.dma)):
                deps.add(p)
        for k in w:
            p = self.last_w.get(k)
            if p is not None and (STRICT or p.eng != eng or p.dma or dma):
                deps.add(p)
            for q in self.readers.get(k, ()):
                if STRICT or q.eng != eng or q.dma or dma:
                    deps.add(q)
        op.deps = deps
        for d in deps:
            d.signal = True
        for k in r:
            self.readers.setdefault(k, []).append(op)
        for k in w:
            self.last_w[k] = op
            self.readers[k] = []
        self.streams[eng].append(op)
        return op

    def op(self, eng, fn, r=(), w=()):
        return self._add(eng, fn, r, w, False)

    def dma(self, q, out, in_, r=(), w=()):
        return self._add(q, lambda e, o=out, i=in_: e.dma_start(out=o, in_=i), r, w, True)

    def emit(self, nc, stack, tag, ndma_sems=12):
        engsem = {e: stack.enter_context(nc.semaphore(f"{tag}_es_{e}")) for e in ("pe", "act", "dve", "pool")}
        dsems = {q: [stack.enter_context(nc.semaphore(f"{tag}_ds_{q}{i}")) for i in range(ndma_sems)]
                 for q in ("sp", "pool", "act") if any(o.dma for o in self.streams[q])}
        for e in self.ENG:
            c = 0
            nd = 0
            for o in self.streams[e]:
                if o.dma:
                    o.sem = dsems[e][nd % ndma_sems]
                    o.target = 16 * (nd // ndma_sems + 1)
                    nd += 1
                elif o.signal:
                    c += 1
                    o.cnt = c
        block = stack.enter_context(nc.Block())

        def run(ename, eng):
            waited = {}
            for o in self.streams[ename]:
                need = {}
                for d in o.deps:
                    if d.dma:
                        s_, v_ = d.sem, d.target
                    else:
                        s_, v_ = engsem[d.eng], d.cnt
                    if need.get(id(s_), (None, 0))[1] < v_:
                        need[id(s_)] = (s_, v_)
                if o.dma and o.target > 16:
                    if need.get(id(o.sem), (None, 0))[1] < o.target - 16:
                        need[id(o.sem)] = (o.sem, o.target - 16)
                for sid, (s_, v_) in need.items():
                    if waited.get(sid, 0) < v_:
                        eng.wait_ge(s_, v_)
                        waited[sid] = v_
                if o.fn is None:
                    continue
                ins = o.fn(eng)
                if o.dma:
                    ins.then_inc(o.sem, 16)
                elif o.signal:
                    ins.then_inc(engsem[ename], 1)

        @block.tensor
        def _(e):
            run("pe", e)

        @block.scalar
        def _(e):
            run("act", e)

        @block.vector
        def _(e):
            run("dve", e)

        @block.gpsimd
        def _(e):
            run("pool", e)

        @block.sync
        def _(e):
            run("sp", e)


def build_program():
    nc = bass.Bass("TRN2", target_bir_lowering=False)
    dt = lambda n, s, d=F32, k="ExternalInput": nc.dram_tensor(n, s, d, kind=k).ap()
    x_d = dt("x", [S, D])
    ct_d = dt("ct", [128, 8])
    wada_d = dt("w_ada", [D, 6 * D])
    bada_d = dt("b_ada", [1, 6 * D])
    gpm_d = dt("gpm_t", [128, 8])
    gpf_d = dt("gpf_t", [128, 8])
    gmix_d = dt("gmix_t", [128, 8])
    gpostm_d = dt("g_post_mix", [1, D])
    gpostf_d = dt("g_post_ffn", [1, D])
    gnorm_d = dt("g_sgu_norm", [1, 512])
    bsgu_d = dt("bsgu_t", [128, 4])
    wsgu_d = dt("wsgu_t", [128, 4, 128])
    win_d = dt("w_in", [D, 2560])
    wout_d = dt("w_out", [D, D])
    wgu_d = dt("w_gate_up", [D, 2 * DFF])
    wdn_d = dt("w_down", [DFF, D])
    ident_d = dt("ident", [128, 128])
    tri_d = dt("tri01", [128, 128])
    oneh_d = dt("onehot", [16, S])
    out_d = dt("out", [S, D], F32, "ExternalOutput")
    mix_d = nc.dram_tensor("mixscr", [32, 128, 8 * 128], BF16).ap()

    with ExitStack() as gl:
        sb = lambda st, n, s, d=F32: st.enter_context(nc.sbuf_tensor(n, s, d))
        PS = [gl.enter_context(nc.psum_tensor(f"ps{i}", [128, 512], F32)) for i in range(8)]
        ident_f = sb(gl, "ident_f", [128, 128])
        ident_b = sb(gl, "ident_b", [128, 128], BF16)
        tri01 = sb(gl, "tri01s", [128, 128])
        tribias = sb(gl, "tribias", [128, 128], BF16)
        mhalf = sb(gl, "mhalf", [128, 8])
        A_m = sb(gl, "A_m", [128, 8]); B_m = sb(gl, "B_m", [128, 8])
        A_f = sb(gl, "A_f", [128, 8]); B_f = sb(gl, "B_f", [128, 8])
        gmix = sb(gl, "gmix", [128, 8])
        crep = sb(gl, "crep", [128, 8, 128], BF16)
        wada_v = wada_d.rearrange("(kc p) n -> p kc n", p=128)

        def ada_section(tr, stk, js, nslots, sink, tg):
            wa = [sb(stk, f"{tg}wa{i}", [128, 8, 512], BF16) for i in range(nslots)]
            bb = [sb(stk, f"{tg}bb{i}", [128, 512]) for i in range(nslots)]
            modc = [sb(stk, f"{tg}modc{i}", [128, 512]) for i in range(nslots)]
            for n_, j in enumerate(js):
                sl = n_ % nslots
                cs = slice(j * 512, (j + 1) * 512)
                tr.dma("pool", wa[sl][:], wada_v[:, :, cs], w=[("wa", sl)])
                tr.dma("sp", bb[sl][:], bada_d[:, cs].partition_broadcast(128), w=[("bb", sl)])
                bank = PS[sl]
                for kc in range(8):
                    tr.op("pe", lambda e, kc=kc, sl=sl, bank=bank: e.matmul(
                        bank[:, :], lhsT=crep[:, kc, :], rhs=wa[sl][:, kc, :], start=(kc == 0), stop=(kc == 7)),
                        r=["crep", ("wa", sl)], w=[("ps", sl)])
                tr.op("dve", lambda e, sl=sl, bank=bank: e.tensor_tensor(
                    out=modc[sl][:], in0=bank[:, :], in1=bb[sl][:], op=ALU.add),
                    r=[("ps", sl), ("bb", sl)], w=[("modc", sl)])
                sink(j, modc[sl], ("modc", sl))

        with ExitStack() as p0:
            tr = Tr()
            ctile = sb(p0, "ctile", [128, 8]); cact = sb(p0, "cact", [128, 8])
            gpm = sb(p0, "gpm", [128, 8]); gpf = sb(p0, "gpf", [128, 8])
            dtmp = sb(p0, "dtmp", [128, 4, 128])
            pp = sb(p0, "pp", [128, 4, 8])
            tr.dma("sp", ident_f[:], ident_d[:, :], w=["ident_f"])
            tr.dma("sp", tri01[:], tri_d[:, :], w=["tri01"])
            tr.dma("sp", ctile[:], ct_d[:, :], w=["ctile"])
            tr.dma("sp", gpm[:], gpm_d[:, :], w=["gpm"])
            tr.dma("sp", gpf[:], gpf_d[:, :], w=["gpf"])
            tr.dma("sp", gmix[:], gmix_d[:, :], w=["gmix"])
            tr.op("dve", lambda e: e.tensor_copy(out=ident_b[:], in_=ident_f[:]), r=["ident_f"], w=["ident_b"])
            tr.op("dve", lambda e: e.tensor_scalar(out=tribias[:], in0=tri01[:], scalar1=-NEGM, scalar2=NEGM,
                                                   op0=ALU.mult, op1=ALU.add), r=["tri01"], w=["tribias"])
            tr.op("pool", lambda e: e.memset(mhalf[:], -0.5), w=["mhalf"])
            tr.op("act", lambda e: e.activation(out=cact[:], in_=ctile[:], func=AF.Silu), r=["ctile"], w=["cact"])
            tr.op("dve", lambda e: e.tensor_copy(out=crep[:], in_=cact[:].unsqueeze(2).to_broadcast([128, 8, 128])),
                  r=["cact"], w=["crep"])
            def sink0(j, mc, mk):
                v, half = j // 2, j % 2
                vi = {0: 0, 1: 1, 3: 2, 4: 3}[v]
                tr.op("dve", lambda e, mc=mc: e.tensor_tensor(
                    out=dtmp[:], in0=mc[:].rearrange("p (a b) -> p a b", a=4),
                    in1=ident_f[:].unsqueeze(1).to_broadcast([128, 4, 128]), op=ALU.mult),
                    r=[mk, "ident_f"], w=["dtmp"])
                tr.op("dve", lambda e, vi=vi, half=half: e.tensor_reduce(
                    out=pp[:, vi, half * 4:(half + 1) * 4], in_=dtmp[:], axis=AX.X, op=ALU.add),
                    r=["dtmp"], w=["pp"])
            ada_section(tr, p0, [0, 1, 2, 3, 6, 7, 8, 9], 2, sink0, "a0")
            tr.op("dve", lambda e: e.scalar_tensor_tensor(out=A_m[:], in0=pp[:, 1, :], scalar=1.0, in1=gpm[:],
                                                          op0=ALU.add, op1=ALU.mult), r=["pp", "gpm"], w=["A_m"])
            tr.op("dve", lambda e: e.tensor_copy(out=B_m[:], in_=pp[:, 0, :]), r=["pp"], w=["B_m"])
            tr.op("dve", lambda e: e.scalar_tensor_tensor(out=A_f[:], in0=pp[:, 3, :], scalar=1.0, in1=gpf[:],
                                                          op0=ALU.add, op1=ALU.mult), r=["pp", "gpf"], w=["A_f"])
            tr.op("dve", lambda e: e.tensor_copy(out=B_f[:], in_=pp[:, 2, :]), r=["pp"], w=["B_f"])
            tr.op("sp", None, r=["crep", "A_m", "B_m", "A_f", "B_f", "gmix", "ident_b", "tribias", "mhalf"])
            tr.emit(nc, p0, "p0")

        with ExitStack() as p1:
            tr = Tr()
            win = sb(p1, "win", [128, 8, 2560], BF16)
            KT = sb(p1, "KT", [80, NH, S], BF16)
            V = sb(p1, "V", [128, 32, NH, 65], BF16)
            kmT = sb(p1, "kmT", [64, NH, 16], BF16)
            ksum = sb(p1, "ksum", [128, 2])
            hT = sb(p1, "hT", [128, 8, 512], BF16)
            QA = sb(p1, "QA", [80, NH, 512], BF16)
            xin = [sb(p1, f"xin{i}", [128, D]) for i in range(1)]
            xn = sb(p1, "xn", [128, D], BF16)
            st = sb(p1, "st", [128, 16])
            Pb = [sb(p1, f"Pb{i}", [128, 512], BF16) for i in range(3)]
            OT = sb(p1, "OT", [65, 512])
            rec = sb(p1, "rec", [128, 4])
            attn = sb(p1, "attn", [128, 4, 512])
            an = sb(p1, "an", [128, D], BF16)
            uh = sb(p1, "uh", [128, 512]); x2 = sb(p1, "x2", [128, 512])
            gvg = sb(p1, "gvg", [128, 512]); y2 = sb(p1, "y2", [128, 512])
            vgn = sb(p1, "vgn", [128, 512], BF16)
            bnst = sb(p1, "bnst", [128, 4, 6]); mv = sb(p1, "mv", [128, 4, 2])
            mixT = sb(p1, "mixT", [128, 8, 128], BF16)
            sgn = sb(p1, "sgn", [128, 4, 512], BF16)
            gnorm = sb(p1, "gnorm", [128, 512])
            bsgu = sb(p1, "bsgu", [128, 4])
            wsT = sb(p1, "wsT", [128, 4, 128], BF16)
            gsb = sb(p1, "gsb", [128, NH, 16]); top8 = sb(p1, "top8", [128, NH, 8])
            mb = sb(p1, "mb", [128, NH, 80], BF16)
            junk = sb(p1, "junk", [128, 512], BF16)

            win_v = win_d.rearrange("(kc p) n -> p kc n", p=128)
            for i in range(5):
                tr.dma("pool", win[:, :, i * 512:(i + 1) * 512], win_v[:, :, i * 512:(i + 1) * 512], w=["win"])
            for h in range(NH):
                tr.dma("pool", KT[64:80, h, :], oneh_d[:, :], w=[("KTo", h)])
            tr.dma("sp", gnorm[:], gnorm_d.partition_broadcast(128), w=["gnorm"])
            tr.dma("sp", bsgu[:], bsgu_d[:, :], w=["bsgu"])
            wsf = y2[:].rearrange("p (g t) -> p g t", g=4)
            tr.dma("sp", wsf, wsgu_d[:, :, :], w=["y2"])
            tr.op("dve", lambda e: e.tensor_tensor(out=wsT[:], in0=wsf,
                                                   in1=tri01[:].unsqueeze(1).to_broadcast([128, 4, 128]), op=ALU.mult),
                  r=["y2"], w=["wsT"])
            tr.op("pool", lambda e: e.memset(V[:, :, :, 64:65], 1.0), w=["Vones"])
            tr.op("pool", lambda e: e.memset(QA[64:80, :, :], 0.0), w=["QAm"])
            tr.op("pool", lambda e: e.memset(gsb[:], -1e30), w=["gsb"])
            tr.op("pool", lambda e: e.memset(mb[:], 0.0), w=["mb"])
            tr.op("pool", lambda e: e.memset(kmT[:], 0.0), w=["kmT"])

            def rstd_ops(src_key, src_ap, dst_ap, dst_key, n):
                tr.op("pool", lambda e: e.tensor_scalar(out=dst_ap, in0=src_ap, scalar1=1.0 / n, scalar2=EPS,
                                                        op0=ALU.mult, op1=ALU.add), r=[src_key], w=[dst_key])
                tr.op("pool", lambda e: e.tensor_tensor(out=dst_ap, in0=dst_ap, in1=mhalf[:, 0:1], op=ALU.pow),
                      r=[dst_key], w=[dst_key])

            MBb = PS[6][:].bitcast(BF16)
            MF = PS[7]
            sctr = [0]
            gctr = [0]

            def gbank():
                i = gctr[0] % 2
                gctr[0] += 1
                return PS[i], ("ps", i)

            def prologue_a(G, tt):
                tok = slice(G * 512 + tt * 128, G * 512 + (tt + 1) * 128)
                xi = 0
                xt = xin[xi]
                tr.dma("sp", xt[:], x_d[tok, :], w=[("xin", xi)])
                tr.op("act", lambda e, xt=xt: e.activation(out=xn[:], in_=xt[:], func=AF.Square,
                                                           accum_out=st[:, 0:1]),
                      r=[("xin", xi)], w=["xn", "st0"])
                rstd_ops("st0", st[:, 0:1], st[:, 1:2], "st1", D)
                tr.op("dve", lambda e, xt=xt: e.tensor_scalar(out=xn[:], in0=xt[:], scalar1=st[:, 1:2], scalar2=None,
                                                              op0=ALU.mult), r=[("xin", xi), "st1"], w=["xn"])
                for c in range(8):
                    tr.op("pe", lambda e, c=c: e.transpose(out=MBb[:, c * 128:(c + 1) * 128],
                                                           in_=xn[:, c * 128:(c + 1) * 128], identity=ident_b[:]),
                          r=["xn"], w=["MB"])
                for c in range(8):
                    tr.op("dve", lambda e, c=c, tt=tt: e.tensor_scalar(
                        out=hT[:, c, tt * 128:(tt + 1) * 128], in0=MBb[:, c * 128:(c + 1) * 128],
                        scalar1=A_m[:, c:c + 1], scalar2=B_m[:, c:c + 1], op0=ALU.mult, op1=ALU.add),
                        r=["MB"], w=[("hT", tt)])

            hTk = [("hT", t) for t in range(4)]

            def inproj_qk(G, oc):
                bank, bk = gbank()
                for kc in range(8):
                    tr.op("pe", lambda e, oc=oc, kc=kc, bank=bank: e.matmul(
                        bank[:, :], lhsT=win[:, kc, oc * 128:(oc + 1) * 128], rhs=hT[:, kc, :],
                        start=(kc == 0), stop=(kc == 7)), r=["win"] + hTk, w=[bk])
                for hh in range(2):
                    h = (oc % 4) * 2 + hh
                    rows = slice(hh * 64, (hh + 1) * 64)
                    if oc < 4:
                        tr.op("dve", lambda e, h=h, rows=rows, bank=bank: e.tensor_copy(
                            out=QA[0:64, h, :], in_=bank[rows, :]), r=[bk], w=[("QA", h)])
                    else:
                        tr.op("dve", lambda e, h=h, rows=rows, bank=bank, G=G: e.tensor_copy(
                            out=KT[0:64, h, G * 512:(G + 1) * 512], in_=bank[rows, :]), r=[bk], w=[("KT", h, G)])
                if oc >= 4:
                    tr.op("dve", lambda e, bank=bank: e.tensor_reduce(
                        out=ksum[:], in_=bank[:, :].rearrange("p (a b) -> p a b", a=2), axis=AX.X, op=ALU.add),
                        r=[bk], w=["ksum"])
                    for hh in range(2):
                        h = (oc % 4) * 2 + hh
                        tr.op("dve", lambda e, h=h, hh=hh, G=G: e.tensor_copy(
                            out=kmT[0:64, h, 2 * G:2 * G + 2], in_=ksum[hh * 64:(hh + 1) * 64, :]),
                            r=["ksum"], w=[("kmT", h)])

            def inproj_vug(G, tt):
                ts_ = slice(tt * 128, (tt + 1) * 128)
                gt = G * 4 + tt
                bank, bk = gbank()
                for kc in range(8):
                    tr.op("pe", lambda e, kc=kc, bank=bank, ts_=ts_: e.matmul(
                        bank[:, :], lhsT=hT[:, kc, ts_], rhs=win[:, kc, 1024:1536],
                        start=(kc == 0), stop=(kc == 7)), r=["win", ("hT", tt)], w=[bk])
                tr.op("dve", lambda e, bank=bank, gt=gt: e.tensor_copy(
                    out=V[:, gt, :, 0:64], in_=bank[:, :].rearrange("p (h d) -> p h d", h=NH)),
                    r=[bk], w=[("V", gt)])
                for which in range(2):
                    dst = uh if which == 0 else gvg
                    dk = "uh" if which == 0 else "gvg"
                    co = 1536 + which * 512
                    bank, bk = gbank()
                    for kc in range(8):
                        tr.op("pe", lambda e, kc=kc, bank=bank, ts_=ts_, co=co: e.matmul(
                            bank[:, :], lhsT=hT[:, kc, ts_], rhs=win[:, kc, co:co + 512],
                            start=(kc == 0), stop=(kc == 7)), r=["win", ("hT", tt)], w=[bk])
                    tr.op("act", lambda e, bank=bank, dst=dst: e.activation(out=dst[:], in_=bank[:, :], func=AF.Identity,
                                                                            scale=0.5), r=[bk], w=[dk])
                    tr.op("act", lambda e, bank=bank: e.activation(out=x2[:], in_=bank[:, :], func=AF.Square),
                          r=[bk], w=["x2"])
                    tr.op("pool", lambda e: e.tensor_scalar(out=x2[:], in0=x2[:], scalar1=0.044715, scalar2=1.0,
                                                            op0=ALU.mult, op1=ALU.add), r=["x2"], w=["x2"])
                    tr.op("pool", lambda e, dst=dst: e.tensor_tensor(out=x2[:], in0=x2[:], in1=dst[:], op=ALU.mult),
                          r=["x2", dk], w=["x2"])
                    tr.op("act", lambda e: e.activation(out=x2[:], in_=x2[:], func=AF.Tanh, scale=2.0 * GELU_C),
                          r=["x2"], w=["x2"])
                    tr.op("dve", lambda e, dst=dst: e.scalar_tensor_tensor(
                        out=dst[:], in0=x2[:], scalar=1.0, in1=dst[:], op0=ALU.add, op1=ALU.mult),
                        r=["x2", dk], w=[dk])
                for g in range(4):
                    tr.op("dve", lambda e, g=g: e.bn_stats(out=bnst[:, g, :], in_=gvg[:, g * 128:(g + 1) * 128]),
                          r=["gvg"], w=["bnst"])
                for g in range(4):
                    tr.op("dve", lambda e, g=g: e.bn_aggr(out=mv[:, g, :], in_=bnst[:, g, :]), r=["bnst"], w=["mv"])
                tr.op("pool", lambda e: e.tensor_scalar(out=st[:, 4:8], in0=mv[:, :, 1], scalar1=1.0, scalar2=EPS,
                                                        op0=ALU.mult, op1=ALU.add), r=["mv"], w=["st4"])
                tr.op("pool", lambda e: e.tensor_tensor(out=st[:, 4:8], in0=st[:, 4:8], in1=mhalf[:, 0:4], op=ALU.pow),
                      r=["st4"], w=["st4"])
                for g in range(4):
                    tr.op("dve", lambda e, g=g: e.tensor_scalar(
                        out=gvg[:, g * 128:(g + 1) * 128], in0=gvg[:, g * 128:(g + 1) * 128],
                        scalar1=mv[:, g, 0:1], scalar2=st[:, 4 + g:5 + g], op0=ALU.subtract, op1=ALU.mult),
                        r=["gvg", "mv", "st4"], w=["gvg"])
                tr.op("dve", lambda e: e.tensor_tensor(out=vgn[:], in0=gvg[:], in1=gnorm[:], op=ALU.mult),
                      r=["gvg", "gnorm"], w=["vgn"])
                bank, bk = gbank()
                for g in range(4):
                    tr.op("pe", lambda e, g=g, bank=bank: e.matmul(
                        bank[:, g * 128:(g + 1) * 128], lhsT=wsT[:, g, :], rhs=vgn[:, g * 128:(g + 1) * 128],
                        start=True, stop=True), r=["wsT", "vgn"], w=[bk])
                for g in range(4):
                    tr.op("dve", lambda e, g=g, bank=bank: e.scalar_tensor_tensor(
                        out=y2[:, g * 128:(g + 1) * 128], in0=bank[:, g * 128:(g + 1) * 128],
                        scalar=bsgu[:, g:g + 1], in1=uh[:, g * 128:(g + 1) * 128], op0=ALU.add, op1=ALU.mult),
                        r=[bk, "uh", "bsgu"], w=["y2"])
                tr.op("act", lambda e: e.activation(out=junk[:], in_=y2[:], func=AF.Square,
                                                    accum_out=st[:, 8:9]), r=["y2"], w=["junk", "st8"])
                rstd_ops("st8", st[:, 8:9], st[:, 9:10], "st9", 512)
                tr.op("dve", lambda e, tt=tt: e.tensor_scalar(out=sgn[:, tt, :], in0=y2[:], scalar1=st[:, 9:10],
                                                               scalar2=None, op0=ALU.mult),
                      r=["y2", "st9"], w=[("sgn", tt)])

            def gate(G):
                if G < 2:
                    return
                for tt in range(4):
                    j = 2 * G + tt // 2
                    ts_ = slice(tt * 128, (tt + 1) * 128)
                    for h in range(NH):
                        tr.op("pe", lambda e, h=h, ts_=ts_: e.matmul(
                            MF[:, 384 + h * 16:384 + (h + 1) * 16], lhsT=QA[0:64, h, ts_], rhs=kmT[:, h, :],
                            start=True, stop=True), r=[("QA", h), ("kmT", h)], w=["MFg"])
                    tr.op("dve", lambda e, j=j: e.tensor_copy(
                        out=gsb[:, :, 0:j], in_=MF[:, 384:512].rearrange("p (h n) -> p h n", h=NH)[:, :, 0:j]),
                        r=["MFg"], w=["gsb"])
                    for h in range(NH):
                        tr.op("dve", lambda e, h=h: e.max(out=top8[:, h, :], in_=gsb[:, h, :]), r=["gsb"], w=["top8"])
                    for h in range(NH):
                        tr.op("dve", lambda e, h=h, j=j: e.tensor_scalar(
                            out=mb[:, h, 64:64 + j], in0=gsb[:, h, 0:j], scalar1=top8[:, h, 2:3], scalar2=NEGM,
                            op0=ALU.is_lt, op1=ALU.mult), r=["gsb", "top8"], w=["mb"])
                    for h in range(NH):
                        tr.op("pe", lambda e, h=h: e.transpose(out=MBb[0:80, h * 128:(h + 1) * 128],
                                                               in_=mb[:, h, :], identity=ident_b[:]),
                              r=["mb"], w=["MB"])
                    tr.op("dve", lambda e, ts_=ts_: e.tensor_copy(
                        out=QA[64:80, :, ts_], in_=MBb[64:80, :].rearrange("p (h t) -> p h t", h=NH)),
                        r=["MB"], w=["QAm"])

            def attn_items(G):
                return [(G, h, kt) for h in range(NH) for kt in range(4 * G + 4)]

            def qk(G, h, kt):
                c0 = 0 if kt <= 4 * G else (kt - 4 * G) * 128
                si = sctr[0] % 2
                sctr[0] += 1
                Sb = PS[2 + si]
                sk = ("ps", 2 + si)
                tri = kt >= 4 * G
                tr.op("pe", lambda e, h=h, kt=kt, c0=c0, Sb=Sb, tri=tri: e.matmul(
                    Sb[:, c0:512], lhsT=KT[0:80, h, kt * 128:(kt + 1) * 128], rhs=QA[0:80, h, c0:512],
                    start=True, stop=(not tri)),
                    r=[("KT", h, kt // 4), ("KTo", h), ("QA", h), "QAm"], w=[sk])
                if tri:
                    ct0 = (kt - 4 * G) * 128
                    tr.op("pe", lambda e, Sb=Sb, ct0=ct0: e.matmul(
                        Sb[:, ct0:ct0 + 128], lhsT=ident_b[:], rhs=tribias[:], start=False, stop=True),
                        r=[], w=[sk])
                return (Sb, sk, c0)

            def exp_pv(G, h, kt, sinfo, idx):
                Sb, sk, c0 = sinfo
                nkt = 4 * G + 4
                pi = idx % 3
                Ob = PS[4 + h % 2]
                ok = ("ps", 4 + h % 2)
                tr.op("act", lambda e, Sb=Sb, c0=c0, pi=pi: e.activation(
                    out=Pb[pi][:, c0:512], in_=Sb[:, c0:512], func=AF.Exp, scale=0.125),
                    r=[sk], w=[("Pb", pi)])

                def pv():
                    tr.op("pe", lambda e, h=h, kt=kt, c0=c0, pi=pi, Ob=Ob, nkt=nkt: e.matmul(
                        Ob[0:65, c0:512], lhsT=V[:, kt, h, :], rhs=Pb[pi][:, c0:512],
                        start=(kt == 0), stop=(kt == nkt - 1)),
                        r=[("V", kt), "Vones", ("Pb", pi)], w=[ok])
                    if kt == nkt - 1:
                        head_finish(G, h, Ob, ok)
                return pv

            def head_finish(G, h, Ob, ok):
                tr.op("dve", lambda e, Ob=Ob: e.tensor_copy(out=OT[:], in_=Ob[0:65, :]), r=[ok], w=["OT"])
                for tt in range(4):
                    tr.op("pe", lambda e, tt=tt: e.transpose(out=MF[:, tt * 65:(tt + 1) * 65],
                                                             in_=OT[:, tt * 128:(tt + 1) * 128],
                                                             identity=ident_f[0:65, 0:65]),
                          r=["OT"], w=["MFo"])
                MFo = MF[:, 0:260].rearrange("p (t d) -> p t d", t=4)
                tr.op("dve", lambda e, MFo=MFo: e.reciprocal(out=rec[:], in_=MFo[:, :, 64]), r=["MFo"], w=["rec"])
                tr.op("dve", lambda e, MFo=MFo, h=h: e.tensor_tensor(
                    out=attn[:, :, h * 64:(h + 1) * 64], in0=MFo[:, :, 0:64],
                    in1=rec[:].unsqueeze(2).to_broadcast([128, 4, 64]), op=ALU.mult),
                    r=["MFo", "rec"], w=["attn"])

            def epilogue(G, tt):
                tr.op("act", lambda e, tt=tt: e.activation(out=junk[:], in_=attn[:, tt, :], func=AF.Square,
                                                           accum_out=st[:, 10:11]), r=["attn"], w=["junk", "st10"])
                rstd_ops("st10", st[:, 10:11], st[:, 11:12], "st11", 512)
                tr.op("dve", lambda e, tt=tt: e.tensor_scalar(out=an[:, 0:512], in0=attn[:, tt, :],
                                                              scalar1=st[:, 11:12], scalar2=None, op0=ALU.mult),
                      r=["attn", "st11"], w=["an"])
                tr.op("dve", lambda e, tt=tt: e.tensor_copy(out=an[:, 512:1024], in_=sgn[:, tt, :]),
                      r=[("sgn", tt)], w=["an"])
                for c in range(8):
                    tr.op("pe", lambda e, c=c: e.transpose(out=MBb[:, c * 128:(c + 1) * 128],
                                                           in_=an[:, c * 128:(c + 1) * 128], identity=ident_b[:]),
                          r=["an"], w=["MB"])
                tr.op("dve", lambda e, tt=tt: e.tensor_tensor(
                    out=mixT[:, :, :], in0=MBb[:, :].rearrange("p (c t) -> p c t", c=8),
                    in1=gmix[:].unsqueeze(2).to_broadcast([128, 8, 128]), op=ALU.mult),
                    r=["MB"], w=["mixT"])
                tr.dma("sp", mix_d[G * 4 + tt].rearrange("p (c t) -> p c t", c=8), mixT[:], r=["mixT"], w=["mixd"])

            def attention(G, side):
                items = attn_items(G)
                n_it = len(items)
                sin = {}
                for i in range(min(LOOK, n_it)):
                    sin[i] = qk(*items[i])
                step = max(1, n_it // (len(side) + 1)) if side else n_it + 1
                for i in range(n_it):
                    if LOOK == 0:
                        sin[i] = qk(*items[i])
                    pv = exp_pv(*items[i], sin.pop(i), i)
                    if LOOK > 0 and i + LOOK < n_it:
                        sin[i + LOOK] = qk(*items[i + LOOK])
                    pv()
                    if side and (i + 1) % step == 0:
                        side.pop(0)()
                while side:
                    side.pop(0)()

            if INTERLEAVE:
                for tt in range(4):
                    prologue_a(0, tt)
                for oc in range(8):
                    inproj_qk(0, oc)
                for tt in range(4):
                    inproj_vug(0, tt)
                gate(0)
                for G in range(8):
                    side = []
                    if G + 1 < 8:
                        side = [(lambda t=t, G=G: prologue_a(G + 1, t)) for t in range(4)]
                    attention(G, side)
                    for tt in range(4):
                        epilogue(G, tt)
                        if G + 1 < 8:
                            inproj_qk(G + 1, tt)
                            inproj_qk(G + 1, tt + 4)
                            inproj_vug(G + 1, tt)
                    if G + 1 < 8:
                        gate(G + 1)
            else:
                for G in range(8):
                    for tt in range(4):
                        prologue_a(G, tt)
                    for oc in range(8):
                        inproj_qk(G, oc)
                    for tt in range(4):
                        inproj_vug(G, tt)
                    gate(G)
                    attention(G, [])
                    for tt in range(4):
                        epilogue(G, tt)
            tr.op("sp", None, r=["mixd"])
            tr.emit(nc, p1, "p1")

        with ExitStack() as p2:
            tr = Tr()
            wout = sb(p2, "wout", [128, 8, D], BF16)
            wgu = sb(p2, "wgu", [128, 8, 2 * DFF], BF16)
            wdn = sb(p2, "wdn", [128, NFC, D], BF16)
            actT = sb(p2, "actT", [128, NFC, 256], BF16)
            hb = sb(p2, "hb", [128, 8, 256], BF16)
            xt2 = [sb(p2, f"xt2{i}", [128, D]) for i in range(2)]
            xn2 = sb(p2, "xn2", [128, D], BF16)
            junk2 = sb(p2, "junk2", [128, D], BF16)
            ytmp = sb(p2, "ytmp", [128, 512])
            sg = sb(p2, "sg", [128, 256])
            st2 = sb(p2, "st2", [128, 16])
            GM = sb(p2, "GM", [128, D]); GF = sb(p2, "GF", [128, D])
            tr.dma("sp", GM[:], gpostm_d.partition_broadcast(128), w=["GM"])
            tr.dma("sp", GF[:], gpostf_d.partition_broadcast(128), w=["GF"])

            def sink2(j, mc, mk):
                G_ = GM if j < 6 else GF
                gk = "GM" if j < 6 else "GF"
                hs = slice((j % 2) * 512, (j % 2 + 1) * 512)
                tr.op("dve", lambda e, G_=G_, hs=hs, mc=mc: e.tensor_tensor(
                    out=G_[:, hs], in0=G_[:, hs], in1=mc[:], op=ALU.mult), r=[mk, gk], w=[gk])
            ada_section(tr, p2, [4, 5, 10, 11], 1, sink2, "a2")
            MBb = PS[6][:].bitcast(BF16)

            def rstd2(src_key, src_ap, dst_ap, dst_key, n):
                tr.op("pool", lambda e: e.tensor_scalar(out=dst_ap, in0=src_ap, scalar1=1.0 / n, scalar2=EPS,
                                                        op0=ALU.mult, op1=ALU.add), r=[src_key], w=[dst_key])
                tr.op("pool", lambda e: e.tensor_tensor(out=dst_ap, in0=dst_ap, in1=mhalf[:, 0:1], op=ALU.pow),
                      r=[dst_key], w=[dst_key])

            wout_v = wout_d.rearrange("(kc p) n -> p kc n", p=128)
            wgu_v = wgu_d.rearrange("(kc p) n -> p kc n", p=128)
            wdn_v = wdn_d.rearrange("(fc p) n -> p fc n", p=128)
            tr.dma("pool", wout[:], wout_v, w=["wout"])
            for fc in range(NFC):
                for pt in range(2):
                    co = pt * DFF + fc * 128
                    tr.dma("pool", wgu[:, :, co:co + 128], wgu_v[:, :, co:co + 128], w=[("wgu", fc)])
            for i in range(2):
                tr.dma("pool", wdn[:, i * 11:(i + 1) * 11, :], wdn_v[:, i * 11:(i + 1) * 11, :], w=["wdn"])
            bctr = [0]

            def pbank():
                i = bctr[0] % 6
                bctr[0] += 1
                return PS[i], ("ps", i)

            for G in range(16):
                for tt in range(2):
                    tr.dma("sp", hb[:, :, tt * 128:(tt + 1) * 128],
                           mix_d[G * 2 + tt].rearrange("p (c t) -> p c t", c=8), w=[("hb", tt)])
                for tt in range(2):
                    ts_ = slice(tt * 128, (tt + 1) * 128)
                    tok = slice(G * 256 + tt * 128, G * 256 + (tt + 1) * 128)
                    xt = xt2[tt]
                    xk = ("xt", tt)
                    tr.dma("sp", xt[:], x_d[tok, :], w=[xk])
                    for half in range(2):
                        hs = slice(half * 512, (half + 1) * 512)
                        bank, bk = pbank()
                        for kc in range(8):
                            tr.op("pe", lambda e, kc=kc, bank=bank, ts_=ts_, hs=hs: e.matmul(
                                bank[:, :], lhsT=hb[:, kc, ts_], rhs=wout[:, kc, hs], start=(kc == 0), stop=(kc == 7)),
                                r=["wout", ("hb", tt)], w=[bk])
                        tr.op("act", lambda e, bank=bank, half=half: e.activation(
                            out=junk2[:, 0:512], in_=bank[:, :], func=AF.Square, accum_out=st2[:, half:half + 1]),
                            r=[bk], w=["junk2", ("ssq", half)])
                        if half == 0:
                            b0, bk0 = bank, bk
                        else:
                            b1, bk1 = bank, bk
                    tr.op("pool", lambda e: e.tensor_tensor(out=st2[:, 2:3], in0=st2[:, 0:1], in1=st2[:, 1:2], op=ALU.add),
                          r=[("ssq", 0), ("ssq", 1)], w=["s2"])
                    rstd2("s2", st2[:, 2:3], st2[:, 3:4], "s3", D)
                    for half, (bank, bk) in enumerate(((b0, bk0), (b1, bk1))):
                        hs = slice(half * 512, (half + 1) * 512)
                        tr.op("dve", lambda e, bank=bank, hs=hs: e.scalar_tensor_tensor(
                            out=ytmp[:], in0=bank[:, :], scalar=st2[:, 3:4], in1=GM[:, hs], op0=ALU.mult, op1=ALU.mult),
                            r=[bk, "s3", "GM"], w=["ytmp"])
                        tr.op("dve", lambda e, xt=xt, hs=hs: e.tensor_tensor(out=xt[:, hs], in0=xt[:, hs], in1=ytmp[:],
                                                                             op=ALU.add), r=["ytmp", xk], w=[xk])
                    tr.op("act", lambda e, xt=xt: e.activation(out=junk2[:], in_=xt[:], func=AF.Square,
                                                               accum_out=st2[:, 4:5]), r=[xk], w=["junk2", "s4"])
                    rstd2("s4", st2[:, 4:5], st2[:, 5:6], "s5", D)
                    tr.op("dve", lambda e, xt=xt: e.tensor_scalar(out=xn2[:], in0=xt[:], scalar1=st2[:, 5:6], scalar2=None,
                                                                  op0=ALU.mult), r=[xk, "s5"], w=["xn2"])
                    for c in range(8):
                        tr.op("pe", lambda e, c=c: e.transpose(out=MBb[:, c * 128:(c + 1) * 128],
                                                               in_=xn2[:, c * 128:(c + 1) * 128], identity=ident_b[:]),
                              r=["xn2"], w=["MB"])
                    for c in range(8):
                        tr.op("dve", lambda e, c=c, ts_=ts_: e.tensor_scalar(
                            out=hb[:, c, ts_], in0=MBb[:, c * 128:(c + 1) * 128],
                            scalar1=A_f[:, c:c + 1], scalar2=B_f[:, c:c + 1], op0=ALU.mult, op1=ALU.add),
                            r=["MB"], w=[("hb", tt)])
                for fc in range(NFC):
                    gb, gk = pbank()
                    ub, uk = pbank()
                    for pt, bank, bk in ((0, gb, gk), (1, ub, uk)):
                        co = pt * DFF + fc * 128
                        for kc in range(8):
                            tr.op("pe", lambda e, kc=kc, bank=bank, co=co: e.matmul(
                                bank[:, 0:256], lhsT=wgu[:, kc, co:co + 128], rhs=hb[:, kc, :],
                                start=(kc == 0), stop=(kc == 7)),
                                r=[("wgu", fc), ("hb", 0), ("hb", 1)], w=[bk])
                    tr.op("act", lambda e, gb=gb: e.activation(out=sg[:], in_=gb[:, 0:256], func=AF.Silu),
                          r=[gk], w=["sg"])
                    tr.op("dve", lambda e, ub=ub, fc=fc: e.tensor_tensor(out=actT[:, fc, :], in0=sg[:], in1=ub[:, 0:256],
                                                                         op=ALU.mult), r=["sg", uk], w=[("actT", fc)])
                for tt in range(2):
                    ts_ = slice(tt * 128, (tt + 1) * 128)
                    tok = slice(G * 256 + tt * 128, G * 256 + (tt + 1) * 128)
                    xt = xt2[tt]
                    xk = ("xt", tt)
                    banks = []
                    for half in range(2):
                        hs = slice(half * 512, (half + 1) * 512)
                        bank, bk = pbank()
                        banks.append((bank, bk))
                        for fc in range(NFC):
                            tr.op("pe", lambda e, fc=fc, bank=bank, ts_=ts_, hs=hs: e.matmul(
                                bank[:, :], lhsT=actT[:, fc, ts_], rhs=wdn[:, fc, hs],
                                start=(fc == 0), stop=(fc == NFC - 1)),
                                r=["wdn", ("actT", fc)], w=[bk])
                        tr.op("act", lambda e, bank=bank, half=half: e.activation(
                            out=junk2[:, 0:512], in_=bank[:, :], func=AF.Square, accum_out=st2[:, 8 + half:9 + half]),
                            r=[bk], w=["junk2", ("ssq2", half)])
                    tr.op("pool", lambda e: e.tensor_tensor(out=st2[:, 10:11], in0=st2[:, 8:9], in1=st2[:, 9:10], op=ALU.add),
                          r=[("ssq2", 0), ("ssq2", 1)], w=["s10"])
                    rstd2("s10", st2[:, 10:11], st2[:, 11:12], "s11", D)
                    for half, (bank, bk) in enumerate(banks):
                        hs = slice(half * 512, (half + 1) * 512)
                        tr.op("dve", lambda e, bank=bank, hs=hs: e.scalar_tensor_tensor(
                            out=ytmp[:], in0=bank[:, :], scalar=st2[:, 11:12], in1=GF[:, hs], op0=ALU.mult, op1=ALU.mult),
                            r=[bk, "s11", "GF"], w=["ytmp"])
                        tr.op("dve", lambda e, xt=xt, hs=hs: e.tensor_tensor(out=xt[:, hs], in0=xt[:, hs], in1=ytmp[:],
                                                                             op=ALU.add), r=["ytmp", xk], w=[xk])
                    tr.dma("sp", out_d[tok, :], xt[:], r=[xk], w=["outd"])
            tr.op("sp", None, r=["outd"])
            tr.emit(nc, p2, "p2")
    return nc


def _consts():
    ident = np.eye(128, dtype=np.float32)
    p = np.arange(128)
    tri01 = (p[:, None] <= p[None, :]).astype(np.float32)
    onehot = (np.arange(S)[None, :] // 256 == np.arange(16)[:, None]).astype(np.float32)
    return ident, tri01, onehot


def kernel(x, c, w_ada, b_ada, g_pre_mix, g_post_mix, w_in, g_sgu_norm, w_sgu, b_sgu,
           g_attn_out, g_sgu_out, w_out, g_pre_ffn, g_post_ffn, w_gate_up, w_down):
    f = lambda a: np.ascontiguousarray(np.asarray(a, dtype=np.float32))
    x = f(x); c = f(c)
    ident, tri01, onehot = _consts()
    pp = lambda v: f(np.asarray(v).reshape(-1, 128).T)
    shared = {
        "w_ada": f(w_ada[0]), "b_ada": f(b_ada[0]).reshape(1, -1),
        "gpm_t": pp(g_pre_mix[0]), "gpf_t": pp(g_pre_ffn[0]),
        "gmix_t": f(np.concatenate([pp(g_attn_out[0]), pp(g_sgu_out[0])], axis=1)),
        "g_post_mix": f(g_post_mix[0]).reshape(1, -1), "g_post_ffn": f(g_post_ffn[0]).reshape(1, -1),
        "g_sgu_norm": f(g_sgu_norm[0]).reshape(1, -1),
        "bsgu_t": f(np.asarray(b_sgu[0]).T),
        "wsgu_t": f(np.transpose(np.asarray(w_sgu[0]), (2, 0, 1))),
        "w_in": f(w_in[0]), "w_out": f(w_out[0]), "w_gate_up": f(w_gate_up[0]), "w_down": f(w_down[0]),
        "ident": ident, "tri01": tri01, "onehot": onehot,
    }
    in_maps = []
    for b in range(8):
        m = dict(shared)
        m["x"] = x[b]
        m["ct"] = pp(c[b])
        in_maps.append(m)
    nc = build_program()
    res = run_bass_kernel_spmd(nc, in_maps, core_ids=list(range(8)))
    return np.stack([np.asarray(r["out"], dtype=np.float32) for r in res.results], axis=0)
```

```python
from contextlib import ExitStack
import numpy as np
import concourse.bass as bass
import concourse.mybir as mybir
from concourse.bass_utils import run_bass_kernel_spmd

F32, BF16 = mybir.dt.float32, mybir.dt.bfloat16
AF = mybir.ActivationFunctionType
ALU = mybir.AluOpType
AX = mybir.AxisListType

S, D, NH, HD = 4096, 1024, 8, 64
DFF = 2816
NFC = DFF // 128
EPS = 1e-6
NEGM = -30000.0
GELU_C = 0.7978845608028654
STRICT = False
LOOK = 1
INTERLEAVE = False


class Op:
    __slots__ = ("eng", "fn", "deps", "signal", "cnt", "dma", "sem", "target")


class Tr:
    ENG = ("pe", "act", "dve", "pool", "sp")

    def __init__(self):
        self.streams = {e: [] for e in self.ENG}
        self.last_w = {}
        self.readers = {}

    def _add(self, eng, fn, r, w, dma):
        op = Op()
        op.eng, op.fn, op.dma, op.signal, op.cnt, op.sem, op.target = eng, fn, dma, False, 0, None, 0
        deps = set()
        for k in r:
            p = self.last_w.get(k)
            if p is not None and (STRICT or not (eng == "pe" and p.eng == "pe" and not p.dma)):
                deps.add(p)
        for k in w:
            p = self.last_w.get(k)
            if p is not None and (STRICT or p.eng != eng or p.dma or dma):
                deps.add(p)
            for q in self.readers.get(k, ()):
                if STRICT or q.eng != eng or q.dma or dma:
                    deps.add(q)
        op.deps = deps
        for d in deps:
            d.signal = True
        for k in r:
            self.readers.setdefault(k, []).append(op)
        for k in w:
            self.last_w[k] = op
            self.readers[k] = []
        self.streams[eng].append(op)
        return op

    def op(self, eng, fn, r=(), w=()):
        return self._add(eng, fn, r, w, False)

    def dma(self, q, out, in_, r=(), w=()):
        return self._add(q, lambda e, o=out, i=in_: e.dma_start(out=o, in_=i), r, w, True)

    def emit(self, nc, stack, tag, ndma_sems=12):
        engsem = {e: stack.enter_context(nc.semaphore(f"{tag}_es_{e}")) for e in ("pe", "act", "dve", "pool")}
        dsems = {q: [stack.enter_context(nc.semaphore(f"{tag}_ds_{q}{i}")) for i in range(ndma_sems)]
                 for q in ("sp", "pool", "act") if any(o.dma for o in self.streams[q])}
        for e in self.ENG:
            c = 0
            nd = 0
            for o in self.streams[e]:
                if o.dma:
                    o.sem = dsems[e][nd % ndma_sems]
                    o.target = 16 * (nd // ndma_sems + 1)
                    nd += 1
                elif o.signal:
                    c += 1
                    o.cnt = c
        block = stack.enter_context(nc.Block())

        def run(ename, eng):
            waited = {}
            for o in self.streams[ename]:
                need = {}
                for d in o.deps:
                    if d.dma:
                        s_, v_ = d.sem, d.target
                    else:
                        s_, v_ = engsem[d.eng], d.cnt
                    if need.get(id(s_), (None, 0))[1] < v_:
                        need[id(s_)] = (s_, v_)
                if o.dma and o.target > 16:
                    if need.get(id(o.sem), (None, 0))[1] < o.target - 16:
                        need[id(o.sem)] = (o.sem, o.target - 16)
                for sid, (s_, v_) in need.items():
                    if waited.get(sid, 0) < v_:
                        eng.wait_ge(s_, v_)
                        waited[sid] = v_
                if o.fn is None:
                    continue
                ins = o.fn(eng)
                if o.dma:
                    ins.then_inc(o.sem, 16)
                elif o.signal:
                    ins.then_inc(engsem[ename], 1)

        @block.tensor
        def _(e):
            run("pe", e)

        @block.scalar
        def _(e):
            run("act", e)

        @block.vector
        def _(e):
            run("dve", e)

        @block.gpsimd
        def _(e):
            run("pool", e)

        @block.sync
        def _(e):
            run("sp", e)


def build_program():
    nc = bass.Bass("TRN2", target_bir_lowering=False)
    dt = lambda n, s, d=F32, k="ExternalInput": nc.dram_tensor(n, s, d, kind=k).ap()
    x_d = dt("x", [S, D])
    ct_d = dt("ct", [128, 8])
    wada_d = dt("w_ada", [D, 6 * D])
    bada_d = dt("b_ada", [1, 6 * D])
    gpm_d = dt("gpm_t", [128, 8])
    gpf_d = dt("gpf_t", [128, 8])
    gmix_d = dt("gmix_t", [128, 8])
    gpostm_d = dt("g_post_mix", [1, D])
    gpostf_d = dt("g_post_ffn", [1, D])
    gnorm_d = dt("g_sgu_norm", [1, 512])
    bsgu_d = dt("bsgu_t", [128, 4])
    wsgu_d = dt("wsgu_t", [128, 4, 128])
    win_d = dt("w_in", [D, 2560])
    wout_d = dt("w_out", [D, D])
    wgu_d = dt("w_gate_up", [D, 2 * DFF])
    wdn_d = dt("w_down", [DFF, D])
    ident_d = dt("ident", [128, 128])
    tri_d = dt("tri01", [128, 128])
    oneh_d = dt("onehot", [16, S])
    out_d = dt("out", [S, D], F32, "ExternalOutput")
    mix_d = nc.dram_tensor("mixscr", [32, 128, 8 * 128], BF16).ap()

    with ExitStack() as gl:
        sb = lambda st, n, s, d=F32: st.enter_context(nc.sbuf_tensor(n, s, d))
        PS = [gl.enter_context(nc.psum_tensor(f"ps{i}", [128, 512], F32)) for i in range(8)]
        ident_f = sb(gl, "ident_f", [128, 128])
        ident_b = sb(gl, "ident_b", [128, 128], BF16)
        tri01 = sb(gl, "tri01s", [128, 128])
        tribias = sb(gl, "tribias", [128, 128], BF16)
        mhalf = sb(gl, "mhalf", [128, 8])
        A_m = sb(gl, "A_m", [128, 8]); B_m = sb(gl, "B_m", [128, 8])
        A_f = sb(gl, "A_f", [128, 8]); B_f = sb(gl, "B_f", [128, 8])
        gmix = sb(gl, "gmix", [128, 8])
        crep = sb(gl, "crep", [128, 8, 128], BF16)
        wada_v = wada_d.rearrange("(kc p) n -> p kc n", p=128)

        def ada_section(tr, stk, js, nslots, sink, tg):
            wa = [sb(stk, f"{tg}wa{i}", [128, 8, 512], BF16) for i in range(nslots)]
            bb = [sb(stk, f"{tg}bb{i}", [128, 512]) for i in range(nslots)]
            modc = [sb(stk, f"{tg}modc{i}", [128, 512]) for i in range(nslots)]
            for n_, j in enumerate(js):
                sl = n_ % nslots
                cs = slice(j * 512, (j + 1) * 512)
                tr.dma("pool", wa[sl][:], wada_v[:, :, cs], w=[("wa", sl)])
                tr.dma("sp", bb[sl][:], bada_d[:, cs].partition_broadcast(128), w=[("bb", sl)])
                bank = PS[sl]
                for kc in range(8):
                    tr.op("pe", lambda e, kc=kc, sl=sl, bank=bank: e.matmul(
                        bank[:, :], lhsT=crep[:, kc, :], rhs=wa[sl][:, kc, :], start=(kc == 0), stop=(kc == 7)),
                        r=["crep", ("wa", sl)], w=[("ps", sl)])
                tr.op("dve", lambda e, sl=sl, bank=bank: e.tensor_tensor(
                    out=modc[sl][:], in0=bank[:, :], in1=bb[sl][:], op=ALU.add),
                    r=[("ps", sl), ("bb", sl)], w=[("modc", sl)])
                sink(j, modc[sl], ("modc", sl))

        with ExitStack() as p0:
            tr = Tr()
            ctile = sb(p0, "ctile", [128, 8]); cact = sb(p0, "cact", [128, 8])
            gpm = sb(p0, "gpm", [128, 8]); gpf = sb(p0, "gpf", [128, 8])
            dtmp = sb(p0, "dtmp", [128, 4, 128])
            pp = sb(p0, "pp", [128, 4, 8])
            tr.dma("sp", ident_f[:], ident_d[:, :], w=["ident_f"])
            tr.dma("sp", tri01[:], tri_d[:, :], w=["tri01"])
            tr.dma("sp", ctile[:], ct_d[:, :], w=["ctile"])
            tr.dma("sp", gpm[:], gpm_d[:, :], w=["gpm"])
            tr.dma("sp", gpf[:], gpf_d[:, :], w=["gpf"])
            tr.dma("sp", gmix[:], gmix_d[:, :], w=["gmix"])
            tr.op("dve", lambda e: e.tensor_copy(out=ident_b[:], in_=ident_f[:]), r=["ident_f"], w=["ident_b"])
            tr.op("dve", lambda e: e.tensor_scalar(out=tribias[:], in0=tri01[:], scalar1=-NEGM, scalar2=NEGM,
                                                   op0=ALU.mult, op1=ALU.add), r=["tri01"], w=["tribias"])
            tr.op("pool", lambda e: e.memset(mhalf[:], -0.5), w=["mhalf"])
            tr.op("act", lambda e: e.activation(out=cact[:], in_=ctile[:], func=AF.Silu), r=["ctile"], w=["cact"])
            tr.op("dve", lambda e: e.tensor_copy(out=crep[:], in_=cact[:].unsqueeze(2).to_broadcast([128, 8, 128])),
                  r=["cact"], w=["crep"])
            def sink0(j, mc, mk):
                v, half = j // 2, j % 2
                vi = {0: 0, 1: 1, 3: 2, 4: 3}[v]
                tr.op("dve", lambda e, mc=mc: e.tensor_tensor(
                    out=dtmp[:], in0=mc[:].rearrange("p (a b) -> p a b", a=4),
                    in1=ident_f[:].unsqueeze(1).to_broadcast([128, 4, 128]), op=ALU.mult),
                    r=[mk, "ident_f"], w=["dtmp"])
                tr.op("dve", lambda e, vi=vi, half=half: e.tensor_reduce(
                    out=pp[:, vi, half * 4:(half + 1) * 4], in_=dtmp[:], axis=AX.X, op=ALU.add),
                    r=["dtmp"], w=["pp"])
            ada_section(tr, p0, [0, 1, 2, 3, 6, 7, 8, 9], 2, sink0, "a0")
            tr.op("dve", lambda e: e.scalar_tensor_tensor(out=A_m[:], in0=pp[:, 1, :], scalar=1.0, in1=gpm[:],
                                                          op0=ALU.add, op1=ALU.mult), r=["pp", "gpm"], w=["A_m"])
            tr.op("dve", lambda e: e.tensor_copy(out=B_m[:], in_=pp[:, 0, :]), r=["pp"], w=["B_m"])
            tr.op("dve", lambda e: e.scalar_tensor_tensor(out=A_f[:], in0=pp[:, 3, :], scalar=1.0, in1=gpf[:],
                                                          op0=ALU.add, op1=ALU.mult), r=["pp", "gpf"], w=["A_f"])
            tr.op("dve", lambda e: e.tensor_copy(out=B_f[:], in_=pp[:, 2, :]), r=["pp"], w=["B_f"])
            tr.op("sp", None, r=["crep", "A_m", "B_m", "A_f", "B_f", "gmix", "ident_b", "tribias", "mhalf"])
            tr.emit(nc, p0, "p0")

        with ExitStack() as p1:
            tr = Tr()
            win = sb(p1, "win", [128, 8, 2560], BF16)
            KT = sb(p1, "KT", [80, NH, S], BF16)
            V = sb(p1, "V", [128, 32, NH, 65], BF16)
            kmT = sb(p1, "kmT", [64, NH, 16], BF16)
            ksum = sb(p1, "ksum", [128, 2])
            hT = sb(p1, "hT", [128, 8, 512], BF16)
            QA = sb(p1, "QA", [80, NH, 512], BF16)
            xin = [sb(p1, f"xin{i}", [128, D]) for i in range(1)]
            xn = sb(p1, "xn", [128, D], BF16)
            st = sb(p1, "st", [128, 16])
            Pb = [sb(p1, f"Pb{i}", [128, 512], BF16) for i in range(3)]
            OT = sb(p1, "OT", [65, 512])
            rec = sb(p1, "rec", [128, 4])
            attn = sb(p1, "attn", [128, 4, 512])
            an = sb(p1, "an", [128, D], BF16)
            uh = sb(p1, "uh", [128, 512]); x2 = sb(p1, "x2", [128, 512])
            gvg = sb(p1, "gvg", [128, 512]); y2 = sb(p1, "y2", [128, 512])
            vgn = sb(p1, "vgn", [128, 512], BF16)
            bnst = sb(p1, "bnst", [128, 4, 6]); mv = sb(p1, "mv", [128, 4, 2])
            mixT = sb(p1, "mixT", [128, 8, 128], BF16)
            sgn = sb(p1, "sgn", [128, 4, 512], BF16)
            gnorm = sb(p1, "gnorm", [128, 512])
            bsgu = sb(p1, "bsgu", [128, 4])
            wsT = sb(p1, "wsT", [128, 4, 128], BF16)
            gsb = sb(p1, "gsb", [128, NH, 16]); top8 = sb(p1, "top8", [128, NH, 8])
            mb = sb(p1, "mb", [128, NH, 80], BF16)
            junk = sb(p1, "junk", [128, 512], BF16)

            win_v = win_d.rearrange("(kc p) n -> p kc n", p=128)
            for i in range(5):
                tr.dma("pool", win[:, :, i * 512:(i + 1) * 512], win_v[:, :, i * 512:(i + 1) * 512], w=["win"])
            for h in range(NH):
                tr.dma("pool", KT[64:80, h, :], oneh_d[:, :], w=[("KTo", h)])
            tr.dma("sp", gnorm[:], gnorm_d.partition_broadcast(128), w=["gnorm"])
            tr.dma("sp", bsgu[:], bsgu_d[:, :], w=["bsgu"])
            wsf = y2[:].rearrange("p (g t) -> p g t", g=4)
            tr.dma("sp", wsf, wsgu_d[:, :, :], w=["y2"])
            tr.op("dve", lambda e: e.tensor_tensor(out=wsT[:], in0=wsf,
                                                   in1=tri01[:].unsqueeze(1).to_broadcast([128, 4, 128]), op=ALU.mult),
                  r=["y2"], w=["wsT"])
            tr.op("pool", lambda e: e.memset(V[:, :, :, 64:65], 1.0), w=["Vones"])
            tr.op("pool", lambda e: e.memset(QA[64:80, :, :], 0.0), w=["QAm"])
            tr.op("pool", lambda e: e.memset(gsb[:], -1e30), w=["gsb"])
            tr.op("pool", lambda e: e.memset(mb[:], 0.0), w=["mb"])
            tr.op("pool", lambda e: e.memset(kmT[:], 0.0), w=["kmT"])

            def rstd_ops(src_key, src_ap, dst_ap, dst_key, n):
                tr.op("pool", lambda e: e.tensor_scalar(out=dst_ap, in0=src_ap, scalar1=1.0 / n, scalar2=EPS,
                                                        op0=ALU.mult, op1=ALU.add), r=[src_key], w=[dst_key])
                tr.op("pool", lambda e: e.tensor_tensor(out=dst_ap, in0=dst_ap, in1=mhalf[:, 0:1], op=ALU.pow),
                      r=[dst_key], w=[dst_key])

            MBb = PS[6][:].bitcast(BF16)
            MF = PS[7]
            sctr = [0]
            gctr = [0]

            def gbank():
                i = gctr[0] % 2
                gctr[0] += 1
                return PS[i], ("ps", i)

            def prologue_a(G, tt):
                tok = slice(G * 512 + tt * 128, G * 512 + (tt + 1) * 128)
                xi = 0
                xt = xin[xi]
                tr.dma("sp", xt[:], x_d[tok, :], w=[("xin", xi)])
                tr.op("act", lambda e, xt=xt: e.activation(out=xn[:], in_=xt[:], func=AF.Square,
                                                           accum_out=st[:, 0:1]),
                      r=[("xin", xi)], w=["xn", "st0"])
                rstd_ops("st0", st[:, 0:1], st[:, 1:2], "st1", D)
                tr.op("dve", lambda e, xt=xt: e.tensor_scalar(out=xn[:], in0=xt[:], scalar1=st[:, 1:2], scalar2=None,
                                                              op0=ALU.mult), r=[("xin", xi), "st1"], w=["xn"])
                for c in range(8):
                    tr.op("pe", lambda e, c=c: e.transpose(out=MBb[:, c * 128:(c + 1) * 128],
                                                           in_=xn[:, c * 128:(c + 1) * 128], identity=ident_b[:]),
                          r=["xn"], w=["MB"])
                for c in range(8):
                    tr.op("dve", lambda e, c=c, tt=tt: e.tensor_scalar(
                        out=hT[:, c, tt * 128:(tt + 1) * 128], in0=MBb[:, c * 128:(c + 1) * 128],
                        scalar1=A_m[:, c:c + 1], scalar2=B_m[:, c:c + 1], op0=ALU.mult, op1=ALU.add),
                        r=["MB"], w=[("hT", tt)])

            hTk = [("hT", t) for t in range(4)]

            def inproj_qk(G, oc):
                bank, bk = gbank()
                for kc in range(8):
                    tr.op("pe", lambda e, oc=oc, kc=kc, bank=bank: e.matmul(
                        bank[:, :], lhsT=win[:, kc, oc * 128:(oc + 1) * 128], rhs=hT[:, kc, :],
                        start=(kc == 0), stop=(kc == 7)), r=["win"] + hTk, w=[bk])
                for hh in range(2):
                    h = (oc % 4) * 2 + hh
                    rows = slice(hh * 64, (hh + 1) * 64)
                    if oc < 4:
                        tr.op("dve", lambda e, h=h, rows=rows, bank=bank: e.tensor_copy(
                            out=QA[0:64, h, :], in_=bank[rows, :]), r=[bk], w=[("QA", h)])
                    else:
                        tr.op("dve", lambda e, h=h, rows=rows, bank=bank, G=G: e.tensor_copy(
                            out=KT[0:64, h, G * 512:(G + 1) * 512], in_=bank[rows, :]), r=[bk], w=[("KT", h, G)])
                if oc >= 4:
                    tr.op("dve", lambda e, bank=bank: e.tensor_reduce(
                        out=ksum[:], in_=bank[:, :].rearrange("p (a b) -> p a b", a=2), axis=AX.X, op=ALU.add),
                        r=[bk], w=["ksum"])
                    for hh in range(2):
                        h = (oc % 4) * 2 + hh
                        tr.op("dve", lambda e, h=h, hh=hh, G=G: e.tensor_copy(
                            out=kmT[0:64, h, 2 * G:2 * G + 2], in_=ksum[hh * 64:(hh + 1) * 64, :]),
                            r=["ksum"], w=[("kmT", h)])

            def inproj_vug(G, tt):
                ts_ = slice(tt * 128, (tt + 1) * 128)
                gt = G * 4 + tt
                bank, bk = gbank()
                for kc in range(8):
                    tr.op("pe", lambda e, kc=kc, bank=bank, ts_=ts_: e.matmul(
                        bank[:, :], lhsT=hT[:, kc, ts_], rhs=win[:, kc, 1024:1536],
                        start=(kc == 0), stop=(kc == 7)), r=["win", ("hT", tt)], w=[bk])
                tr.op("dve", lambda e, bank=bank, gt=gt: e.tensor_copy(
                    out=V[:, gt, :, 0:64], in_=bank[:, :].rearrange("p (h d) -> p h d", h=NH)),
                    r=[bk], w=[("V", gt)])
                for which in range(2):
                    dst = uh if which == 0 else gvg
                    dk = "uh" if which == 0 else "gvg"
                    co = 1536 + which * 512
                    bank, bk = gbank()
                    for kc in range(8):
                        tr.op("pe", lambda e, kc=kc, bank=bank, ts_=ts_, co=co: e.matmul(
                            bank[:, :], lhsT=hT[:, kc, ts_], rhs=win[:, kc, co:co + 512],
                            start=(kc == 0), stop=(kc == 7)), r=["win", ("hT", tt)], w=[bk])
                    tr.op("act", lambda e, bank=bank, dst=dst: e.activation(out=dst[:], in_=bank[:, :], func=AF.Identity,
                                                                            scale=0.5), r=[bk], w=[dk])
                    tr.op("act", lambda e, bank=bank: e.activation(out=x2[:], in_=bank[:, :], func=AF.Square),
                          r=[bk], w=["x2"])
                    tr.op("pool", lambda e: e.tensor_scalar(out=x2[:], in0=x2[:], scalar1=0.044715, scalar2=1.0,
                                                            op0=ALU.mult, op1=ALU.add), r=["x2"], w=["x2"])
                    tr.op("pool", lambda e, dst=dst: e.tensor_tensor(out=x2[:], in0=x2[:], in1=dst[:], op=ALU.mult),
                          r=["x2", dk], w=["x2"])
                    tr.op("act", lambda e: e.activation(out=x2[:], in_=x2[:], func=AF.Tanh, scale=2.0 * GELU_C),
                          r=["x2"], w=["x2"])
                    tr.op("dve", lambda e, dst=dst: e.scalar_tensor_tensor(
                        out=dst[:], in0=x2[:], scalar=1.0, in1=dst[:], op0=ALU.add, op1=ALU.mult),
                        r=["x2", dk], w=[dk])
                for g in range(4):
                    tr.op("dve", lambda e, g=g: e.bn_stats(out=bnst[:, g, :], in_=gvg[:, g * 128:(g + 1) * 128]),
                          r=["gvg"], w=["bnst"])
                for g in range(4):
                    tr.op("dve", lambda e, g=g: e.bn_aggr(out=mv[:, g, :], in_=bnst[:, g, :]), r=["bnst"], w=["mv"])
                tr.op("pool", lambda e: e.tensor_scalar(out=st[:, 4:8], in0=mv[:, :, 1], scalar1=1.0, scalar2=EPS,
                                                        op0=ALU.mult, op1=ALU.add), r=["mv"], w=["st4"])
                tr.op("pool", lambda e: e.tensor_tensor(out=st[:, 4:8], in0=st[:, 4:8], in1=mhalf[:, 0:4], op=ALU.pow),
                      r=["st4"], w=["st4"])
                for g in range(4):
                    tr.op("dve", lambda e, g=g: e.tensor_scalar(
                        out=gvg[:, g * 128:(g + 1) * 128], in0=gvg[:, g * 128:(g + 1) * 128],
                        scalar1=mv[:, g, 0:1], scalar2=st[:, 4 + g:5 + g], op0=ALU.subtract, op1=ALU.mult),
                        r=["gvg", "mv", "st4"], w=["gvg"])
                tr.op("dve", lambda e: e.tensor_tensor(out=vgn[:], in0=gvg[:], in1=gnorm[:], op=ALU.mult),
                      r=["gvg", "gnorm"], w=["vgn"])
                bank, bk = gbank()
                for g in range(4):
                    tr.op("pe", lambda e, g=g, bank=bank: e.matmul(
                        bank[:, g * 128:(g + 1) * 128], lhsT=wsT[:, g, :], rhs=vgn[:, g * 128:(g + 1) * 128],
                        start=True, stop=True), r=["wsT", "vgn"], w=[bk])
                for g in range(4):
                    tr.op("dve", lambda e, g=g, bank=bank: e.scalar_tensor_tensor(
                        out=y2[:, g * 128:(g + 1) * 128], in0=bank[:, g * 128:(g + 1) * 128],
                        scalar=bsgu[:, g:g + 1], in1=uh[:, g * 128:(g + 1) * 128], op0=ALU.add, op1=ALU.mult),
                        r=[bk, "uh", "bsgu"], w=["y2"])
                tr.op("act", lambda e: e.activation(out=junk[:], in_=y2[:], func=AF.Square,
                                                    accum_out=st[:, 8:9]), r=["y2"], w=["junk", "st8"])
                rstd_ops("st8", st[:, 8:9], st[:, 9:10], "st9", 512)
                tr.op("dve", lambda e, tt=tt: e.tensor_scalar(out=sgn[:, tt, :], in0=y2[:], scalar1=st[:, 9:10],
                                                               scalar2=None, op0=ALU.mult),
                      r=["y2", "st9"], w=[("sgn", tt)])

            def gate(G):
                if G < 2:
                    return
                for tt in range(4):
                    j = 2 * G + tt // 2
                    ts_ = slice(tt * 128, (tt + 1) * 128)
                    for h in range(NH):
                        tr.op("pe", lambda e, h=h, ts_=ts_: e.matmul(
                            MF[:, 384 + h * 16:384 + (h + 1) * 16], lhsT=QA[0:64, h, ts_], rhs=kmT[:, h, :],
                            start=True, stop=True), r=[("QA", h), ("kmT", h)], w=["MFg"])
                    tr.op("dve", lambda e, j=j: e.tensor_copy(
                        out=gsb[:, :, 0:j], in_=MF[:, 384:512].rearrange("p (h n) -> p h n", h=NH)[:, :, 0:j]),
                        r=["MFg"], w=["gsb"])
                    for h in range(NH):
                        tr.op("dve", lambda e, h=h: e.max(out=top8[:, h, :], in_=gsb[:, h, :]), r=["gsb"], w=["top8"])
                    for h in range(NH):
                        tr.op("dve", lambda e, h=h, j=j: e.tensor_scalar(
                            out=mb[:, h, 64:64 + j], in0=gsb[:, h, 0:j], scalar1=top8[:, h, 2:3], scalar2=NEGM,
                            op0=ALU.is_lt, op1=ALU.mult), r=["gsb", "top8"], w=["mb"])
                    for h in range(NH):
                        tr.op("pe", lambda e, h=h: e.transpose(out=MBb[0:80, h * 128:(h + 1) * 128],
                                                               in_=mb[:, h, :], identity=ident_b[:]),
                              r=["mb"], w=["MB"])
                    tr.op("dve", lambda e, ts_=ts_: e.tensor_copy(
                        out=QA[64:80, :, ts_], in_=MBb[64:80, :].rearrange("p (h t) -> p h t", h=NH)),
                        r=["MB"], w=["QAm"])

            def attn_items(G):
                return [(G, h, kt) for h in range(NH) for kt in range(4 * G + 4)]

            def qk(G, h, kt):
                c0 = 0 if kt <= 4 * G else (kt - 4 * G) * 128
                si = sctr[0] % 2
                sctr[0] += 1
                Sb = PS[2 + si]
                sk = ("ps", 2 + si)
                tri = kt >= 4 * G
                tr.op("pe", lambda e, h=h, kt=kt, c0=c0, Sb=Sb, tri=tri: e.matmul(
                    Sb[:, c0:512], lhsT=KT[0:80, h, kt * 128:(kt + 1) * 128], rhs=QA[0:80, h, c0:512],
                    start=True, stop=(not tri)),
                    r=[("KT", h, kt // 4), ("KTo", h), ("QA", h), "QAm"], w=[sk])
                if tri:
                    ct0 = (kt - 4 * G) * 128
                    tr.op("pe", lambda e, Sb=Sb, ct0=ct0: e.matmul(
                        Sb[:, ct0:ct0 + 128], lhsT=ident_b[:], rhs=tribias[:], start=False, stop=True),
                        r=[], w=[sk])
                return (Sb, sk, c0)

            def exp_pv(G, h, kt, sinfo, idx):
                Sb, sk, c0 = sinfo
                nkt = 4 * G + 4
                pi = idx % 3
                Ob = PS[4 + h % 2]
                ok = ("ps", 4 + h % 2)
                tr.op("act", lambda e, Sb=Sb, c0=c0, pi=pi: e.activation(
                    out=Pb[pi][:, c0:512], in_=Sb[:, c0:512], func=AF.Exp, scale=0.125),
                    r=[sk], w=[("Pb", pi)])

                def pv():
                    tr.op("pe", lambda e, h=h, kt=kt, c0=c0, pi=pi, Ob=Ob, nkt=nkt: e.matmul(
                        Ob[0:65, c0:512], lhsT=V[:, kt, h, :], rhs=Pb[pi][:, c0:512],
                        start=(kt == 0), stop=(kt == nkt - 1)),
                        r=[("V", kt), "Vones", ("Pb", pi)], w=[ok])
                    if kt == nkt - 1:
                        head_finish(G, h, Ob, ok)
                return pv

            def head_finish(G, h, Ob, ok):
                tr.op("dve", lambda e, Ob=Ob: e.tensor_copy(out=OT[:], in_=Ob[0:65, :]), r=[ok], w=["OT"])
                for tt in range(4):
                    tr.op("pe", lambda e, tt=tt: e.transpose(out=MF[:, tt * 65:(tt + 1) * 65],
                                                             in_=OT[:, tt * 128:(tt + 1) * 128],
                                                             identity=ident_f[0:65, 0:65]),
                          r=["OT"], w=["MFo"])
                MFo = MF[:, 0:260].rearrange("p (t d) -> p t d", t=4)
                tr.op("dve", lambda e, MFo=MFo: e.reciprocal(out=rec[:], in_=MFo[:, :, 64]), r=["MFo"], w=["rec"])
                tr.op("dve", lambda e, MFo=MFo, h=h: e.tensor_tensor(
                    out=attn[:, :, h * 64:(h + 1) * 64], in0=MFo[:, :, 0:64],
                    in1=rec[:].unsqueeze(2).to_broadcast([128, 4, 64]), op=ALU.mult),
                    r=["MFo", "rec"], w=["attn"])

            def epilogue(G, tt):
                tr.op("act", lambda e, tt=tt: e.activation(out=junk[:], in_=attn[:, tt, :], func=AF.Square,
                                                           accum_out=st[:, 10:11]), r=["attn"], w=["junk", "st10"])
                rstd_ops("st10", st[:, 10:11], st[:, 11:12], "st11", 512)
                tr.op("dve", lambda e, tt=tt: e.tensor_scalar(out=an[:, 0:512], in0=attn[:, tt, :],
                                                              scalar1=st[:, 11:12], scalar2=None, op0=ALU.mult),
                      r=["attn", "st11"], w=["an"])
                tr.op("dve", lambda e, tt=tt: e.tensor_copy(out=an[:, 512:1024], in_=sgn[:, tt, :]),
                      r=[("sgn", tt)], w=["an"])
                for c in range(8):
                    tr.op("pe", lambda e, c=c: e.transpose(out=MBb[:, c * 128:(c + 1) * 128],
                                                           in_=an[:, c * 128:(c + 1) * 128], identity=ident_b[:]),
                          r=["an"], w=["MB"])
                tr.op("dve", lambda e, tt=tt: e.tensor_tensor(
                    out=mixT[:, :, :], in0=MBb[:, :].rearrange("p (c t) -> p c t", c=8),
                    in1=gmix[:].unsqueeze(2).to_broadcast([128, 8, 128]), op=ALU.mult),
                    r=["MB"], w=["mixT"])
                tr.dma("sp", mix_d[G * 4 + tt].rearrange("p (c t) -> p c t", c=8), mixT[:], r=["mixT"], w=["mixd"])

            def attention(G, side):
                items = attn_items(G)
                n_it = len(items)
                sin = {}
                for i in range(min(LOOK, n_it)):
                    sin[i] = qk(*items[i])
                step = max(1, n_it // (len(side) + 1)) if side else n_it + 1
                for i in range(n_it):
                    if LOOK == 0:
                        sin[i] = qk(*items[i])
                    pv = exp_pv(*items[i], sin.pop(i), i)
                    if LOOK > 0 and i + LOOK < n_it:
                        sin[i + LOOK] = qk(*items[i + LOOK])
                    pv()
                    if side and (i + 1) % step == 0:
                        side.pop(0)()
                while side:
                    side.pop(0)()

            if INTERLEAVE:
                for tt in range(4):
                    prologue_a(0, tt)
                for oc in range(8):
                    inproj_qk(0, oc)
                for tt in range(4):
                    inproj_vug(0, tt)
                gate(0)
                for G in range(8):
                    side = []
                    if G + 1 < 8:
                        side = [(lambda t=t, G=G: prologue_a(G + 1, t)) for t in range(4)]
                    attention(G, side)
                    for tt in range(4):
                        epilogue(G, tt)
                        if G + 1 < 8:
                            inproj_qk(G + 1, tt)
                            inproj_qk(G + 1, tt + 4)
                            inproj_vug(G + 1, tt)
                    if G + 1 < 8:
                        gate(G + 1)
            else:
                for G in range(8):
                    for tt in range(4):
                        prologue_a(G, tt)
                    for oc in range(8):
                        inproj_qk(G, oc)
                    for tt in range(4):
                        inproj_vug(G, tt)
                    gate(G)
                    attention(G, [])
                    for tt in range(4):
                        epilogue(G, tt)
            tr.op("sp", None, r=["mixd"])
            tr.emit(nc, p1, "p1")

        with ExitStack() as p2:
            tr = Tr()
            wout = sb(p2, "wout", [128, 8, D], BF16)
            wgu = sb(p2, "wgu", [128, 8, 2 * DFF], BF16)
            wdn = sb(p2, "wdn", [128, NFC, D], BF16)
            actT = sb(p2, "actT", [128, NFC, 256], BF16)
            hb = sb(p2, "hb", [128, 8, 256], BF16)
            xt2 = [sb(p2, f"xt2{i}", [128, D]) for i in range(2)]
            xn2 = sb(p2, "xn2", [128, D], BF16)
            junk2 = sb(p2, "junk2", [128, D], BF16)
            ytmp = sb(p2, "ytmp", [128, 512])
            sg = sb(p2, "sg", [128, 256])
            st2 = sb(p2, "st2", [128, 16])
            GM = sb(p2, "GM", [128, D]); GF = sb(p2, "GF", [128, D])
            tr.dma("sp", GM[:], gpostm_d.partition_broadcast(128), w=["GM"])
            tr.dma("sp", GF[:], gpostf_d.partition_broadcast(128), w=["GF"])

            def sink2(j, mc, mk):
                G_ = GM if j < 6 else GF
                gk = "GM" if j < 6 else "GF"
                hs = slice((j % 2) * 512, (j % 2 + 1) * 512)
                tr.op("dve", lambda e, G_=G_, hs=hs, mc=mc: e.tensor_tensor(
                    out=G_[:, hs], in0=G_[:, hs], in1=mc[:], op=ALU.mult), r=[mk, gk], w=[gk])
            ada_section(tr, p2, [4, 5, 10, 11], 1, sink2, "a2")
            MBb = PS[6][:].bitcast(BF16)

            def rstd2(src_key, src_ap, dst_ap, dst_key, n):
                tr.op("pool", lambda e: e.tensor_scalar(out=dst_ap, in0=src_ap, scalar1=1.0 / n, scalar2=EPS,
                                                        op0=ALU.mult, op1=ALU.add), r=[src_key], w=[dst_key])
                tr.op("pool", lambda e: e.tensor_tensor(out=dst_ap, in0=dst_ap, in1=mhalf[:, 0:1], op=ALU.pow),
                      r=[dst_key], w=[dst_key])

            wout_v = wout_d.rearrange("(kc p) n -> p kc n", p=128)
            wgu_v = wgu_d.rearrange("(kc p) n -> p kc n", p=128)
            wdn_v = wdn_d.rearrange("(fc p) n -> p fc n", p=128)
            tr.dma("pool", wout[:], wout_v, w=["wout"])
            for fc in range(NFC):
                for pt in range(2):
                    co = pt * DFF + fc * 128
                    tr.dma("pool", wgu[:, :, co:co + 128], wgu_v[:, :, co:co + 128], w=[("wgu", fc)])
            for i in range(2):
                tr.dma("pool", wdn[:, i * 11:(i + 1) * 11, :], wdn_v[:, i * 11:(i + 1) * 11, :], w=["wdn"])
            bctr = [0]

            def pbank():
                i = bctr[0] % 6
                bctr[0] += 1
                return PS[i], ("ps", i)

            for G in range(16):
                for tt in range(2):
                    tr.dma("sp", hb[:, :, tt * 128:(tt + 1) * 128],
                           mix_d[G * 2 + tt].rearrange("p (c t) -> p c t", c=8), w=[("hb", tt)])
                for tt in range(2):
                    ts_ = slice(tt * 128, (tt + 1) * 128)
                    tok = slice(G * 256 + tt * 128, G * 256 + (tt + 1) * 128)
                    xt = xt2[tt]
                    xk = ("xt", tt)
                    tr.dma("sp", xt[:], x_d[tok, :], w=[xk])
                    for half in range(2):
                        hs = slice(half * 512, (half + 1) * 512)
                        bank, bk = pbank()
                        for kc in range(8):
                            tr.op("pe", lambda e, kc=kc, bank=bank, ts_=ts_, hs=hs: e.matmul(
                                bank[:, :], lhsT=hb[:, kc, ts_], rhs=wout[:, kc, hs], start=(kc == 0), stop=(kc == 7)),
                                r=["wout", ("hb", tt)], w=[bk])
                        tr.op("act", lambda e, bank=bank, half=half: e.activation(
                            out=junk2[:, 0:512], in_=bank[:, :], func=AF.Square, accum_out=st2[:, half:half + 1]),
                            r=[bk], w=["junk2", ("ssq", half)])
                        if half == 0:
                            b0, bk0 = bank, bk
                        else:
                            b1, bk1 = bank, bk
                    tr.op("pool", lambda e: e.tensor_tensor(out=st2[:, 2:3], in0=st2[:, 0:1], in1=st2[:, 1:2], op=ALU.add),
                          r=[("ssq", 0), ("ssq", 1)], w=["s2"])
                    rstd2("s2", st2[:, 2:3], st2[:, 3:4], "s3", D)
                    for half, (bank, bk) in enumerate(((b0, bk0), (b1, bk1))):
                        hs = slice(half * 512, (half + 1) * 512)
                        tr.op("dve", lambda e, bank=bank, hs=hs: e.scalar_tensor_tensor(
                            out=ytmp[:], in0=bank[:, :], scalar=st2[:, 3:4], in1=GM[:, hs], op0=ALU.mult, op1=ALU.mult),
                            r=[bk, "s3", "GM"], w=["ytmp"])
                        tr.op("dve", lambda e, xt=xt, hs=hs: e.tensor_tensor(out=xt[:, hs], in0=xt[:, hs], in1=ytmp[:],
                                                                             op=ALU.add), r=["ytmp", xk], w=[xk])
                    tr.op("act", lambda e, xt=xt: e.activation(out=junk2[:], in_=xt[:], func=AF.Square,
                                                               accum_out=st2[:, 4:5]), r=[xk], w=["junk2", "s4"])
                    rstd2("s4", st2[:, 4:5], st2[:, 5:6], "s5", D)
                    tr.op("dve", lambda e, xt=xt: e.tensor_scalar(out=xn2[:], in0=xt[:], scalar1=st2[:, 5:6], scalar2=None,
                                                                  op0=ALU.mult), r=[xk, "s5"], w=["xn2"])
                    for c in range(8):
                        tr.op("pe", lambda e, c=c: e.transpose(out=MBb[:, c * 128:(c + 1) * 128],
                                                               in_=xn2[:, c * 128:(c + 1) * 128], identity=ident_b[:]),
                              r=["xn2"], w=["MB"])
                    for c in range(8):
                        tr.op("dve", lambda e, c=c, ts_=ts_: e.tensor_scalar(
                            out=hb[:, c, ts_], in0=MBb[:, c * 128:(c + 1) * 128],
                            scalar1=A_f[:, c:c + 1], scalar2=B_f[:, c:c + 1], op0=ALU.mult, op1=ALU.add),
                            r=["MB"], w=[("hb", tt)])
                for fc in range(NFC):
                    gb, gk = pbank()
                    ub, uk = pbank()
                    for pt, bank, bk in ((0, gb, gk), (1, ub, uk)):
                        co = pt * DFF + fc * 128
                        for kc in range(8):
                            tr.op("pe", lambda e, kc=kc, bank=bank, co=co: e.matmul(
                                bank[:, 0:256], lhsT=wgu[:, kc, co:co + 128], rhs=hb[:, kc, :],
                                start=(kc == 0), stop=(kc == 7)),
                                r=[("wgu", fc), ("hb", 0), ("hb", 1)], w=[bk])
                    tr.op("act", lambda e, gb=gb: e.activation(out=sg[:], in_=gb[:, 0:256], func=AF.Silu),
                          r=[gk], w=["sg"])
                    tr.op("dve", lambda e, ub=ub, fc=fc: e.tensor_tensor(out=actT[:, fc, :], in0=sg[:], in1=ub[:, 0:256],
                                                                         op=ALU.mult), r=["sg", uk], w=[("actT", fc)])
                for tt in range(2):
                    ts_ = slice(tt * 128, (tt + 1) * 128)
                    tok = slice(G * 256 + tt * 128, G * 256 + (tt + 1) * 128)
                    xt = xt2[tt]
                    xk = ("xt", tt)
                    banks = []
                    for half in range(2):
                        hs = slice(half * 512, (half + 1) * 512)
                        bank, bk = pbank()
                        banks.append((bank, bk))
                        for fc in range(NFC):
                            tr.op("pe", lambda e, fc=fc, bank=bank, ts_=ts_, hs=hs: e.matmul(
                                bank[:, :], lhsT=actT[:, fc, ts_], rhs=wdn[:, fc, hs],
                                start=(fc == 0), stop=(fc == NFC - 1)),
                                r=["wdn", ("actT", fc)], w=[bk])
                        tr.op("act", lambda e, bank=bank, half=half: e.activation(
                            out=junk2[:, 0:512], in_=bank[:, :], func=AF.Square, accum_out=st2[:, 8 + half:9 + half]),
                            r=[bk], w=["junk2", ("ssq2", half)])
                    tr.op("pool", lambda e: e.tensor_tensor(out=st2[:, 10:11], in0=st2[:, 8:9], in1=st2[:, 9:10], op=ALU.add),
                          r=[("ssq2", 0), ("ssq2", 1)], w=["s10"])
                    rstd2("s10", st2[:, 10:11], st2[:, 11:12], "s11", D)
                    for half, (bank, bk) in enumerate(banks):
                        hs = slice(half * 512, (half + 1) * 512)
                        tr.op("dve", lambda e, bank=bank, hs=hs: e.scalar_tensor_tensor(
                            out=ytmp[:], in0=bank[:, :], scalar=st2[:, 11:12], in1=GF[:, hs], op0=ALU.mult, op1=ALU.mult),
                            r=[bk, "s11", "GF"], w=["ytmp"])
                        tr.op("dve", lambda e, xt=xt, hs=hs: e.tensor_tensor(out=xt[:, hs], in0=xt[:, hs], in1=ytmp[:],
                                                                             op=ALU.add), r=["ytmp", xk], w=[xk])
                    tr.dma("sp", out_d[tok, :], xt[:], r=[xk], w=["outd"])
            tr.op("sp", None, r=["outd"])
            tr.emit(nc, p2, "p2")
    return nc


def _consts():
    ident = np.eye(128, dtype=np.float32)
    p = np.arange(128)
    tri01 = (p[:, None] <= p[None, :]).astype(np.float32)
    onehot = (np.arange(S)[None, :] // 256 == np.arange(16)[:, None]).astype(np.float32)
    return ident, tri01, onehot


def kernel(x, c, w_ada, b_ada, g_pre_mix, g_post_mix, w_in, g_sgu_norm, w_sgu, b_sgu,
           g_attn_out, g_sgu_out, w_out, g_pre_ffn, g_post_ffn, w_gate_up, w_down):
    f = lambda a: np.ascontiguousarray(np.asarray(a, dtype=np.float32))
    x = f(x); c = f(c)
    ident, tri01, onehot = _consts()
    pp = lambda v: f(np.asarray(v).reshape(-1, 128).T)
    shared = {
        "w_ada": f(w_ada[0]), "b_ada": f(b_ada[0]).reshape(1, -1),
        "gpm_t": pp(g_pre_mix[0]), "gpf_t": pp(g_pre_ffn[0]),
        "gmix_t": f(np.concatenate([pp(g_attn_out[0]), pp(g_sgu_out[0])], axis=1)),
        "g_post_mix": f(g_post_mix[0]).reshape(1, -1), "g_post_ffn": f(g_post_ffn[0]).reshape(1, -1),
        "g_sgu_norm": f(g_sgu_norm[0]).reshape(1, -1),
        "bsgu_t": f(np.asarray(b_sgu[0]).T),
        "wsgu_t": f(np.transpose(np.asarray(w_sgu[0]), (2, 0, 1))),
        "w_in": f(w_in[0]), "w_out": f(w_out[0]), "w_gate_up": f(w_gate_up[0]), "w_down": f(w_down[0]),
        "ident": ident, "tri01": tri01, "onehot": onehot,
    }
    in_maps = []
    for b in range(8):
        m = dict(shared)
        m["x"] = x[b]
        m["ct"] = pp(c[b])
        in_maps.append(m)
    nc = build_program()
    res = run_bass_kernel_spmd(nc, in_maps, core_ids=list(range(8)))
    return np.stack([np.asarray(r["out"], dtype=np.float32) for r in res.results], axis=0)
```

```python
from contextlib import ExitStack
import numpy as np
import concourse.bass as bass
import concourse.mybir as mybir
from concourse.bass_utils import run_bass_kernel_spmd

F32, BF16 = mybir.dt.float32, mybir.dt.bfloat16
AF = mybir.ActivationFunctionType
ALU = mybir.AluOpType
AX = mybir.AxisListType

S, D, NH, HD = 4096, 1024, 8, 64
DFF = 2816
NFC = DFF // 128
EPS = 1e-6
NEGM = -30000.0
GELU_C = 0.7978845608028654
STRICT = False
LOOK = 1
INTERLEAVE = True


class Op:
    __slots__ = ("eng", "fn", "deps", "signal", "cnt", "dma", "sem", "target")


class Tr:
    ENG = ("pe", "act", "dve", "pool", "sp")

    def __init__(self):
        self.streams = {e: [] for e in self.ENG}
        self.last_w = {}
        self.readers = {}

    def _add(self, eng, fn, r, w, dma):
        op = Op()
        op.eng, op.fn, op.dma, op.signal, op.cnt, op.sem, op.target = eng, fn, dma, False, 0, None, 0
        deps = set()
        for k in r:
            p = self.last_w.get(k)
            if p is not None and (STRICT or not (eng == "pe" and p.eng == "pe" and not p.dma)):
                deps.add(p)
        for k in w:
            p = self.last_w.get(k)
            if p is not None and (STRICT or p.eng != eng or p.dma or dma):
                deps.add(p)
            lastr = {}
            for q in self.readers.get(k, ()):
                if STRICT or q.eng != eng or q.dma or dma:
                    if q.dma or STRICT:
                        deps.add(q)
                    else:
                        lastr[q.eng] = q
            deps.update(lastr.values())
        op.deps = deps
        for d in deps:
            d.signal = True
        for k in r:
            self.readers.setdefault(k, []).append(op)
        for k in w:
            self.last_w[k] = op
            self.readers[k] = []
        self.streams[eng].append(op)
        return op

    def op(self, eng, fn, r=(), w=()):
        return self._add(eng, fn, r, w, False)

    def dma(self, q, out, in_, r=(), w=()):
        return self._add(q, lambda e, o=out, i=in_: e.dma_start(out=o, in_=i), r, w, True)

    def emit(self, nc, stack, tag, ndma_sems=12):
        engsem = {e: stack.enter_context(nc.semaphore(f"{tag}_es_{e}")) for e in ("pe", "act", "dve", "pool")}
        dsems = {q: [stack.enter_context(nc.semaphore(f"{tag}_ds_{q}{i}")) for i in range(ndma_sems)]
                 for q in ("sp", "pool", "act") if any(o.dma for o in self.streams[q])}
        for e in self.ENG:
            c = 0
            nd = 0
            for o in self.streams[e]:
                if o.dma:
                    o.sem = dsems[e][nd % ndma_sems]
                    o.target = 16 * (nd // ndma_sems + 1)
                    nd += 1
                elif o.signal:
                    c += 1
                    o.cnt = c
        block = stack.enter_context(nc.Block())

        def run(ename, eng):
            waited = {}
            for o in self.streams[ename]:
                need = {}
                for d in o.deps:
                    if d.dma:
                        s_, v_ = d.sem, d.target
                    else:
                        s_, v_ = engsem[d.eng], d.cnt
                    if need.get(id(s_), (None, 0))[1] < v_:
                        need[id(s_)] = (s_, v_)
                if o.dma and o.target > 16:
                    if need.get(id(o.sem), (None, 0))[1] < o.target - 16:
                        need[id(o.sem)] = (o.sem, o.target - 16)
                for sid, (s_, v_) in need.items():
                    if waited.get(sid, 0) < v_:
                        eng.wait_ge(s_, v_)
                        waited[sid] = v_
                if o.fn is None:
                    continue
                ins = o.fn(eng)
                if o.dma:
                    ins.then_inc(o.sem, 16)
                elif o.signal:
                    ins.then_inc(engsem[ename], 1)

        @block.tensor
        def _(e):
            run("pe", e)

        @block.scalar
        def _(e):
            run("act", e)

        @block.vector
        def _(e):
            run("dve", e)

        @block.gpsimd
        def _(e):
            run("pool", e)

        @block.sync
        def _(e):
            run("sp", e)


def build_program():
    nc = bass.Bass("TRN2", target_bir_lowering=False)
    dt = lambda n, s, d=F32, k="ExternalInput": nc.dram_tensor(n, s, d, kind=k).ap()
    x_d = dt("x", [S, D])
    ct_d = dt("ct", [128, 8])
    wada_d = dt("w_ada", [D, 6 * D])
    bada_d = dt("b_ada", [1, 6 * D])
    gpm_d = dt("gpm_t", [128, 8])
    gpf_d = dt("gpf_t", [128, 8])
    gmix_d = dt("gmix_t", [128, 8])
    gpostm_d = dt("g_post_mix", [1, D])
    gpostf_d = dt("g_post_ffn", [1, D])
    gnorm_d = dt("g_sgu_norm", [1, 512])
    bsgu_d = dt("bsgu_t", [128, 4])
    wsgu_d = dt("wsgu_t", [128, 4, 128])
    win_d = dt("w_in", [D, 2560])
    wout_d = dt("w_out", [D, D])
    wgu_d = dt("w_gate_up", [D, 2 * DFF])
    wdn_d = dt("w_down", [DFF, D])
    ident_d = dt("ident", [128, 128])
    tri_d = dt("tri01", [128, 128])
    oneh_d = dt("onehot", [16, S])
    out_d = dt("out", [S, D], F32, "ExternalOutput")
    mix_d = nc.dram_tensor("mixscr", [32, 128, 8 * 128], BF16).ap()

    with ExitStack() as gl:
        sb = lambda st, n, s, d=F32: st.enter_context(nc.sbuf_tensor(n, s, d))
        PS = [gl.enter_context(nc.psum_tensor(f"ps{i}", [128, 512], F32)) for i in range(8)]
        ident_f = sb(gl, "ident_f", [128, 128])
        ident_b = sb(gl, "ident_b", [128, 128], BF16)
        tri01 = sb(gl, "tri01s", [128, 128])
        tribias = sb(gl, "tribias", [128, 128], BF16)
        mhalf = sb(gl, "mhalf", [128, 8])
        A_m = sb(gl, "A_m", [128, 8]); B_m = sb(gl, "B_m", [128, 8])
        A_f = sb(gl, "A_f", [128, 8]); B_f = sb(gl, "B_f", [128, 8])
        gmix = sb(gl, "gmix", [128, 8])
        crep = sb(gl, "crep", [128, 8, 128], BF16)
        wada_v = wada_d.rearrange("(kc p) n -> p kc n", p=128)

        def ada_section(tr, stk, js, nslots, sink, tg):
            wa = [sb(stk, f"{tg}wa{i}", [128, 8, 512], BF16) for i in range(nslots)]
            bb = [sb(stk, f"{tg}bb{i}", [128, 512]) for i in range(nslots)]
            modc = [sb(stk, f"{tg}modc{i}", [128, 512]) for i in range(nslots)]
            for n_, j in enumerate(js):
                sl = n_ % nslots
                cs = slice(j * 512, (j + 1) * 512)
                tr.dma("pool", wa[sl][:], wada_v[:, :, cs], w=[("wa", sl)])
                tr.dma("sp", bb[sl][:], bada_d[:, cs].partition_broadcast(128), w=[("bb", sl)])
                bank = PS[sl]
                for kc in range(8):
                    tr.op("pe", lambda e, kc=kc, sl=sl, bank=bank: e.matmul(
                        bank[:, :], lhsT=crep[:, kc, :], rhs=wa[sl][:, kc, :], start=(kc == 0), stop=(kc == 7)),
                        r=["crep", ("wa", sl)], w=[("ps", sl)])
                tr.op("dve", lambda e, sl=sl, bank=bank: e.tensor_tensor(
                    out=modc[sl][:], in0=bank[:, :], in1=bb[sl][:], op=ALU.add),
                    r=[("ps", sl), ("bb", sl)], w=[("modc", sl)])
                sink(j, modc[sl], ("modc", sl))

        with ExitStack() as p0:
            tr = Tr()
            ctile = sb(p0, "ctile", [128, 8]); cact = sb(p0, "cact", [128, 8])
            gpm = sb(p0, "gpm", [128, 8]); gpf = sb(p0, "gpf", [128, 8])
            dtmp = sb(p0, "dtmp", [128, 4, 128])
            pp = sb(p0, "pp", [128, 4, 8])
            tr.dma("sp", ident_f[:], ident_d[:, :], w=["ident_f"])
            tr.dma("sp", tri01[:], tri_d[:, :], w=["tri01"])
            tr.dma("sp", ctile[:], ct_d[:, :], w=["ctile"])
            tr.dma("sp", gpm[:], gpm_d[:, :], w=["gpm"])
            tr.dma("sp", gpf[:], gpf_d[:, :], w=["gpf"])
            tr.dma("sp", gmix[:], gmix_d[:, :], w=["gmix"])
            tr.op("dve", lambda e: e.tensor_copy(out=ident_b[:], in_=ident_f[:]), r=["ident_f"], w=["ident_b"])
            tr.op("dve", lambda e: e.tensor_scalar(out=tribias[:], in0=tri01[:], scalar1=-NEGM, scalar2=NEGM,
                                                   op0=ALU.mult, op1=ALU.add), r=["tri01"], w=["tribias"])
            tr.op("pool", lambda e: e.memset(mhalf[:], -0.5), w=["mhalf"])
            tr.op("act", lambda e: e.activation(out=cact[:], in_=ctile[:], func=AF.Silu), r=["ctile"], w=["cact"])
            tr.op("dve", lambda e: e.tensor_copy(out=crep[:], in_=cact[:].unsqueeze(2).to_broadcast([128, 8, 128])),
                  r=["cact"], w=["crep"])
            def sink0(j, mc, mk):
                v, half = j // 2, j % 2
                vi = {0: 0, 1: 1, 3: 2, 4: 3}[v]
                tr.op("dve", lambda e, mc=mc: e.tensor_tensor(
                    out=dtmp[:], in0=mc[:].rearrange("p (a b) -> p a b", a=4),
                    in1=ident_f[:].unsqueeze(1).to_broadcast([128, 4, 128]), op=ALU.mult),
                    r=[mk, "ident_f"], w=["dtmp"])
                tr.op("dve", lambda e, vi=vi, half=half: e.tensor_reduce(
                    out=pp[:, vi, half * 4:(half + 1) * 4], in_=dtmp[:], axis=AX.X, op=ALU.add),
                    r=["dtmp"], w=["pp"])
            ada_section(tr, p0, [0, 1, 2, 3, 6, 7, 8, 9], 2, sink0, "a0")
            tr.op("dve", lambda e: e.scalar_tensor_tensor(out=A_m[:], in0=pp[:, 1, :], scalar=1.0, in1=gpm[:],
                                                          op0=ALU.add, op1=ALU.mult), r=["pp", "gpm"], w=["A_m"])
            tr.op("dve", lambda e: e.tensor_copy(out=B_m[:], in_=pp[:, 0, :]), r=["pp"], w=["B_m"])
            tr.op("dve", lambda e: e.scalar_tensor_tensor(out=A_f[:], in0=pp[:, 3, :], scalar=1.0, in1=gpf[:],
                                                          op0=ALU.add, op1=ALU.mult), r=["pp", "gpf"], w=["A_f"])
            tr.op("dve", lambda e: e.tensor_copy(out=B_f[:], in_=pp[:, 2, :]), r=["pp"], w=["B_f"])
            tr.op("sp", None, r=["crep", "A_m", "B_m", "A_f", "B_f", "gmix", "ident_b", "tribias", "mhalf"])
            tr.emit(nc, p0, "p0")

        with ExitStack() as p1:
            tr = Tr()
            win = sb(p1, "win", [128, 8, 2560], BF16)
            KT = sb(p1, "KT", [80, NH, S], BF16)
            V = sb(p1, "V", [128, 32, NH, 65], BF16)
            kmT = sb(p1, "kmT", [64, NH, 16], BF16)
            ksum = sb(p1, "ksum", [128, 2])
            hT = sb(p1, "hT", [128, 8, 512], BF16)
            QA = sb(p1, "QA", [80, NH, 512], BF16)
            xin = [sb(p1, f"xin{i}", [128, D]) for i in range(1)]
            xn = sb(p1, "xn", [128, D], BF16)
            st = sb(p1, "st", [128, 16])
            Pb = [sb(p1, f"Pb{i}", [128, 512], BF16) for i in range(3)]
            OT = sb(p1, "OT", [65, 512])
            rec = sb(p1, "rec", [128, 4])
            attn2 = [sb(p1, f"attn{i}", [128, 4, 512]) for i in range(2)]
            an = sb(p1, "an", [128, D], BF16)
            uh = sb(p1, "uh", [128, 512]); x2 = sb(p1, "x2", [128, 512])
            gvg = sb(p1, "gvg", [128, 512]); y2 = sb(p1, "y2", [128, 512])
            vgn = sb(p1, "vgn", [128, 512], BF16)
            bnst = sb(p1, "bnst", [128, 4, 6]); mv = sb(p1, "mv", [128, 4, 2])
            mixT = sb(p1, "mixT", [128, 8, 128], BF16)
            sgn = sb(p1, "sgn", [128, 4, 512], BF16)
            gnorm = sb(p1, "gnorm", [128, 512])
            bsgu = sb(p1, "bsgu", [128, 4])
            wsT = sb(p1, "wsT", [128, 4, 128], BF16)
            gsb = sb(p1, "gsb", [128, NH, 16]); top8 = sb(p1, "top8", [128, NH, 8])
            mb = sb(p1, "mb", [128, NH, 80], BF16)
            junk = sb(p1, "junk", [128, 512], BF16)

            win_v = win_d.rearrange("(kc p) n -> p kc n", p=128)
            for i in range(5):
                tr.dma("pool", win[:, :, i * 512:(i + 1) * 512], win_v[:, :, i * 512:(i + 1) * 512], w=["win"])
            for h in range(NH):
                tr.dma("pool", KT[64:80, h, :], oneh_d[:, :], w=[("KTo", h)])
            tr.dma("sp", gnorm[:], gnorm_d.partition_broadcast(128), w=["gnorm"])
            tr.dma("sp", bsgu[:], bsgu_d[:, :], w=["bsgu"])
            wsf = y2[:].rearrange("p (g t) -> p g t", g=4)
            tr.dma("sp", wsf, wsgu_d[:, :, :], w=["y2"])
            tr.op("dve", lambda e: e.tensor_tensor(out=wsT[:], in0=wsf,
                                                   in1=tri01[:].unsqueeze(1).to_broadcast([128, 4, 128]), op=ALU.mult),
                  r=["y2"], w=["wsT"])
            tr.op("pool", lambda e: e.memset(V[:, :, :, 64:65], 1.0), w=["Vones"])
            tr.op("pool", lambda e: e.memset(QA[64:80, :, :], 0.0), w=["QAm"])
            tr.op("pool", lambda e: e.memset(gsb[:], -1e30), w=["gsb"])
            tr.op("pool", lambda e: e.memset(mb[:], 0.0), w=["mb"])
            tr.op("pool", lambda e: e.memset(kmT[:], 0.0), w=["kmT"])

            def rstd_ops(src_key, src_ap, dst_ap, dst_key, n):
                tr.op("pool", lambda e: e.tensor_scalar(out=dst_ap, in0=src_ap, scalar1=1.0 / n, scalar2=EPS,
                                                        op0=ALU.mult, op1=ALU.add), r=[src_key], w=[dst_key])
                tr.op("pool", lambda e: e.tensor_tensor(out=dst_ap, in0=dst_ap, in1=mhalf[:, 0:1], op=ALU.pow),
                      r=[dst_key], w=[dst_key])

            MBb = PS[6][:].bitcast(BF16)
            MF = PS[7]
            sctr = [0]
            gctr = [0]

            def gbank():
                i = gctr[0] % 2
                gctr[0] += 1
                return PS[i], ("ps", i)

            def prologue_a(G, tt):
                tok = slice(G * 512 + tt * 128, G * 512 + (tt + 1) * 128)
                xi = 0
                xt = xin[xi]
                tr.dma("sp", xt[:], x_d[tok, :], w=[("xin", xi)])
                tr.op("act", lambda e, xt=xt: e.activation(out=xn[:], in_=xt[:], func=AF.Square,
                                                           accum_out=st[:, 0:1]),
                      r=[("xin", xi)], w=["xn", "st0"])
                rstd_ops("st0", st[:, 0:1], st[:, 1:2], "st1", D)
                tr.op("dve", lambda e, xt=xt: e.tensor_scalar(out=xn[:], in0=xt[:], scalar1=st[:, 1:2], scalar2=None,
                                                              op0=ALU.mult), r=[("xin", xi), "st1"], w=["xn"])
                for c in range(8):
                    tr.op("pe", lambda e, c=c: e.transpose(out=MBb[:, c * 128:(c + 1) * 128],
                                                           in_=xn[:, c * 128:(c + 1) * 128], identity=ident_b[:]),
                          r=["xn"], w=["MB"])
                for c in range(8):
                    tr.op("dve", lambda e, c=c, tt=tt: e.tensor_scalar(
                        out=hT[:, c, tt * 128:(tt + 1) * 128], in0=MBb[:, c * 128:(c + 1) * 128],
                        scalar1=A_m[:, c:c + 1], scalar2=B_m[:, c:c + 1], op0=ALU.mult, op1=ALU.add),
                        r=["MB"], w=[("hT", tt)])

            hTk = [("hT", t) for t in range(4)]

            def inproj_qk(G, oc):
                bank, bk = gbank()
                for kc in range(8):
                    tr.op("pe", lambda e, oc=oc, kc=kc, bank=bank: e.matmul(
                        bank[:, :], lhsT=win[:, kc, oc * 128:(oc + 1) * 128], rhs=hT[:, kc, :],
                        start=(kc == 0), stop=(kc == 7)), r=["win"] + hTk, w=[bk])
                for hh in range(2):
                    h = (oc % 4) * 2 + hh
                    rows = slice(hh * 64, (hh + 1) * 64)
                    if oc < 4:
                        tr.op("dve", lambda e, h=h, rows=rows, bank=bank: e.tensor_copy(
                            out=QA[0:64, h, :], in_=bank[rows, :]), r=[bk], w=[("QA", h)])
                    else:
                        tr.op("dve", lambda e, h=h, rows=rows, bank=bank, G=G: e.tensor_copy(
                            out=KT[0:64, h, G * 512:(G + 1) * 512], in_=bank[rows, :]), r=[bk], w=[("KT", h, G)])
                if oc >= 4:
                    tr.op("dve", lambda e, bank=bank: e.tensor_reduce(
                        out=ksum[:], in_=bank[:, :].rearrange("p (a b) -> p a b", a=2), axis=AX.X, op=ALU.add),
                        r=[bk], w=["ksum"])
                    for hh in range(2):
                        h = (oc % 4) * 2 + hh
                        tr.op("dve", lambda e, h=h, hh=hh, G=G: e.tensor_copy(
                            out=kmT[0:64, h, 2 * G:2 * G + 2], in_=ksum[hh * 64:(hh + 1) * 64, :]),
                            r=["ksum"], w=[("kmT", h)])

            def inproj_v(G, tt):
                ts_ = slice(tt * 128, (tt + 1) * 128)
                gt = G * 4 + tt
                bank, bk = gbank()
                for kc in range(8):
                    tr.op("pe", lambda e, kc=kc, bank=bank, ts_=ts_: e.matmul(
                        bank[:, :], lhsT=hT[:, kc, ts_], rhs=win[:, kc, 1024:1536],
                        start=(kc == 0), stop=(kc == 7)), r=["win", ("hT", tt)], w=[bk])
                tr.op("dve", lambda e, bank=bank, gt=gt: e.tensor_copy(
                    out=V[:, gt, :, 0:64], in_=bank[:, :].rearrange("p (h d) -> p h d", h=NH)),
                    r=[bk], w=[("V", gt)])

            def sgu(G, tt):
                ts_ = slice(tt * 128, (tt + 1) * 128)
                for which in range(2):
                    dst = uh if which == 0 else gvg
                    dk = "uh" if which == 0 else "gvg"
                    co = 1536 + which * 512
                    bank, bk = gbank()
                    for kc in range(8):
                        tr.op("pe", lambda e, kc=kc, bank=bank, ts_=ts_, co=co: e.matmul(
                            bank[:, :], lhsT=hT[:, kc, ts_], rhs=win[:, kc, co:co + 512],
                            start=(kc == 0), stop=(kc == 7)), r=["win", ("hT", tt)], w=[bk])
                    tr.op("act", lambda e, bank=bank, dst=dst: e.activation(out=dst[:], in_=bank[:, :], func=AF.Identity,
                                                                            scale=0.5), r=[bk], w=[dk])
                    tr.op("act", lambda e, bank=bank: e.activation(out=x2[:], in_=bank[:, :], func=AF.Square),
                          r=[bk], w=["x2"])
                    tr.op("pool", lambda e: e.tensor_scalar(out=x2[:], in0=x2[:], scalar1=0.044715, scalar2=1.0,
                                                            op0=ALU.mult, op1=ALU.add), r=["x2"], w=["x2"])
                    tr.op("pool", lambda e, dst=dst: e.tensor_tensor(out=x2[:], in0=x2[:], in1=dst[:], op=ALU.mult),
                          r=["x2", dk], w=["x2"])
                    tr.op("act", lambda e: e.activation(out=x2[:], in_=x2[:], func=AF.Tanh, scale=2.0 * GELU_C),
                          r=["x2"], w=["x2"])
                    tr.op("dve", lambda e, dst=dst: e.scalar_tensor_tensor(
                        out=dst[:], in0=x2[:], scalar=1.0, in1=dst[:], op0=ALU.add, op1=ALU.mult),
                        r=["x2", dk], w=[dk])
                for g in range(4):
                    tr.op("dve", lambda e, g=g: e.bn_stats(out=bnst[:, g, :], in_=gvg[:, g * 128:(g + 1) * 128]),
                          r=["gvg"], w=["bnst"])
                for g in range(4):
                    tr.op("dve", lambda e, g=g: e.bn_aggr(out=mv[:, g, :], in_=bnst[:, g, :]), r=["bnst"], w=["mv"])
                tr.op("pool", lambda e: e.tensor_scalar(out=st[:, 4:8], in0=mv[:, :, 1], scalar1=1.0, scalar2=EPS,
                                                        op0=ALU.mult, op1=ALU.add), r=["mv"], w=["st4"])
                tr.op("pool", lambda e: e.tensor_tensor(out=st[:, 4:8], in0=st[:, 4:8], in1=mhalf[:, 0:4], op=ALU.pow),
                      r=["st4"], w=["st4"])
                for g in range(4):
                    tr.op("dve", lambda e, g=g: e.tensor_scalar(
                        out=gvg[:, g * 128:(g + 1) * 128], in0=gvg[:, g * 128:(g + 1) * 128],
                        scalar1=mv[:, g, 0:1], scalar2=st[:, 4 + g:5 + g], op0=ALU.subtract, op1=ALU.mult),
                        r=["gvg", "mv", "st4"], w=["gvg"])
                tr.op("dve", lambda e: e.tensor_tensor(out=vgn[:], in0=gvg[:], in1=gnorm[:], op=ALU.mult),
                      r=["gvg", "gnorm"], w=["vgn"])
                bank, bk = gbank()
                for g in range(4):
                    tr.op("pe", lambda e, g=g, bank=bank: e.matmul(
                        bank[:, g * 128:(g + 1) * 128], lhsT=wsT[:, g, :], rhs=vgn[:, g * 128:(g + 1) * 128],
                        start=True, stop=True), r=["wsT", "vgn"], w=[bk])
                for g in range(4):
                    tr.op("dve", lambda e, g=g, bank=bank: e.scalar_tensor_tensor(
                        out=y2[:, g * 128:(g + 1) * 128], in0=bank[:, g * 128:(g + 1) * 128],
                        scalar=bsgu[:, g:g + 1], in1=uh[:, g * 128:(g + 1) * 128], op0=ALU.add, op1=ALU.mult),
                        r=[bk, "uh", "bsgu"], w=["y2"])
                tr.op("act", lambda e: e.activation(out=junk[:], in_=y2[:], func=AF.Square,
                                                    accum_out=st[:, 8:9]), r=["y2"], w=["junk", "st8"])
                rstd_ops("st8", st[:, 8:9], st[:, 9:10], "st9", 512)
                tr.op("dve", lambda e, tt=tt: e.tensor_scalar(out=sgn[:, tt, :], in0=y2[:], scalar1=st[:, 9:10],
                                                               scalar2=None, op0=ALU.mult),
                      r=["y2", "st9"], w=[("sgn", tt)])

            def gate(G):
                if G < 2:
                    return
                for tt in range(4):
                    j = 2 * G + tt // 2
                    ts_ = slice(tt * 128, (tt + 1) * 128)
                    for h in range(NH):
                        tr.op("pe", lambda e, h=h, ts_=ts_: e.matmul(
                            MF[:, 384 + h * 16:384 + (h + 1) * 16], lhsT=QA[0:64, h, ts_], rhs=kmT[:, h, :],
                            start=True, stop=True), r=[("QA", h), ("kmT", h)], w=["MFg"])
                    tr.op("dve", lambda e, j=j: e.tensor_copy(
                        out=gsb[:, :, 0:j], in_=MF[:, 384:512].rearrange("p (h n) -> p h n", h=NH)[:, :, 0:j]),
                        r=["MFg"], w=["gsb"])
                    for h in range(NH):
                        tr.op("dve", lambda e, h=h: e.max(out=top8[:, h, :], in_=gsb[:, h, :]), r=["gsb"], w=["top8"])
                    for h in range(NH):
                        tr.op("dve", lambda e, h=h, j=j: e.tensor_scalar(
                            out=mb[:, h, 64:64 + j], in0=gsb[:, h, 0:j], scalar1=top8[:, h, 2:3], scalar2=NEGM,
                            op0=ALU.is_lt, op1=ALU.mult), r=["gsb", "top8"], w=["mb"])
                    for h in range(NH):
                        tr.op("pe", lambda e, h=h: e.transpose(out=MBb[0:80, h * 128:(h + 1) * 128],
                                                               in_=mb[:, h, :], identity=ident_b[:]),
                              r=["mb"], w=["MB"])
                    tr.op("dve", lambda e, ts_=ts_: e.tensor_copy(
                        out=QA[64:80, :, ts_], in_=MBb[64:80, :].rearrange("p (h t) -> p h t", h=NH)),
                        r=["MB"], w=["QAm"])

            def attn_items(G):
                return [(G, h, kt) for h in range(NH) for kt in range(4 * G + 4)]

            def qk(G, h, kt):
                c0 = 0 if kt <= 4 * G else (kt - 4 * G) * 128
                si = sctr[0] % 2
                sctr[0] += 1
                Sb = PS[2 + si]
                sk = ("ps", 2 + si)
                tri = kt >= 4 * G
                tr.op("pe", lambda e, h=h, kt=kt, c0=c0, Sb=Sb, tri=tri: e.matmul(
                    Sb[:, c0:512], lhsT=KT[0:80, h, kt * 128:(kt + 1) * 128], rhs=QA[0:80, h, c0:512],
                    start=True, stop=(not tri)),
                    r=[("KT", h, kt // 4), ("KTo", h), ("QA", h), "QAm"], w=[sk])
                if tri:
                    ct0 = (kt - 4 * G) * 128
                    tr.op("pe", lambda e, Sb=Sb, ct0=ct0: e.matmul(
                        Sb[:, ct0:ct0 + 128], lhsT=ident_b[:], rhs=tribias[:], start=False, stop=True),
                        r=[], w=[sk])
                return (Sb, sk, c0)

            def exp_pv(G, h, kt, sinfo, idx):
                Sb, sk, c0 = sinfo
                nkt = 4 * G + 4
                pi = idx % 3
                Ob = PS[4 + h % 2]
                ok = ("ps", 4 + h % 2)
                tr.op("act", lambda e, Sb=Sb, c0=c0, pi=pi: e.activation(
                    out=Pb[pi][:, c0:512], in_=Sb[:, c0:512], func=AF.Exp, scale=0.125),
                    r=[sk], w=[("Pb", pi)])

                def pv():
                    tr.op("pe", lambda e, h=h, kt=kt, c0=c0, pi=pi, Ob=Ob, nkt=nkt: e.matmul(
                        Ob[0:65, c0:512], lhsT=V[:, kt, h, :], rhs=Pb[pi][:, c0:512],
                        start=(kt == 0), stop=(kt == nkt - 1)),
                        r=[("V", kt), "Vones", ("Pb", pi)], w=[ok])
                    if kt == nkt - 1:
                        head_finish(G, h, Ob, ok)
                return pv

            def head_finish(G, h, Ob, ok):
                tr.op("dve", lambda e, Ob=Ob: e.tensor_copy(out=OT[:], in_=Ob[0:65, :]), r=[ok], w=["OT"])
                for tt in range(4):
                    tr.op("pe", lambda e, tt=tt: e.transpose(out=MF[:, tt * 65:(tt + 1) * 65],
                                                             in_=OT[:, tt * 128:(tt + 1) * 128],
                                                             identity=ident_f[0:65, 0:65]),
                          r=["OT"], w=["MFo"])
                MFo = MF[:, 0:260].rearrange("p (t d) -> p t d", t=4)
                tr.op("dve", lambda e, MFo=MFo: e.reciprocal(out=rec[:], in_=MFo[:, :, 64]), r=["MFo"], w=["rec"])
                attn = attn2[G % 2]
                tr.op("dve", lambda e, MFo=MFo, h=h, attn=attn: e.tensor_tensor(
                    out=attn[:, :, h * 64:(h + 1) * 64], in0=MFo[:, :, 0:64],
                    in1=rec[:].unsqueeze(2).to_broadcast([128, 4, 64]), op=ALU.mult),
                    r=["MFo", "rec"], w=[("attn", G % 2)])

            def epilogue(G, tt):
                attn = attn2[G % 2]
                ak = ("attn", G % 2)
                tr.op("act", lambda e, tt=tt, attn=attn: e.activation(out=junk[:], in_=attn[:, tt, :], func=AF.Square,
                                                                      accum_out=st[:, 10:11]), r=[ak], w=["junk", "st10"])
                rstd_ops("st10", st[:, 10:11], st[:, 11:12], "st11", 512)
                tr.op("dve", lambda e, tt=tt, attn=attn: e.tensor_scalar(out=an[:, 0:512], in0=attn[:, tt, :],
                                                                         scalar1=st[:, 11:12], scalar2=None, op0=ALU.mult),
                      r=[ak, "st11"], w=["an"])
                tr.op("dve", lambda e, tt=tt: e.tensor_copy(out=an[:, 512:1024], in_=sgn[:, tt, :]),
                      r=[("sgn", tt)], w=["an"])
                for c in range(8):
                    tr.op("pe", lambda e, c=c: e.transpose(out=MBb[:, c * 128:(c + 1) * 128],
                                                           in_=an[:, c * 128:(c + 1) * 128], identity=ident_b[:]),
                          r=["an"], w=["MB"])
                tr.op("dve", lambda e, tt=tt: e.tensor_tensor(
                    out=mixT[:, :, :], in0=MBb[:, :].rearrange("p (c t) -> p c t", c=8),
                    in1=gmix[:].unsqueeze(2).to_broadcast([128, 8, 128]), op=ALU.mult),
                    r=["MB"], w=["mixT"])
                tr.dma("sp", mix_d[G * 4 + tt].rearrange("p (c t) -> p c t", c=8), mixT[:], r=["mixT"], w=["mixd"])

            def attention(G, side):
                items = attn_items(G)
                n_it = len(items)
                sin = {}
                for i in range(min(LOOK, n_it)):
                    sin[i] = qk(*items[i])
                step = max(1, n_it // (len(side) + 1)) if side else n_it + 1
                for i in range(n_it):
                    if LOOK == 0:
                        sin[i] = qk(*items[i])
                    pv = exp_pv(*items[i], sin.pop(i), i)
                    if LOOK > 0 and i + LOOK < n_it:
                        sin[i + LOOK] = qk(*items[i + LOOK])
                    pv()
                    if side and (i + 1) % step == 0:
                        side.pop(0)()
                while side:
                    side.pop(0)()

            if INTERLEAVE:
                for tt in range(4):
                    prologue_a(0, tt)
                for oc in range(8):
                    inproj_qk(0, oc)
                for tt in range(4):
                    inproj_v(0, tt)
                gate(0)
                for G in range(8):
                    side = []
                    for t in range(4):
                        if G > 0:
                            side.append(lambda t=t, G=G: epilogue(G - 1, t))
                        side.append(lambda t=t, G=G: sgu(G, t))
                    if G + 1 < 8:
                        side += [(lambda t=t, G=G: prologue_a(G + 1, t)) for t in range(4)]
                    attention(G, side)
                    if G + 1 < 8:
                        for oc in range(8):
                            inproj_qk(G + 1, oc)
                        for tt in range(4):
                            inproj_v(G + 1, tt)
                        gate(G + 1)
                for tt in range(4):
                    epilogue(7, tt)
            else:
                for G in range(8):
                    for tt in range(4):
                        prologue_a(G, tt)
                    for oc in range(8):
                        inproj_qk(G, oc)
                    for tt in range(4):
                        inproj_v(G, tt)
                        sgu(G, tt)
                    gate(G)
                    attention(G, [])
                    for tt in range(4):
                        epilogue(G, tt)
            tr.op("sp", None, r=["mixd"])
            tr.emit(nc, p1, "p1")

        with ExitStack() as p2:
            tr = Tr()
            wout = sb(p2, "wout", [128, 8, D], BF16)
            wgu = sb(p2, "wgu", [128, 8, 2 * DFF], BF16)
            wdn = sb(p2, "wdn", [128, NFC, D], BF16)
            actT = sb(p2, "actT", [128, NFC, 256], BF16)
            hb = sb(p2, "hb", [128, 8, 256], BF16)
            xt2 = [sb(p2, f"xt2{i}", [128, D]) for i in range(2)]
            xn2 = sb(p2, "xn2", [128, D], BF16)
            junk2 = sb(p2, "junk2", [128, D], BF16)
            ytmp = sb(p2, "ytmp", [128, 512])
            sg = sb(p2, "sg", [128, 256])
            st2 = sb(p2, "st2", [128, 16])
            GM = sb(p2, "GM", [128, D]); GF = sb(p2, "GF", [128, D])
            tr.dma("sp", GM[:], gpostm_d.partition_broadcast(128), w=["GM"])
            tr.dma("sp", GF[:], gpostf_d.partition_broadcast(128), w=["GF"])

            def sink2(j, mc, mk):
                G_ = GM if j < 6 else GF
                gk = "GM" if j < 6 else "GF"
                hs = slice((j % 2) * 512, (j % 2 + 1) * 512)
                tr.op("dve", lambda e, G_=G_, hs=hs, mc=mc: e.tensor_tensor(
                    out=G_[:, hs], in0=G_[:, hs], in1=mc[:], op=ALU.mult), r=[mk, gk], w=[gk])
            ada_section(tr, p2, [4, 5, 10, 11], 1, sink2, "a2")
            MBb = PS[6][:].bitcast(BF16)

            def rstd2(src_key, src_ap, dst_ap, dst_key, n):
                tr.op("pool", lambda e: e.tensor_scalar(out=dst_ap, in0=src_ap, scalar1=1.0 / n, scalar2=EPS,
                                                        op0=ALU.mult, op1=ALU.add), r=[src_key], w=[dst_key])
                tr.op("pool", lambda e: e.tensor_tensor(out=dst_ap, in0=dst_ap, in1=mhalf[:, 0:1], op=ALU.pow),
                      r=[dst_key], w=[dst_key])

            wout_v = wout_d.rearrange("(kc p) n -> p kc n", p=128)
            wgu_v = wgu_d.rearrange("(kc p) n -> p kc n", p=128)
            wdn_v = wdn_d.rearrange("(fc p) n -> p fc n", p=128)
            tr.dma("pool", wout[:], wout_v, w=["wout"])
            for fc in range(NFC):
                for pt in range(2):
                    co = pt * DFF + fc * 128
                    tr.dma("pool", wgu[:, :, co:co + 128], wgu_v[:, :, co:co + 128], w=[("wgu", fc)])
            for i in range(2):
                tr.dma("pool", wdn[:, i * 11:(i + 1) * 11, :], wdn_v[:, i * 11:(i + 1) * 11, :], w=["wdn"])
            bctr = [0]

            def pbank():
                i = bctr[0] % 6
                bctr[0] += 1
                return PS[i], ("ps", i)

            for G in range(16):
                for tt in range(2):
                    tr.dma("sp", hb[:, :, tt * 128:(tt + 1) * 128],
                           mix_d[G * 2 + tt].rearrange("p (c t) -> p c t", c=8), w=[("hb", tt)])
                for tt in range(2):
                    ts_ = slice(tt * 128, (tt + 1) * 128)
                    tok = slice(G * 256 + tt * 128, G * 256 + (tt + 1) * 128)
                    xt = xt2[tt]
                    xk = ("xt", tt)
                    tr.dma("sp", xt[:], x_d[tok, :], w=[xk])
                    for half in range(2):
                        hs = slice(half * 512, (half + 1) * 512)
                        bank, bk = pbank()
                        for kc in range(8):
                            tr.op("pe", lambda e, kc=kc, bank=bank, ts_=ts_, hs=hs: e.matmul(
                                bank[:, :], lhsT=hb[:, kc, ts_], rhs=wout[:, kc, hs], start=(kc == 0), stop=(kc == 7)),
                                r=["wout", ("hb", tt)], w=[bk])
                        tr.op("act", lambda e, bank=bank, half=half: e.activation(
                            out=junk2[:, 0:512], in_=bank[:, :], func=AF.Square, accum_out=st2[:, half:half + 1]),
                            r=[bk], w=["junk2", ("ssq", half)])
                        if half == 0:
                            b0, bk0 = bank, bk
                        else:
                            b1, bk1 = bank, bk
                    tr.op("pool", lambda e: e.tensor_tensor(out=st2[:, 2:3], in0=st2[:, 0:1], in1=st2[:, 1:2], op=ALU.add),
                          r=[("ssq", 0), ("ssq", 1)], w=["s2"])
                    rstd2("s2", st2[:, 2:3], st2[:, 3:4], "s3", D)
                    for half, (bank, bk) in enumerate(((b0, bk0), (b1, bk1))):
                        hs = slice(half * 512, (half + 1) * 512)
                        tr.op("dve", lambda e, bank=bank, hs=hs: e.scalar_tensor_tensor(
                            out=ytmp[:], in0=bank[:, :], scalar=st2[:, 3:4], in1=GM[:, hs], op0=ALU.mult, op1=ALU.mult),
                            r=[bk, "s3", "GM"], w=["ytmp"])
                        tr.op("dve", lambda e, xt=xt, hs=hs: e.tensor_tensor(out=xt[:, hs], in0=xt[:, hs], in1=ytmp[:],
                                                                             op=ALU.add), r=["ytmp", xk], w=[xk])
                    tr.op("act", lambda e, xt=xt: e.activation(out=junk2[:], in_=xt[:], func=AF.Square,
                                                               accum_out=st2[:, 4:5]), r=[xk], w=["junk2", "s4"])
                    rstd2("s4", st2[:, 4:5], st2[:, 5:6], "s5", D)
                    tr.op("dve", lambda e, xt=xt: e.tensor_scalar(out=xn2[:], in0=xt[:], scalar1=st2[:, 5:6], scalar2=None,
                                                                  op0=ALU.mult), r=[xk, "s5"], w=["xn2"])
                    for c in range(8):
                        tr.op("pe", lambda e, c=c: e.transpose(out=MBb[:, c * 128:(c + 1) * 128],
                                                               in_=xn2[:, c * 128:(c + 1) * 128], identity=ident_b[:]),
                              r=["xn2"], w=["MB"])
                    for c in range(8):
                        tr.op("dve", lambda e, c=c, ts_=ts_: e.tensor_scalar(
                            out=hb[:, c, ts_], in0=MBb[:, c * 128:(c + 1) * 128],
                            scalar1=A_f[:, c:c + 1], scalar2=B_f[:, c:c + 1], op0=ALU.mult, op1=ALU.add),
                            r=["MB"], w=[("hb", tt)])
                for fc in range(NFC):
                    gb, gk = pbank()
                    ub, uk = pbank()
                    for pt, bank, bk in ((0, gb, gk), (1, ub, uk)):
                        co = pt * DFF + fc * 128
                        for kc in range(8):
                            tr.op("pe", lambda e, kc=kc, bank=bank, co=co: e.matmul(
                                bank[:, 0:256], lhsT=wgu[:, kc, co:co + 128], rhs=hb[:, kc, :],
                                start=(kc == 0), stop=(kc == 7)),
                                r=[("wgu", fc), ("hb", 0), ("hb", 1)], w=[bk])
                    tr.op("act", lambda e, gb=gb: e.activation(out=sg[:], in_=gb[:, 0:256], func=AF.Silu),
                          r=[gk], w=["sg"])
                    tr.op("dve", lambda e, ub=ub, fc=fc: e.tensor_tensor(out=actT[:, fc, :], in0=sg[:], in1=ub[:, 0:256],
                                                                         op=ALU.mult), r=["sg", uk], w=[("actT", fc)])
                for tt in range(2):
                    ts_ = slice(tt * 128, (tt + 1) * 128)
                    tok = slice(G * 256 + tt * 128, G * 256 + (tt + 1) * 128)
                    xt = xt2[tt]
                    xk = ("xt", tt)
                    banks = []
                    for half in range(2):
                        hs = slice(half * 512, (half + 1) * 512)
                        bank, bk = pbank()
                        banks.append((bank, bk))
                        for fc in range(NFC):
                            tr.op("pe", lambda e, fc=fc, bank=bank, ts_=ts_, hs=hs: e.matmul(
                                bank[:, :], lhsT=actT[:, fc, ts_], rhs=wdn[:, fc, hs],
                                start=(fc == 0), stop=(fc == NFC - 1)),
                                r=["wdn", ("actT", fc)], w=[bk])
                        tr.op("act", lambda e, bank=bank, half=half: e.activation(
                            out=junk2[:, 0:512], in_=bank[:, :], func=AF.Square, accum_out=st2[:, 8 + half:9 + half]),
                            r=[bk], w=["junk2", ("ssq2", half)])
                    tr.op("pool", lambda e: e.tensor_tensor(out=st2[:, 10:11], in0=st2[:, 8:9], in1=st2[:, 9:10], op=ALU.add),
                          r=[("ssq2", 0), ("ssq2", 1)], w=["s10"])
                    rstd2("s10", st2[:, 10:11], st2[:, 11:12], "s11", D)
                    for half, (bank, bk) in enumerate(banks):
                        hs = slice(half * 512, (half + 1) * 512)
                        tr.op("dve", lambda e, bank=bank, hs=hs: e.scalar_tensor_tensor(
                            out=ytmp[:], in0=bank[:, :], scalar=st2[:, 11:12], in1=GF[:, hs], op0=ALU.mult, op1=ALU.mult),
                            r=[bk, "s11", "GF"], w=["ytmp"])
                        tr.op("dve", lambda e, xt=xt, hs=hs: e.tensor_tensor(out=xt[:, hs], in0=xt[:, hs], in1=ytmp[:],
                                                                             op=ALU.add), r=["ytmp", xk], w=[xk])
                    tr.dma("sp", out_d[tok, :], xt[:], r=[xk], w=["outd"])
            tr.op("sp", None, r=["outd"])
            tr.emit(nc, p2, "p2")
    return nc


def _consts():
    ident = np.eye(128, dtype=np.float32)
    p = np.arange(128)
    tri01 = (p[:, None] <= p[None, :]).astype(np.float32)
    onehot = (np.arange(S)[None, :] // 256 == np.arange(16)[:, None]).astype(np.float32)
    return ident, tri01, onehot


def kernel(x, c, w_ada, b_ada, g_pre_mix, g_post_mix, w_in, g_sgu_norm, w_sgu, b_sgu,
           g_attn_out, g_sgu_out, w_out, g_pre_ffn, g_post_ffn, w_gate_up, w_down):
    f = lambda a: np.ascontiguousarray(np.asarray(a, dtype=np.float32))
    x = f(x); c = f(c)
    ident, tri01, onehot = _consts()
    pp = lambda v: f(np.asarray(v).reshape(-1, 128).T)
    shared = {
        "w_ada": f(w_ada[0]), "b_ada": f(b_ada[0]).reshape(1, -1),
        "gpm_t": pp(g_pre_mix[0]), "gpf_t": pp(g_pre_ffn[0]),
        "gmix_t": f(np.concatenate([pp(g_attn_out[0]), pp(g_sgu_out[0])], axis=1)),
        "g_post_mix": f(g_post_mix[0]).reshape(1, -1), "g_post_ffn": f(g_post_ffn[0]).reshape(1, -1),
        "g_sgu_norm": f(g_sgu_norm[0]).reshape(1, -1),
        "bsgu_t": f(np.asarray(b_sgu[0]).T),
        "wsgu_t": f(np.transpose(np.asarray(w_sgu[0]), (2, 0, 1))),
        "w_in": f(w_in[0]), "w_out": f(w_out[0]), "w_gate_up": f(w_gate_up[0]), "w_down": f(w_down[0]),
        "ident": ident, "tri01": tri01, "onehot": onehot,
    }
    in_maps = []
    for b in range(8):
        m = dict(shared)
        m["x"] = x[b]
        m["ct"] = pp(c[b])
        in_maps.append(m)
    nc = build_program()
    res = run_bass_kernel_spmd(nc, in_maps, core_ids=list(range(8)))
    return np.stack([np.asarray(r["out"], dtype=np.float32) for r in res.results], axis=0)
```

```python
from contextlib import ExitStack
import numpy as np
import concourse.bass as bass
import concourse.mybir as mybir
from concourse.bass_utils import run_bass_kernel_spmd

F32, BF16 = mybir.dt.float32, mybir.dt.bfloat16
AF = mybir.ActivationFunctionType
ALU = mybir.AluOpType
AX = mybir.AxisListType

S, D, NH, HD = 4096, 1024, 8, 64
DFF = 2816
NFC = DFF // 128
EPS = 1e-6
NEGM = -30000.0
GELU_C = 0.7978845608028654
STRICT = False
LOOK = 1
INTERLEAVE = True


class Op:
    __slots__ = ("eng", "fn", "deps", "signal", "cnt", "dma", "sem", "target")


class Tr:
    ENG = ("pe", "act", "dve", "pool", "sp")

    def __init__(self):
        self.streams = {e: [] for e in self.ENG}
        self.last_w = {}
        self.readers = {}

    def _add(self, eng, fn, r, w, dma):
        op = Op()
        op.eng, op.fn, op.dma, op.signal, op.cnt, op.sem, op.target = eng, fn, dma, False, 0, None, 0
        deps = set()
        for k in r:
            p = self.last_w.get(k)
            if p is not None and (STRICT or not (eng == "pe" and p.eng == "pe" and not p.dma)):
                deps.add(p)
        for k in w:
            p = self.last_w.get(k)
            if p is not None and (STRICT or p.eng != eng or p.dma or dma):
                deps.add(p)
            lastr = {}
            for q in self.readers.get(k, ()):
                if STRICT or q.eng != eng or q.dma or dma:
                    if q.dma or STRICT:
                        deps.add(q)
                    else:
                        lastr[q.eng] = q
            deps.update(lastr.values())
        op.deps = deps
        for d in deps:
            d.signal = True
        for k in r:
            self.readers.setdefault(k, []).append(op)
        for k in w:
            self.last_w[k] = op
            self.readers[k] = []
        self.streams[eng].append(op)
        return op

    cap = None

    def op(self, eng, fn, r=(), w=()):
        if self.cap is not None:
            self.cap.append((eng, fn, tuple(r), tuple(w), False))
            return None
        return self._add(eng, fn, r, w, False)

    def dma(self, q, out, in_, r=(), w=()):
        fn = lambda e, o=out, i=in_: e.dma_start(out=o, in_=i)
        if self.cap is not None:
            self.cap.append((q, fn, tuple(r), tuple(w), True))
            return None
        return self._add(q, fn, r, w, True)

    def capture(self, fns):
        self.cap = []
        for f in fns:
            f()
        ops, self.cap = self.cap, None
        return ops

    def flush(self, ops, n):
        for _ in range(min(n, len(ops))):
            eng, fn, r, w, dma = ops.pop(0)
            self._add(eng, fn, r, w, dma)

    def emit(self, nc, stack, tag, ndma_sems=12):
        engsem = {e: stack.enter_context(nc.semaphore(f"{tag}_es_{e}")) for e in ("pe", "act", "dve", "pool")}
        dsems = {q: [stack.enter_context(nc.semaphore(f"{tag}_ds_{q}{i}")) for i in range(ndma_sems)]
                 for q in ("sp", "pool", "act") if any(o.dma for o in self.streams[q])}
        for e in self.ENG:
            c = 0
            nd = 0
            for o in self.streams[e]:
                if o.dma:
                    o.sem = dsems[e][nd % ndma_sems]
                    o.target = 16 * (nd // ndma_sems + 1)
                    nd += 1
                elif o.signal:
                    c += 1
                    o.cnt = c
        block = stack.enter_context(nc.Block())

        def run(ename, eng):
            waited = {}
            for o in self.streams[ename]:
                need = {}
                for d in o.deps:
                    if d.dma:
                        s_, v_ = d.sem, d.target
                    else:
                        s_, v_ = engsem[d.eng], d.cnt
                    if need.get(id(s_), (None, 0))[1] < v_:
                        need[id(s_)] = (s_, v_)
                if o.dma and o.target > 16:
                    if need.get(id(o.sem), (None, 0))[1] < o.target - 16:
                        need[id(o.sem)] = (o.sem, o.target - 16)
                for sid, (s_, v_) in need.items():
                    if waited.get(sid, 0) < v_:
                        eng.wait_ge(s_, v_)
                        waited[sid] = v_
                if o.fn is None:
                    continue
                ins = o.fn(eng)
                if o.dma:
                    ins.then_inc(o.sem, 16)
                elif o.signal:
                    ins.then_inc(engsem[ename], 1)

        @block.tensor
        def _(e):
            run("pe", e)

        @block.scalar
        def _(e):
            run("act", e)

        @block.vector
        def _(e):
            run("dve", e)

        @block.gpsimd
        def _(e):
            run("pool", e)

        @block.sync
        def _(e):
            run("sp", e)


def build_program():
    nc = bass.Bass("TRN2", target_bir_lowering=False)
    dt = lambda n, s, d=F32, k="ExternalInput": nc.dram_tensor(n, s, d, kind=k).ap()
    x_d = dt("x", [S, D])
    ct_d = dt("ct", [128, 8])
    wada_d = dt("w_ada", [D, 6 * D])
    bada_d = dt("b_ada", [1, 6 * D])
    gpm_d = dt("gpm_t", [128, 8])
    gpf_d = dt("gpf_t", [128, 8])
    gmix_d = dt("gmix_t", [128, 8])
    gpostm_d = dt("g_post_mix", [1, D])
    gpostf_d = dt("g_post_ffn", [1, D])
    gnorm_d = dt("g_sgu_norm", [1, 512])
    bsgu_d = dt("bsgu_t", [128, 4])
    wsgu_d = dt("wsgu_t", [128, 4, 128])
    win_d = dt("w_in", [D, 2560])
    wout_d = dt("w_out", [D, D])
    wgu_d = dt("w_gate_up", [D, 2 * DFF])
    wdn_d = dt("w_down", [DFF, D])
    ident_d = dt("ident", [128, 128])
    tri_d = dt("tri01", [128, 128])
    oneh_d = dt("onehot", [16, S])
    out_d = dt("out", [S, D], F32, "ExternalOutput")
    mix_d = nc.dram_tensor("mixscr", [32, 128, 8 * 128], BF16).ap()

    with ExitStack() as gl:
        sb = lambda st, n, s, d=F32: st.enter_context(nc.sbuf_tensor(n, s, d))
        PS = [gl.enter_context(nc.psum_tensor(f"ps{i}", [128, 512], F32)) for i in range(8)]
        ident_f = sb(gl, "ident_f", [128, 128])
        ident_b = sb(gl, "ident_b", [128, 128], BF16)
        tri01 = sb(gl, "tri01s", [128, 128])
        tribias = sb(gl, "tribias", [128, 128], BF16)
        mhalf = sb(gl, "mhalf", [128, 8])
        A_m = sb(gl, "A_m", [128, 8]); B_m = sb(gl, "B_m", [128, 8])
        A_f = sb(gl, "A_f", [128, 8]); B_f = sb(gl, "B_f", [128, 8])
        gmix = sb(gl, "gmix", [128, 8])
        crep = sb(gl, "crep", [128, 8, 128], BF16)
        wada_v = wada_d.rearrange("(kc p) n -> p kc n", p=128)

        def ada_section(tr, stk, js, nslots, sink, tg):
            wa = [sb(stk, f"{tg}wa{i}", [128, 8, 512], BF16) for i in range(nslots)]
            bb = [sb(stk, f"{tg}bb{i}", [128, 512]) for i in range(nslots)]
            modc = [sb(stk, f"{tg}modc{i}", [128, 512]) for i in range(nslots)]
            for n_, j in enumerate(js):
                sl = n_ % nslots
                cs = slice(j * 512, (j + 1) * 512)
                tr.dma("pool", wa[sl][:], wada_v[:, :, cs], w=[("wa", sl)])
                tr.dma("sp", bb[sl][:], bada_d[:, cs].partition_broadcast(128), w=[("bb", sl)])
                bank = PS[sl]
                for kc in range(8):
                    tr.op("pe", lambda e, kc=kc, sl=sl, bank=bank: e.matmul(
                        bank[:, :], lhsT=crep[:, kc, :], rhs=wa[sl][:, kc, :], start=(kc == 0), stop=(kc == 7)),
                        r=["crep", ("wa", sl)], w=[("ps", sl)])
                tr.op("dve", lambda e, sl=sl, bank=bank: e.tensor_tensor(
                    out=modc[sl][:], in0=bank[:, :], in1=bb[sl][:], op=ALU.add),
                    r=[("ps", sl), ("bb", sl)], w=[("modc", sl)])
                sink(j, modc[sl], ("modc", sl))

        with ExitStack() as p0:
            tr = Tr()
            ctile = sb(p0, "ctile", [128, 8]); cact = sb(p0, "cact", [128, 8])
            gpm = sb(p0, "gpm", [128, 8]); gpf = sb(p0, "gpf", [128, 8])
            dtmp = sb(p0, "dtmp", [128, 4, 128])
            pp = sb(p0, "pp", [128, 4, 8])
            tr.dma("sp", ident_f[:], ident_d[:, :], w=["ident_f"])
            tr.dma("sp", tri01[:], tri_d[:, :], w=["tri01"])
            tr.dma("sp", ctile[:], ct_d[:, :], w=["ctile"])
            tr.dma("sp", gpm[:], gpm_d[:, :], w=["gpm"])
            tr.dma("sp", gpf[:], gpf_d[:, :], w=["gpf"])
            tr.dma("sp", gmix[:], gmix_d[:, :], w=["gmix"])
            tr.op("dve", lambda e: e.tensor_copy(out=ident_b[:], in_=ident_f[:]), r=["ident_f"], w=["ident_b"])
            tr.op("dve", lambda e: e.tensor_scalar(out=tribias[:], in0=tri01[:], scalar1=-NEGM, scalar2=NEGM,
                                                   op0=ALU.mult, op1=ALU.add), r=["tri01"], w=["tribias"])
            tr.op("pool", lambda e: e.memset(mhalf[:], -0.5), w=["mhalf"])
            tr.op("act", lambda e: e.activation(out=cact[:], in_=ctile[:], func=AF.Silu), r=["ctile"], w=["cact"])
            tr.op("dve", lambda e: e.tensor_copy(out=crep[:], in_=cact[:].unsqueeze(2).to_broadcast([128, 8, 128])),
                  r=["cact"], w=["crep"])
            def sink0(j, mc, mk):
                v, half = j // 2, j % 2
                vi = {0: 0, 1: 1, 3: 2, 4: 3}[v]
                tr.op("dve", lambda e, mc=mc: e.tensor_tensor(
                    out=dtmp[:], in0=mc[:].rearrange("p (a b) -> p a b", a=4),
                    in1=ident_f[:].unsqueeze(1).to_broadcast([128, 4, 128]), op=ALU.mult),
                    r=[mk, "ident_f"], w=["dtmp"])
                tr.op("dve", lambda e, vi=vi, half=half: e.tensor_reduce(
                    out=pp[:, vi, half * 4:(half + 1) * 4], in_=dtmp[:], axis=AX.X, op=ALU.add),
                    r=["dtmp"], w=["pp"])
            ada_section(tr, p0, [0, 1, 2, 3, 6, 7, 8, 9], 2, sink0, "a0")
            tr.op("dve", lambda e: e.scalar_tensor_tensor(out=A_m[:], in0=pp[:, 1, :], scalar=1.0, in1=gpm[:],
                                                          op0=ALU.add, op1=ALU.mult), r=["pp", "gpm"], w=["A_m"])
            tr.op("dve", lambda e: e.tensor_copy(out=B_m[:], in_=pp[:, 0, :]), r=["pp"], w=["B_m"])
            tr.op("dve", lambda e: e.scalar_tensor_tensor(out=A_f[:], in0=pp[:, 3, :], scalar=1.0, in1=gpf[:],
                                                          op0=ALU.add, op1=ALU.mult), r=["pp", "gpf"], w=["A_f"])
            tr.op("dve", lambda e: e.tensor_copy(out=B_f[:], in_=pp[:, 2, :]), r=["pp"], w=["B_f"])
            tr.op("sp", None, r=["crep", "A_m", "B_m", "A_f", "B_f", "gmix", "ident_b", "tribias", "mhalf"])
            tr.emit(nc, p0, "p0")

        with ExitStack() as p1:
            tr = Tr()
            win = sb(p1, "win", [128, 8, 2560], BF16)
            KT = sb(p1, "KT", [80, NH, S], BF16)
            V = sb(p1, "V", [128, 32, NH, 65], BF16)
            kmT = sb(p1, "kmT", [64, NH, 16], BF16)
            ksum = sb(p1, "ksum", [128, 2])
            hT = sb(p1, "hT", [128, 8, 512], BF16)
            QA = sb(p1, "QA", [80, NH, 512], BF16)
            xin = [sb(p1, f"xin{i}", [128, D]) for i in range(1)]
            xn = sb(p1, "xn", [128, D], BF16)
            st = sb(p1, "st", [128, 16])
            Pb = [sb(p1, f"Pb{i}", [128, 512], BF16) for i in range(3)]
            OT = sb(p1, "OT", [65, 512])
            rec = sb(p1, "rec", [128, 4])
            attn2 = [sb(p1, f"attn{i}", [128, 4, 512]) for i in range(2)]
            an = sb(p1, "an", [128, D], BF16)
            uh = sb(p1, "uh", [128, 512]); x2 = sb(p1, "x2", [128, 512])
            gvg = sb(p1, "gvg", [128, 512]); y2 = sb(p1, "y2", [128, 512])
            vgn = sb(p1, "vgn", [128, 512], BF16)
            bnst = sb(p1, "bnst", [128, 4, 6]); mv = sb(p1, "mv", [128, 4, 2])
            mixT = sb(p1, "mixT", [128, 8, 128], BF16)
            sgn = sb(p1, "sgn", [128, 4, 512], BF16)
            gnorm = sb(p1, "gnorm", [128, 512])
            bsgu = sb(p1, "bsgu", [128, 4])
            wsT = sb(p1, "wsT", [128, 4, 128], BF16)
            gsb = sb(p1, "gsb", [128, NH, 16]); top8 = sb(p1, "top8", [128, NH, 8])
            mb = sb(p1, "mb", [128, NH, 80], BF16)
            junk = sb(p1, "junk", [128, 512], BF16)

            win_v = win_d.rearrange("(kc p) n -> p kc n", p=128)
            for i in range(5):
                tr.dma("pool", win[:, :, i * 512:(i + 1) * 512], win_v[:, :, i * 512:(i + 1) * 512], w=["win"])
            for h in range(NH):
                tr.dma("pool", KT[64:80, h, :], oneh_d[:, :], w=[("KTo", h)])
            tr.dma("sp", gnorm[:], gnorm_d.partition_broadcast(128), w=["gnorm"])
            tr.dma("sp", bsgu[:], bsgu_d[:, :], w=["bsgu"])
            wsf = y2[:].rearrange("p (g t) -> p g t", g=4)
            tr.dma("sp", wsf, wsgu_d[:, :, :], w=["y2"])
            tr.op("dve", lambda e: e.tensor_tensor(out=wsT[:], in0=wsf,
                                                   in1=tri01[:].unsqueeze(1).to_broadcast([128, 4, 128]), op=ALU.mult),
                  r=["y2"], w=["wsT"])
            tr.op("pool", lambda e: e.memset(V[:, :, :, 64:65], 1.0), w=["Vones"])
            tr.op("pool", lambda e: e.memset(QA[64:80, :, :], 0.0), w=["QAm"])
            tr.op("pool", lambda e: e.memset(gsb[:], -1e30), w=["gsb"])
            tr.op("pool", lambda e: e.memset(mb[:], 0.0), w=["mb"])
            tr.op("pool", lambda e: e.memset(kmT[:], 0.0), w=["kmT"])

            def rstd_ops(src_key, src_ap, dst_ap, dst_key, n):
                tr.op("pool", lambda e: e.tensor_scalar(out=dst_ap, in0=src_ap, scalar1=1.0 / n, scalar2=EPS,
                                                        op0=ALU.mult, op1=ALU.add), r=[src_key], w=[dst_key])
                tr.op("pool", lambda e: e.tensor_tensor(out=dst_ap, in0=dst_ap, in1=mhalf[:, 0:1], op=ALU.pow),
                      r=[dst_key], w=[dst_key])

            MBb = PS[6][:].bitcast(BF16)
            MF = PS[7]
            sctr = [0]
            gctr = [0]

            def gbank():
                i = gctr[0] % 2
                gctr[0] += 1
                return PS[i], ("ps", i)

            def prologue_a(G, tt):
                tok = slice(G * 512 + tt * 128, G * 512 + (tt + 1) * 128)
                xi = 0
                xt = xin[xi]
                tr.dma("sp", xt[:], x_d[tok, :], w=[("xin", xi)])
                tr.op("act", lambda e, xt=xt: e.activation(out=xn[:], in_=xt[:], func=AF.Square,
                                                           accum_out=st[:, 0:1]),
                      r=[("xin", xi)], w=["xn", "st0"])
                rstd_ops("st0", st[:, 0:1], st[:, 1:2], "st1", D)
                tr.op("dve", lambda e, xt=xt: e.tensor_scalar(out=xn[:], in0=xt[:], scalar1=st[:, 1:2], scalar2=None,
                                                              op0=ALU.mult), r=[("xin", xi), "st1"], w=["xn"])
                for c in range(8):
                    tr.op("pe", lambda e, c=c: e.transpose(out=MBb[:, c * 128:(c + 1) * 128],
                                                           in_=xn[:, c * 128:(c + 1) * 128], identity=ident_b[:]),
                          r=["xn"], w=["MB"])
                for c in range(8):
                    tr.op("dve", lambda e, c=c, tt=tt: e.tensor_scalar(
                        out=hT[:, c, tt * 128:(tt + 1) * 128], in0=MBb[:, c * 128:(c + 1) * 128],
                        scalar1=A_m[:, c:c + 1], scalar2=B_m[:, c:c + 1], op0=ALU.mult, op1=ALU.add),
                        r=["MB"], w=[("hT", tt)])

            hTk = [("hT", t) for t in range(4)]

            def inproj_qk(G, oc):
                bank, bk = gbank()
                for kc in range(8):
                    tr.op("pe", lambda e, oc=oc, kc=kc, bank=bank: e.matmul(
                        bank[:, :], lhsT=win[:, kc, oc * 128:(oc + 1) * 128], rhs=hT[:, kc, :],
                        start=(kc == 0), stop=(kc == 7)), r=["win"] + hTk, w=[bk])
                for hh in range(2):
                    h = (oc % 4) * 2 + hh
                    rows = slice(hh * 64, (hh + 1) * 64)
                    if oc < 4:
                        tr.op("dve", lambda e, h=h, rows=rows, bank=bank: e.tensor_copy(
                            out=QA[0:64, h, :], in_=bank[rows, :]), r=[bk], w=[("QA", h)])
                    else:
                        tr.op("dve", lambda e, h=h, rows=rows, bank=bank, G=G: e.tensor_copy(
                            out=KT[0:64, h, G * 512:(G + 1) * 512], in_=bank[rows, :]), r=[bk], w=[("KT", h, G)])
                if oc >= 4:
                    tr.op("dve", lambda e, bank=bank: e.tensor_reduce(
                        out=ksum[:], in_=bank[:, :].rearrange("p (a b) -> p a b", a=2), axis=AX.X, op=ALU.add),
                        r=[bk], w=["ksum"])
                    for hh in range(2):
                        h = (oc % 4) * 2 + hh
                        tr.op("dve", lambda e, h=h, hh=hh, G=G: e.tensor_copy(
                            out=kmT[0:64, h, 2 * G:2 * G + 2], in_=ksum[hh * 64:(hh + 1) * 64, :]),
                            r=["ksum"], w=[("kmT", h)])

            def inproj_v(G, tt):
                ts_ = slice(tt * 128, (tt + 1) * 128)
                gt = G * 4 + tt
                bank, bk = gbank()
                for kc in range(8):
                    tr.op("pe", lambda e, kc=kc, bank=bank, ts_=ts_: e.matmul(
                        bank[:, :], lhsT=hT[:, kc, ts_], rhs=win[:, kc, 1024:1536],
                        start=(kc == 0), stop=(kc == 7)), r=["win", ("hT", tt)], w=[bk])
                tr.op("dve", lambda e, bank=bank, gt=gt: e.tensor_copy(
                    out=V[:, gt, :, 0:64], in_=bank[:, :].rearrange("p (h d) -> p h d", h=NH)),
                    r=[bk], w=[("V", gt)])

            def sgu(G, tt):
                ts_ = slice(tt * 128, (tt + 1) * 128)
                for which in range(2):
                    dst = uh if which == 0 else gvg
                    dk = "uh" if which == 0 else "gvg"
                    co = 1536 + which * 512
                    bank, bk = gbank()
                    for kc in range(8):
                        tr.op("pe", lambda e, kc=kc, bank=bank, ts_=ts_, co=co: e.matmul(
                            bank[:, :], lhsT=hT[:, kc, ts_], rhs=win[:, kc, co:co + 512],
                            start=(kc == 0), stop=(kc == 7)), r=["win", ("hT", tt)], w=[bk])
                    tr.op("act", lambda e, bank=bank, dst=dst: e.activation(out=dst[:], in_=bank[:, :], func=AF.Identity,
                                                                            scale=0.5), r=[bk], w=[dk])
                    tr.op("act", lambda e, bank=bank: e.activation(out=x2[:], in_=bank[:, :], func=AF.Square),
                          r=[bk], w=["x2"])
                    tr.op("pool", lambda e: e.tensor_scalar(out=x2[:], in0=x2[:], scalar1=0.044715, scalar2=1.0,
                                                            op0=ALU.mult, op1=ALU.add), r=["x2"], w=["x2"])
                    tr.op("pool", lambda e, dst=dst: e.tensor_tensor(out=x2[:], in0=x2[:], in1=dst[:], op=ALU.mult),
                          r=["x2", dk], w=["x2"])
                    tr.op("act", lambda e: e.activation(out=x2[:], in_=x2[:], func=AF.Tanh, scale=2.0 * GELU_C),
                          r=["x2"], w=["x2"])
                    tr.op("dve", lambda e, dst=dst: e.scalar_tensor_tensor(
                        out=dst[:], in0=x2[:], scalar=1.0, in1=dst[:], op0=ALU.add, op1=ALU.mult),
                        r=["x2", dk], w=[dk])
                for g in range(4):
                    tr.op("dve", lambda e, g=g: e.bn_stats(out=bnst[:, g, :], in_=gvg[:, g * 128:(g + 1) * 128]),
                          r=["gvg"], w=["bnst"])
                for g in range(4):
                    tr.op("dve", lambda e, g=g: e.bn_aggr(out=mv[:, g, :], in_=bnst[:, g, :]), r=["bnst"], w=["mv"])
                tr.op("pool", lambda e: e.tensor_scalar(out=st[:, 4:8], in0=mv[:, :, 1], scalar1=1.0, scalar2=EPS,
                                                        op0=ALU.mult, op1=ALU.add), r=["mv"], w=["st4"])
                tr.op("pool", lambda e: e.tensor_tensor(out=st[:, 4:8], in0=st[:, 4:8], in1=mhalf[:, 0:4], op=ALU.pow),
                      r=["st4"], w=["st4"])
                for g in range(4):
                    tr.op("dve", lambda e, g=g: e.tensor_scalar(
                        out=gvg[:, g * 128:(g + 1) * 128], in0=gvg[:, g * 128:(g + 1) * 128],
                        scalar1=mv[:, g, 0:1], scalar2=st[:, 4 + g:5 + g], op0=ALU.subtract, op1=ALU.mult),
                        r=["gvg", "mv", "st4"], w=["gvg"])
                tr.op("dve", lambda e: e.tensor_tensor(out=vgn[:], in0=gvg[:], in1=gnorm[:], op=ALU.mult),
                      r=["gvg", "gnorm"], w=["vgn"])
                bank, bk = gbank()
                for g in range(4):
                    tr.op("pe", lambda e, g=g, bank=bank: e.matmul(
                        bank[:, g * 128:(g + 1) * 128], lhsT=wsT[:, g, :], rhs=vgn[:, g * 128:(g + 1) * 128],
                        start=True, stop=True), r=["wsT", "vgn"], w=[bk])
                for g in range(4):
                    tr.op("dve", lambda e, g=g, bank=bank: e.scalar_tensor_tensor(
                        out=y2[:, g * 128:(g + 1) * 128], in0=bank[:, g * 128:(g + 1) * 128],
                        scalar=bsgu[:, g:g + 1], in1=uh[:, g * 128:(g + 1) * 128], op0=ALU.add, op1=ALU.mult),
                        r=[bk, "uh", "bsgu"], w=["y2"])
                tr.op("act", lambda e: e.activation(out=junk[:], in_=y2[:], func=AF.Square,
                                                    accum_out=st[:, 8:9]), r=["y2"], w=["junk", "st8"])
                rstd_ops("st8", st[:, 8:9], st[:, 9:10], "st9", 512)
                tr.op("dve", lambda e, tt=tt: e.tensor_scalar(out=sgn[:, tt, :], in0=y2[:], scalar1=st[:, 9:10],
                                                               scalar2=None, op0=ALU.mult),
                      r=["y2", "st9"], w=[("sgn", tt)])

            def gate(G):
                if G < 2:
                    return
                for tt in range(4):
                    j = 2 * G + tt // 2
                    ts_ = slice(tt * 128, (tt + 1) * 128)
                    for h in range(NH):
                        tr.op("pe", lambda e, h=h, ts_=ts_: e.matmul(
                            MF[:, 384 + h * 16:384 + (h + 1) * 16], lhsT=QA[0:64, h, ts_], rhs=kmT[:, h, :],
                            start=True, stop=True), r=[("QA", h), ("kmT", h)], w=["MFg"])
                    tr.op("dve", lambda e, j=j: e.tensor_copy(
                        out=gsb[:, :, 0:j], in_=MF[:, 384:512].rearrange("p (h n) -> p h n", h=NH)[:, :, 0:j]),
                        r=["MFg"], w=["gsb"])
                    for h in range(NH):
                        tr.op("dve", lambda e, h=h: e.max(out=top8[:, h, :], in_=gsb[:, h, :]), r=["gsb"], w=["top8"])
                    for h in range(NH):
                        tr.op("dve", lambda e, h=h, j=j: e.tensor_scalar(
                            out=mb[:, h, 64:64 + j], in0=gsb[:, h, 0:j], scalar1=top8[:, h, 2:3], scalar2=NEGM,
                            op0=ALU.is_lt, op1=ALU.mult), r=["gsb", "top8"], w=["mb"])
                    for h in range(NH):
                        tr.op("pe", lambda e, h=h: e.transpose(out=MBb[0:80, h * 128:(h + 1) * 128],
                                                               in_=mb[:, h, :], identity=ident_b[:]),
                              r=["mb"], w=["MB"])
                    tr.op("dve", lambda e, ts_=ts_: e.tensor_copy(
                        out=QA[64:80, :, ts_], in_=MBb[64:80, :].rearrange("p (h t) -> p h t", h=NH)),
                        r=["MB"], w=["QAm"])

            def attn_items(G):
                return [(G, h, kt) for h in range(NH) for kt in range(4 * G + 4)]

            def qk(G, h, kt):
                c0 = 0 if kt <= 4 * G else (kt - 4 * G) * 128
                si = sctr[0] % 2
                sctr[0] += 1
                Sb = PS[2 + si]
                sk = ("ps", 2 + si)
                tri = kt >= 4 * G
                tr.op("pe", lambda e, h=h, kt=kt, c0=c0, Sb=Sb, tri=tri: e.matmul(
                    Sb[:, c0:512], lhsT=KT[0:80, h, kt * 128:(kt + 1) * 128], rhs=QA[0:80, h, c0:512],
                    start=True, stop=(not tri)),
                    r=[("KT", h, kt // 4), ("KTo", h), ("QA", h), "QAm"], w=[sk])
                if tri:
                    ct0 = (kt - 4 * G) * 128
                    tr.op("pe", lambda e, Sb=Sb, ct0=ct0: e.matmul(
                        Sb[:, ct0:ct0 + 128], lhsT=ident_b[:], rhs=tribias[:], start=False, stop=True),
                        r=[], w=[sk])
                return (Sb, sk, c0)

            def exp_pv(G, h, kt, sinfo, idx):
                Sb, sk, c0 = sinfo
                nkt = 4 * G + 4
                pi = idx % 3
                Ob = PS[4 + h % 2]
                ok = ("ps", 4 + h % 2)
                tr.op("act", lambda e, Sb=Sb, c0=c0, pi=pi: e.activation(
                    out=Pb[pi][:, c0:512], in_=Sb[:, c0:512], func=AF.Exp, scale=0.125),
                    r=[sk], w=[("Pb", pi)])

                def pv():
                    tr.op("pe", lambda e, h=h, kt=kt, c0=c0, pi=pi, Ob=Ob, nkt=nkt: e.matmul(
                        Ob[0:65, c0:512], lhsT=V[:, kt, h, :], rhs=Pb[pi][:, c0:512],
                        start=(kt == 0), stop=(kt == nkt - 1)),
                        r=[("V", kt), "Vones", ("Pb", pi)], w=[ok])
                    if kt == nkt - 1:
                        head_finish(G, h, Ob, ok)
                return pv

            def head_finish(G, h, Ob, ok):
                tr.op("dve", lambda e, Ob=Ob: e.tensor_copy(out=OT[:], in_=Ob[0:65, :]), r=[ok], w=["OT"])
                for tt in range(4):
                    tr.op("pe", lambda e, tt=tt: e.transpose(out=MF[:, tt * 65:(tt + 1) * 65],
                                                             in_=OT[:, tt * 128:(tt + 1) * 128],
                                                             identity=ident_f[0:65, 0:65]),
                          r=["OT"], w=["MFo"])
                MFo = MF[:, 0:260].rearrange("p (t d) -> p t d", t=4)
                tr.op("dve", lambda e, MFo=MFo: e.reciprocal(out=rec[:], in_=MFo[:, :, 64]), r=["MFo"], w=["rec"])
                attn = attn2[G % 2]
                tr.op("dve", lambda e, MFo=MFo, h=h, attn=attn: e.tensor_tensor(
                    out=attn[:, :, h * 64:(h + 1) * 64], in0=MFo[:, :, 0:64],
                    in1=rec[:].unsqueeze(2).to_broadcast([128, 4, 64]), op=ALU.mult),
                    r=["MFo", "rec"], w=[("attn", G % 2)])

            def epilogue(G, tt):
                attn = attn2[G % 2]
                ak = ("attn", G % 2)
                tr.op("act", lambda e, tt=tt, attn=attn: e.activation(out=junk[:], in_=attn[:, tt, :], func=AF.Square,
                                                                      accum_out=st[:, 10:11]), r=[ak], w=["junk", "st10"])
                rstd_ops("st10", st[:, 10:11], st[:, 11:12], "st11", 512)
                tr.op("dve", lambda e, tt=tt, attn=attn: e.tensor_scalar(out=an[:, 0:512], in0=attn[:, tt, :],
                                                                         scalar1=st[:, 11:12], scalar2=None, op0=ALU.mult),
                      r=[ak, "st11"], w=["an"])
                tr.op("dve", lambda e, tt=tt: e.tensor_copy(out=an[:, 512:1024], in_=sgn[:, tt, :]),
                      r=[("sgn", tt)], w=["an"])
                for c in range(8):
                    tr.op("pe", lambda e, c=c: e.transpose(out=MBb[:, c * 128:(c + 1) * 128],
                                                           in_=an[:, c * 128:(c + 1) * 128], identity=ident_b[:]),
                          r=["an"], w=["MB"])
                tr.op("dve", lambda e, tt=tt: e.tensor_tensor(
                    out=mixT[:, :, :], in0=MBb[:, :].rearrange("p (c t) -> p c t", c=8),
                    in1=gmix[:].unsqueeze(2).to_broadcast([128, 8, 128]), op=ALU.mult),
                    r=["MB"], w=["mixT"])
                tr.dma("sp", mix_d[G * 4 + tt].rearrange("p (c t) -> p c t", c=8), mixT[:], r=["mixT"], w=["mixd"])

            def attention(G, side):
                items = attn_items(G)
                n_it = len(items)
                sin = {}
                for i in range(min(LOOK, n_it)):
                    sin[i] = qk(*items[i])
                sops = tr.capture(side)
                per = -(-len(sops) // max(1, n_it - 2))
                for i in range(n_it):
                    if LOOK == 0:
                        sin[i] = qk(*items[i])
                    pv = exp_pv(*items[i], sin.pop(i), i)
                    if LOOK > 0 and i + LOOK < n_it:
                        sin[i + LOOK] = qk(*items[i + LOOK])
                    pv()
                    tr.flush(sops, per)
                tr.flush(sops, len(sops))

            if INTERLEAVE:
                for tt in range(4):
                    prologue_a(0, tt)
                for oc in range(8):
                    inproj_qk(0, oc)
                for tt in range(4):
                    inproj_v(0, tt)
                gate(0)
                for G in range(8):
                    side = []
                    for t in range(4):
                        if G > 0:
                            side.append(lambda t=t, G=G: epilogue(G - 1, t))
                        side.append(lambda t=t, G=G: sgu(G, t))
                    if G + 1 < 8:
                        side += [(lambda t=t, G=G: prologue_a(G + 1, t)) for t in range(4)]
                    attention(G, side)
                    if G + 1 < 8:
                        for oc in range(8):
                            inproj_qk(G + 1, oc)
                        for tt in range(4):
                            inproj_v(G + 1, tt)
                        gate(G + 1)
                for tt in range(4):
                    epilogue(7, tt)
            else:
                for G in range(8):
                    for tt in range(4):
                        prologue_a(G, tt)
                    for oc in range(8):
                        inproj_qk(G, oc)
                    for tt in range(4):
                        inproj_v(G, tt)
                        sgu(G, tt)
                    gate(G)
                    attention(G, [])
                    for tt in range(4):
                        epilogue(G, tt)
            tr.op("sp", None, r=["mixd"])
            tr.emit(nc, p1, "p1")

        with ExitStack() as p2:
            tr = Tr()
            wout = sb(p2, "wout", [128, 8, D], BF16)
            wgu = sb(p2, "wgu", [128, 8, 2 * DFF], BF16)
            wdn = sb(p2, "wdn", [128, NFC, D], BF16)
            actT = sb(p2, "actT", [128, NFC, 256], BF16)
            hb = sb(p2, "hb", [128, 8, 256], BF16)
            xt2 = [sb(p2, f"xt2{i}", [128, D]) for i in range(2)]
            xn2 = sb(p2, "xn2", [128, D], BF16)
            junk2 = sb(p2, "junk2", [128, D], BF16)
            ytmp = sb(p2, "ytmp", [128, 512])
            sg = sb(p2, "sg", [128, 256])
            st2 = sb(p2, "st2", [128, 16])
            GM = sb(p2, "GM", [128, D]); GF = sb(p2, "GF", [128, D])
            tr.dma("sp", GM[:], gpostm_d.partition_broadcast(128), w=["GM"])
            tr.dma("sp", GF[:], gpostf_d.partition_broadcast(128), w=["GF"])

            def sink2(j, mc, mk):
                G_ = GM if j < 6 else GF
                gk = "GM" if j < 6 else "GF"
                hs = slice((j % 2) * 512, (j % 2 + 1) * 512)
                tr.op("dve", lambda e, G_=G_, hs=hs, mc=mc: e.tensor_tensor(
                    out=G_[:, hs], in0=G_[:, hs], in1=mc[:], op=ALU.mult), r=[mk, gk], w=[gk])
            ada_section(tr, p2, [4, 5, 10, 11], 1, sink2, "a2")
            MBb = PS[6][:].bitcast(BF16)

            def rstd2(src_key, src_ap, dst_ap, dst_key, n):
                tr.op("pool", lambda e: e.tensor_scalar(out=dst_ap, in0=src_ap, scalar1=1.0 / n, scalar2=EPS,
                                                        op0=ALU.mult, op1=ALU.add), r=[src_key], w=[dst_key])
                tr.op("pool", lambda e: e.tensor_tensor(out=dst_ap, in0=dst_ap, in1=mhalf[:, 0:1], op=ALU.pow),
                      r=[dst_key], w=[dst_key])

            wout_v = wout_d.rearrange("(kc p) n -> p kc n", p=128)
            wgu_v = wgu_d.rearrange("(kc p) n -> p kc n", p=128)
            wdn_v = wdn_d.rearrange("(fc p) n -> p fc n", p=128)
            tr.dma("pool", wout[:], wout_v, w=["wout"])
            for fc in range(NFC):
                for pt in range(2):
                    co = pt * DFF + fc * 128
                    tr.dma("pool", wgu[:, :, co:co + 128], wgu_v[:, :, co:co + 128], w=[("wgu", fc)])
            for i in range(2):
                tr.dma("pool", wdn[:, i * 11:(i + 1) * 11, :], wdn_v[:, i * 11:(i + 1) * 11, :], w=["wdn"])
            bctr = [0]

            def pbank():
                i = bctr[0] % 6
                bctr[0] += 1
                return PS[i], ("ps", i)

            for G in range(16):
                for tt in range(2):
                    tr.dma("sp", hb[:, :, tt * 128:(tt + 1) * 128],
                           mix_d[G * 2 + tt].rearrange("p (c t) -> p c t", c=8), w=[("hb", tt)])
                for tt in range(2):
                    ts_ = slice(tt * 128, (tt + 1) * 128)
                    tok = slice(G * 256 + tt * 128, G * 256 + (tt + 1) * 128)
                    xt = xt2[tt]
                    xk = ("xt", tt)
                    tr.dma("sp", xt[:], x_d[tok, :], w=[xk])
                    for half in range(2):
                        hs = slice(half * 512, (half + 1) * 512)
                        bank, bk = pbank()
                        for kc in range(8):
                            tr.op("pe", lambda e, kc=kc, bank=bank, ts_=ts_, hs=hs: e.matmul(
                                bank[:, :], lhsT=hb[:, kc, ts_], rhs=wout[:, kc, hs], start=(kc == 0), stop=(kc == 7)),
                                r=["wout", ("hb", tt)], w=[bk])
                        tr.op("act", lambda e, bank=bank, half=half: e.activation(
                            out=junk2[:, 0:512], in_=bank[:, :], func=AF.Square, accum_out=st2[:, half:half + 1]),
                            r=[bk], w=["junk2", ("ssq", half)])
                        if half == 0:
                            b0, bk0 = bank, bk
                        else:
                            b1, bk1 = bank, bk
                    tr.op("pool", lambda e: e.tensor_tensor(out=st2[:, 2:3], in0=st2[:, 0:1], in1=st2[:, 1:2], op=ALU.add),
                          r=[("ssq", 0), ("ssq", 1)], w=["s2"])
                    rstd2("s2", st2[:, 2:3], st2[:, 3:4], "s3", D)
                    for half, (bank, bk) in enumerate(((b0, bk0), (b1, bk1))):
                        hs = slice(half * 512, (half + 1) * 512)
                        tr.op("dve", lambda e, bank=bank, hs=hs: e.scalar_tensor_tensor(
                            out=ytmp[:], in0=bank[:, :], scalar=st2[:, 3:4], in1=GM[:, hs], op0=ALU.mult, op1=ALU.mult),
                            r=[bk, "s3", "GM"], w=["ytmp"])
                        tr.op("dve", lambda e, xt=xt, hs=hs: e.tensor_tensor(out=xt[:, hs], in0=xt[:, hs], in1=ytmp[:],
                                                                             op=ALU.add), r=["ytmp", xk], w=[xk])
                    tr.op("act", lambda e, xt=xt: e.activation(out=junk2[:], in_=xt[:], func=AF.Square,
                                                               accum_out=st2[:, 4:5]), r=[xk], w=["junk2", "s4"])
                    rstd2("s4", st2[:, 4:5], st2[:, 5:6], "s5", D)
                    tr.op("dve", lambda e, xt=xt: e.tensor_scalar(out=xn2[:], in0=xt[:], scalar1=st2[:, 5:6], scalar2=None,
                                                                  op0=ALU.mult), r=[xk, "s5"], w=["xn2"])
                    for c in range(8):
                        tr.op("pe", lambda e, c=c: e.transpose(out=MBb[:, c * 128:(c + 1) * 128],
                                                               in_=xn2[:, c * 128:(c + 1) * 128], identity=ident_b[:]),
                              r=["xn2"], w=["MB"])
                    for c in range(8):
                        tr.op("dve", lambda e, c=c, ts_=ts_: e.tensor_scalar(
                            out=hb[:, c, ts_], in0=MBb[:, c * 128:(c + 1) * 128],
                            scalar1=A_f[:, c:c + 1], scalar2=B_f[:, c:c + 1], op0=ALU.mult, op1=ALU.add),
                            r=["MB"], w=[("hb", tt)])
                for fc in range(NFC):
                    gb, gk = pbank()
                    ub, uk = pbank()
                    for pt, bank, bk in ((0, gb, gk), (1, ub, uk)):
                        co = pt * DFF + fc * 128
                        for kc in range(8):
                            tr.op("pe", lambda e, kc=kc, bank=bank, co=co: e.matmul(
                                bank[:, 0:256], lhsT=wgu[:, kc, co:co + 128], rhs=hb[:, kc, :],
                                start=(kc == 0), stop=(kc == 7)),
                                r=[("wgu", fc), ("hb", 0), ("hb", 1)], w=[bk])
                    tr.op("act", lambda e, gb=gb: e.activation(out=sg[:], in_=gb[:, 0:256], func=AF.Silu),
                          r=[gk], w=["sg"])
                    tr.op("dve", lambda e, ub=ub, fc=fc: e.tensor_tensor(out=actT[:, fc, :], in0=sg[:], in1=ub[:, 0:256],
                                                                         op=ALU.mult), r=["sg", uk], w=[("actT", fc)])
                for tt in range(2):
                    ts_ = slice(tt * 128, (tt + 1) * 128)
                    tok = slice(G * 256 + tt * 128, G * 256 + (tt + 1) * 128)
                    xt = xt2[tt]
                    xk = ("xt", tt)
                    banks = []
                    for half in range(2):
                        hs = slice(half * 512, (half + 1) * 512)
                        bank, bk = pbank()
                        banks.append((bank, bk))
                        for fc in range(NFC):
                            tr.op("pe", lambda e, fc=fc, bank=bank, ts_=ts_, hs=hs: e.matmul(
                                bank[:, :], lhsT=actT[:, fc, ts_], rhs=wdn[:, fc, hs],
                                start=(fc == 0), stop=(fc == NFC - 1)),
                                r=["wdn", ("actT", fc)], w=[bk])
                        tr.op("act", lambda e, bank=bank, half=half: e.activation(
                            out=junk2[:, 0:512], in_=bank[:, :], func=AF.Square, accum_out=st2[:, 8 + half:9 + half]),
                            r=[bk], w=["junk2", ("ssq2", half)])
                    tr.op("pool", lambda e: e.tensor_tensor(out=st2[:, 10:11], in0=st2[:, 8:9], in1=st2[:, 9:10], op=ALU.add),
                          r=[("ssq2", 0), ("ssq2", 1)], w=["s10"])
                    rstd2("s10", st2[:, 10:11], st2[:, 11:12], "s11", D)
                    for half, (bank, bk) in enumerate(banks):
                        hs = slice(half * 512, (half + 1) * 512)
                        tr.op("dve", lambda e, bank=bank, hs=hs: e.scalar_tensor_tensor(
                            out=ytmp[:], in0=bank[:, :], scalar=st2[:, 11:12], in1=GF[:, hs], op0=ALU.mult, op1=ALU.mult),
                            r=[bk, "s11", "GF"], w=["ytmp"])
                        tr.op("dve", lambda e, xt=xt, hs=hs: e.tensor_tensor(out=xt[:, hs], in0=xt[:, hs], in1=ytmp[:],
                                                                             op=ALU.add), r=["ytmp", xk], w=[xk])
                    tr.dma("sp", out_d[tok, :], xt[:], r=[xk], w=["outd"])
            tr.op("sp", None, r=["outd"])
            tr.emit(nc, p2, "p2")
    return nc


def _consts():
    ident = np.eye(128, dtype=np.float32)
    p = np.arange(128)
    tri01 = (p[:, None] <= p[None, :]).astype(np.float32)
    onehot = (np.arange(S)[None, :] // 256 == np.arange(16)[:, None]).astype(np.float32)
    return ident, tri01, onehot


def kernel(x, c, w_ada, b_ada, g_pre_mix, g_post_mix, w_in, g_sgu_norm, w_sgu, b_sgu,
           g_attn_out, g_sgu_out, w_out, g_pre_ffn, g_post_ffn, w_gate_up, w_down):
    f = lambda a: np.ascontiguousarray(np.asarray(a, dtype=np.float32))
    x = f(x); c = f(c)
    ident, tri01, onehot = _consts()
    pp = lambda v: f(np.asarray(v).reshape(-1, 128).T)
    shared = {
        "w_ada": f(w_ada[0]), "b_ada": f(b_ada[0]).reshape(1, -1),
        "gpm_t": pp(g_pre_mix[0]), "gpf_t": pp(g_pre_ffn[0]),
        "gmix_t": f(np.concatenate([pp(g_attn_out[0]), pp(g_sgu_out[0])], axis=1)),
        "g_post_mix": f(g_post_mix[0]).reshape(1, -1), "g_post_ffn": f(g_post_ffn[0]).reshape(1, -1),
        "g_sgu_norm": f(g_sgu_norm[0]).reshape(1, -1),
        "bsgu_t": f(np.asarray(b_sgu[0]).T),
        "wsgu_t": f(np.transpose(np.asarray(w_sgu[0]), (2, 0, 1))),
        "w_in": f(w_in[0]), "w_out": f(w_out[0]), "w_gate_up": f(w_gate_up[0]), "w_down": f(w_down[0]),
        "ident": ident, "tri01": tri01, "onehot": onehot,
    }
    in_maps = []
    for b in range(8):
        m = dict(shared)
        m["x"] = x[b]
        m["ct"] = pp(c[b])
        in_maps.append(m)
    nc = build_program()
    res = run_bass_kernel_spmd(nc, in_maps, core_ids=list(range(8)))
    return np.stack([np.asarray(r["out"], dtype=np.float32) for r in res.results], axis=0)
```

```python
from contextlib import ExitStack
import numpy as np
import concourse.bass as bass
import concourse.mybir as mybir
from concourse.bass_utils import run_bass_kernel_spmd

F32, BF16 = mybir.dt.float32, mybir.dt.bfloat16
AF = mybir.ActivationFunctionType
ALU = mybir.AluOpType
AX = mybir.AxisListType

S, D, NH, HD = 4096, 1024, 8, 64
DFF = 2816
NFC = DFF // 128
EPS = 1e-6
NEGM = -30000.0
GELU_C = 0.7978845608028654
STRICT = False
LOOK = 1
INTERLEAVE = True


class Op:
    __slots__ = ("eng", "fn", "deps", "signal", "cnt", "dma", "sem", "target")


class Tr:
    ENG = ("pe", "act", "dve", "pool", "sp")

    def __init__(self):
        self.streams = {e: [] for e in self.ENG}
        self.last_w = {}
        self.readers = {}

    def _add(self, eng, fn, r, w, dma):
        op = Op()
        op.eng, op.fn, op.dma, op.signal, op.cnt, op.sem, op.target = eng, fn, dma, False, 0, None, 0
        deps = set()
        for k in r:
            p = self.last_w.get(k)
            if p is not None and (STRICT or not (eng == "pe" and p.eng == "pe" and not p.dma)):
                deps.add(p)
        for k in w:
            p = self.last_w.get(k)
            if p is not None and (STRICT or p.eng != eng or p.dma or dma):
                deps.add(p)
            lastr = {}
            for q in self.readers.get(k, ()):
                if STRICT or q.eng != eng or q.dma or dma:
                    if q.dma or STRICT:
                        deps.add(q)
                    else:
                        lastr[q.eng] = q
            deps.update(lastr.values())
        op.deps = deps
        for d in deps:
            d.signal = True
        for k in r:
            self.readers.setdefault(k, []).append(op)
        for k in w:
            self.last_w[k] = op
            self.readers[k] = []
        self.streams[eng].append(op)
        return op

    cap = None

    def op(self, eng, fn, r=(), w=()):
        if self.cap is not None:
            self.cap.append((eng, fn, tuple(r), tuple(w), False))
            return None
        return self._add(eng, fn, r, w, False)

    def dma(self, q, out, in_, r=(), w=()):
        fn = lambda e, o=out, i=in_: e.dma_start(out=o, in_=i)
        if self.cap is not None:
            self.cap.append((q, fn, tuple(r), tuple(w), True))
            return None
        return self._add(q, fn, r, w, True)

    def capture(self, fns):
        self.cap = []
        for f in fns:
            f()
        ops, self.cap = self.cap, None
        return ops

    def flush(self, ops, n):
        for _ in range(min(n, len(ops))):
            eng, fn, r, w, dma = ops.pop(0)
            self._add(eng, fn, r, w, dma)

    def emit(self, nc, stack, tag, ndma_sems=12):
        engsem = {e: stack.enter_context(nc.semaphore(f"{tag}_es_{e}")) for e in ("pe", "act", "dve", "pool")}
        dsems = {q: [stack.enter_context(nc.semaphore(f"{tag}_ds_{q}{i}")) for i in range(ndma_sems)]
                 for q in ("sp", "pool", "act") if any(o.dma for o in self.streams[q])}
        for e in self.ENG:
            c = 0
            nd = 0
            for o in self.streams[e]:
                if o.dma:
                    o.sem = dsems[e][nd % ndma_sems]
                    o.target = 16 * (nd // ndma_sems + 1)
                    nd += 1
                elif o.signal:
                    c += 1
                    o.cnt = c
        block = stack.enter_context(nc.Block())

        def run(ename, eng):
            waited = {}
            for o in self.streams[ename]:
                need = {}
                for d in o.deps:
                    if d.dma:
                        s_, v_ = d.sem, d.target
                    else:
                        s_, v_ = engsem[d.eng], d.cnt
                    if need.get(id(s_), (None, 0))[1] < v_:
                        need[id(s_)] = (s_, v_)
                if o.dma and o.target > 16:
                    if need.get(id(o.sem), (None, 0))[1] < o.target - 16:
                        need[id(o.sem)] = (o.sem, o.target - 16)
                for sid, (s_, v_) in need.items():
                    if waited.get(sid, 0) < v_:
                        eng.wait_ge(s_, v_)
                        waited[sid] = v_
                if o.fn is None:
                    continue
                ins = o.fn(eng)
                if o.dma:
                    ins.then_inc(o.sem, 16)
                elif o.signal:
                    ins.then_inc(engsem[ename], 1)

        @block.tensor
        def _(e):
            run("pe", e)

        @block.scalar
        def _(e):
            run("act", e)

        @block.vector
        def _(e):
            run("dve", e)

        @block.gpsimd
        def _(e):
            run("pool", e)

        @block.sync
        def _(e):
            run("sp", e)


def build_program():
    nc = bass.Bass("TRN2", target_bir_lowering=False)
    dt = lambda n, s, d=F32, k="ExternalInput": nc.dram_tensor(n, s, d, kind=k).ap()
    x_d = dt("x", [S, D])
    ct_d = dt("ct", [128, 8])
    wada_d = dt("w_ada", [D, 6 * D])
    bada_d = dt("b_ada", [1, 6 * D])
    gpm_d = dt("gpm_t", [128, 8])
    gpf_d = dt("gpf_t", [128, 8])
    gmix_d = dt("gmix_t", [128, 8])
    gpostm_d = dt("g_post_mix", [1, D])
    gpostf_d = dt("g_post_ffn", [1, D])
    gnorm_d = dt("g_sgu_norm", [1, 512])
    bsgu_d = dt("bsgu_t", [128, 4])
    wsgu_d = dt("wsgu_t", [128, 4, 128])
    win_d = dt("w_in", [D, 2560])
    wout_d = dt("w_out", [D, D])
    wgu_d = dt("w_gate_up", [D, 2 * DFF])
    wdn_d = dt("w_down", [DFF, D])
    ident_d = dt("ident", [128, 128])
    tri_d = dt("tri01", [128, 128])
    oneh_d = dt("onehot", [16, S])
    out_d = dt("out", [S, D], F32, "ExternalOutput")
    mix_d = nc.dram_tensor("mixscr", [32, 128, 8 * 128], BF16).ap()

    with ExitStack() as gl:
        sb = lambda st, n, s, d=F32: st.enter_context(nc.sbuf_tensor(n, s, d))
        PS = [gl.enter_context(nc.psum_tensor(f"ps{i}", [128, 512], F32)) for i in range(8)]
        ident_f = sb(gl, "ident_f", [128, 128])
        ident_b = sb(gl, "ident_b", [128, 128], BF16)
        tri01 = sb(gl, "tri01s", [128, 128])
        tribias = sb(gl, "tribias", [128, 128], BF16)
        mhalf = sb(gl, "mhalf", [128, 8])
        A_m = sb(gl, "A_m", [128, 8]); B_m = sb(gl, "B_m", [128, 8])
        A_f = sb(gl, "A_f", [128, 8]); B_f = sb(gl, "B_f", [128, 8])
        gmix = sb(gl, "gmix", [128, 8])
        crep = sb(gl, "crep", [128, 8, 128], BF16)
        wada_v = wada_d.rearrange("(kc p) n -> p kc n", p=128)

        def ada_section(tr, stk, js, nslots, sink, tg, bufs=None):
            if bufs is None:
                wa = [sb(stk, f"{tg}wa{i}", [128, 8, 512], BF16)[:] for i in range(nslots)]
                bb = [sb(stk, f"{tg}bb{i}", [128, 512])[:] for i in range(nslots)]
                modc = [sb(stk, f"{tg}modc{i}", [128, 512])[:] for i in range(nslots)]
                wak = [[("wa", i)] for i in range(nslots)]
                bbk = [[("bb", i)] for i in range(nslots)]
                mck = [[("modc", i)] for i in range(nslots)]
            else:
                wa, wak, bb, bbk, modc, mck = [[b] for b in bufs]
            for n_, j in enumerate(js):
                sl = n_ % nslots
                cs = slice(j * 512, (j + 1) * 512)
                tr.dma("pool", wa[sl], wada_v[:, :, cs], w=wak[sl])
                tr.dma("sp", bb[sl], bada_d[:, cs].partition_broadcast(128), w=bbk[sl])
                bank = PS[sl]
                for kc in range(8):
                    tr.op("pe", lambda e, kc=kc, sl=sl, bank=bank: e.matmul(
                        bank[:, :], lhsT=crep[:, kc, :], rhs=wa[sl][:, kc, :], start=(kc == 0), stop=(kc == 7)),
                        r=["crep"] + wak[sl], w=[("ps", sl)])
                tr.op("dve", lambda e, sl=sl, bank=bank: e.tensor_tensor(
                    out=modc[sl], in0=bank[:, :], in1=bb[sl], op=ALU.add),
                    r=[("ps", sl)] + bbk[sl], w=mck[sl])
                sink(j, modc[sl], mck[sl])

        with ExitStack() as p0:
            tr = Tr()
            ctile = sb(p0, "ctile", [128, 8]); cact = sb(p0, "cact", [128, 8])
            gpm = sb(p0, "gpm", [128, 8]); gpf = sb(p0, "gpf", [128, 8])
            dtmp = sb(p0, "dtmp", [128, 4, 128])
            pp = sb(p0, "pp", [128, 4, 8])
            tr.dma("sp", ident_f[:], ident_d[:, :], w=["ident_f"])
            tr.dma("sp", tri01[:], tri_d[:, :], w=["tri01"])
            tr.dma("sp", ctile[:], ct_d[:, :], w=["ctile"])
            tr.dma("sp", gpm[:], gpm_d[:, :], w=["gpm"])
            tr.dma("sp", gpf[:], gpf_d[:, :], w=["gpf"])
            tr.dma("sp", gmix[:], gmix_d[:, :], w=["gmix"])
            tr.op("dve", lambda e: e.tensor_copy(out=ident_b[:], in_=ident_f[:]), r=["ident_f"], w=["ident_b"])
            tr.op("dve", lambda e: e.tensor_scalar(out=tribias[:], in0=tri01[:], scalar1=-NEGM, scalar2=NEGM,
                                                   op0=ALU.mult, op1=ALU.add), r=["tri01"], w=["tribias"])
            tr.op("pool", lambda e: e.memset(mhalf[:], -0.5), w=["mhalf"])
            tr.op("act", lambda e: e.activation(out=cact[:], in_=ctile[:], func=AF.Silu), r=["ctile"], w=["cact"])
            tr.op("dve", lambda e: e.tensor_copy(out=crep[:], in_=cact[:].unsqueeze(2).to_broadcast([128, 8, 128])),
                  r=["cact"], w=["crep"])
            def sink0(j, mc, mk):
                v, half = j // 2, j % 2
                vi = {0: 0, 1: 1, 3: 2, 4: 3}[v]
                tr.op("dve", lambda e, mc=mc: e.tensor_tensor(
                    out=dtmp[:], in0=mc.rearrange("p (a b) -> p a b", a=4),
                    in1=ident_f[:].unsqueeze(1).to_broadcast([128, 4, 128]), op=ALU.mult),
                    r=list(mk) + ["ident_f"], w=["dtmp"])
                tr.op("dve", lambda e, vi=vi, half=half: e.tensor_reduce(
                    out=pp[:, vi, half * 4:(half + 1) * 4], in_=dtmp[:], axis=AX.X, op=ALU.add),
                    r=["dtmp"], w=["pp"])
            ada_section(tr, p0, [0, 1, 2, 3, 6, 7, 8, 9], 2, sink0, "a0")
            tr.op("dve", lambda e: e.scalar_tensor_tensor(out=A_m[:], in0=pp[:, 1, :], scalar=1.0, in1=gpm[:],
                                                          op0=ALU.add, op1=ALU.mult), r=["pp", "gpm"], w=["A_m"])
            tr.op("dve", lambda e: e.tensor_copy(out=B_m[:], in_=pp[:, 0, :]), r=["pp"], w=["B_m"])
            tr.op("dve", lambda e: e.scalar_tensor_tensor(out=A_f[:], in0=pp[:, 3, :], scalar=1.0, in1=gpf[:],
                                                          op0=ALU.add, op1=ALU.mult), r=["pp", "gpf"], w=["A_f"])
            tr.op("dve", lambda e: e.tensor_copy(out=B_f[:], in_=pp[:, 2, :]), r=["pp"], w=["B_f"])
            tr.op("sp", None, r=["crep", "A_m", "B_m", "A_f", "B_f", "gmix", "ident_b", "tribias", "mhalf"])
            tr.emit(nc, p0, "p0")

        with ExitStack() as p1:
            tr = Tr()
            win = sb(p1, "win", [128, 8, 2560], BF16)
            KT = sb(p1, "KT", [80, NH, S], BF16)
            V = sb(p1, "V", [128, 32, NH, 65], BF16)
            kmT = sb(p1, "kmT", [64, NH, 16], BF16)
            ksum = sb(p1, "ksum", [128, 2])
            hT = sb(p1, "hT", [128, 8, 512], BF16)
            QA = sb(p1, "QA", [80, NH, 512], BF16)
            xin = [sb(p1, f"xin{i}", [128, D]) for i in range(1)]
            xn = sb(p1, "xn", [128, D], BF16)
            st = sb(p1, "st", [128, 16])
            Pb = [sb(p1, f"Pb{i}", [128, 512], BF16) for i in range(3)]
            OT = sb(p1, "OT", [65, 512])
            rec = sb(p1, "rec", [128, 4])
            attn2 = [sb(p1, f"attn{i}", [128, 4, 512]) for i in range(2)]
            an = sb(p1, "an", [128, D], BF16)
            uh = sb(p1, "uh", [128, 512]); x2 = sb(p1, "x2", [128, 512])
            gvg = sb(p1, "gvg", [128, 512]); y2 = sb(p1, "y2", [128, 512])
            vgn = sb(p1, "vgn", [128, 512], BF16)
            bnst = sb(p1, "bnst", [128, 4, 6]); mv = sb(p1, "mv", [128, 4, 2])
            mixT = sb(p1, "mixT", [128, 8, 128], BF16)
            sgn = sb(p1, "sgn", [128, 4, 512], BF16)
            gnorm = sb(p1, "gnorm", [128, 512])
            bsgu = sb(p1, "bsgu", [128, 4])
            wsT = sb(p1, "wsT", [128, 4, 128], BF16)
            gsb = sb(p1, "gsb", [128, NH, 16]); top8 = sb(p1, "top8", [128, NH, 8])
            mb = sb(p1, "mb", [128, NH, 80], BF16)
            junk = sb(p1, "junk", [128, 512], BF16)

            win_v = win_d.rearrange("(kc p) n -> p kc n", p=128)
            for i in range(5):
                tr.dma("pool", win[:, :, i * 512:(i + 1) * 512], win_v[:, :, i * 512:(i + 1) * 512], w=["win"])
            for h in range(NH):
                tr.dma("pool", KT[64:80, h, :], oneh_d[:, :], w=[("KTo", h)])
            tr.dma("sp", gnorm[:], gnorm_d.partition_broadcast(128), w=["gnorm"])
            tr.dma("sp", bsgu[:], bsgu_d[:, :], w=["bsgu"])
            wsf = y2[:].rearrange("p (g t) -> p g t", g=4)
            tr.dma("sp", wsf, wsgu_d[:, :, :], w=["y2"])
            tr.op("dve", lambda e: e.tensor_tensor(out=wsT[:], in0=wsf,
                                                   in1=tri01[:].unsqueeze(1).to_broadcast([128, 4, 128]), op=ALU.mult),
                  r=["y2"], w=["wsT"])
            tr.op("pool", lambda e: e.memset(V[:, :, :, 64:65], 1.0), w=["Vones"])
            tr.op("pool", lambda e: e.memset(QA[64:80, :, :], 0.0), w=["QAm"])
            tr.op("pool", lambda e: e.memset(gsb[:], -1e30), w=["gsb"])
            tr.op("pool", lambda e: e.memset(mb[:], 0.0), w=["mb"])
            tr.op("pool", lambda e: e.memset(kmT[:], 0.0), w=["kmT"])

            def rstd_ops(src_key, src_ap, dst_ap, dst_key, n):
                tr.op("pool", lambda e: e.tensor_scalar(out=dst_ap, in0=src_ap, scalar1=1.0 / n, scalar2=EPS,
                                                        op0=ALU.mult, op1=ALU.add), r=[src_key], w=[dst_key])
                tr.op("pool", lambda e: e.tensor_tensor(out=dst_ap, in0=dst_ap, in1=mhalf[:, 0:1], op=ALU.pow),
                      r=[dst_key], w=[dst_key])

            MBb = PS[6][:].bitcast(BF16)
            MF = PS[7]
            sctr = [0]
            gctr = [0]

            def gbank():
                i = gctr[0] % 2
                gctr[0] += 1
                return PS[i], ("ps", i)

            def prologue_a(G, tt):
                tok = slice(G * 512 + tt * 128, G * 512 + (tt + 1) * 128)
                xi = 0
                xt = xin[xi]
                tr.dma("sp", xt[:], x_d[tok, :], w=[("xin", xi)])
                tr.op("act", lambda e, xt=xt: e.activation(out=xn[:], in_=xt[:], func=AF.Square,
                                                           accum_out=st[:, 0:1]),
                      r=[("xin", xi)], w=["xn", "st0"])
                rstd_ops("st0", st[:, 0:1], st[:, 1:2], "st1", D)
                tr.op("dve", lambda e, xt=xt: e.tensor_scalar(out=xn[:], in0=xt[:], scalar1=st[:, 1:2], scalar2=None,
                                                              op0=ALU.mult), r=[("xin", xi), "st1"], w=["xn"])
                for c in range(8):
                    tr.op("pe", lambda e, c=c: e.transpose(out=MBb[:, c * 128:(c + 1) * 128],
                                                           in_=xn[:, c * 128:(c + 1) * 128], identity=ident_b[:]),
                          r=["xn"], w=["MB"])
                for c in range(8):
                    tr.op("dve", lambda e, c=c, tt=tt: e.tensor_scalar(
                        out=hT[:, c, tt * 128:(tt + 1) * 128], in0=MBb[:, c * 128:(c + 1) * 128],
                        scalar1=A_m[:, c:c + 1], scalar2=B_m[:, c:c + 1], op0=ALU.mult, op1=ALU.add),
                        r=["MB"], w=[("hT", tt)])

            hTk = [("hT", t) for t in range(4)]

            def inproj_qk(G, oc):
                bank, bk = gbank()
                for kc in range(8):
                    tr.op("pe", lambda e, oc=oc, kc=kc, bank=bank: e.matmul(
                        bank[:, :], lhsT=win[:, kc, oc * 128:(oc + 1) * 128], rhs=hT[:, kc, :],
                        start=(kc == 0), stop=(kc == 7)), r=["win"] + hTk, w=[bk])
                for hh in range(2):
                    h = (oc % 4) * 2 + hh
                    rows = slice(hh * 64, (hh + 1) * 64)
                    if oc < 4:
                        tr.op("dve", lambda e, h=h, rows=rows, bank=bank: e.tensor_copy(
                            out=QA[0:64, h, :], in_=bank[rows, :]), r=[bk], w=[("QA", h)])
                    else:
                        tr.op("dve", lambda e, h=h, rows=rows, bank=bank, G=G: e.tensor_copy(
                            out=KT[0:64, h, G * 512:(G + 1) * 512], in_=bank[rows, :]), r=[bk], w=[("KT", h, G)])
                if oc >= 4:
                    tr.op("dve", lambda e, bank=bank: e.tensor_reduce(
                        out=ksum[:], in_=bank[:, :].rearrange("p (a b) -> p a b", a=2), axis=AX.X, op=ALU.add),
                        r=[bk], w=["ksum"])
                    for hh in range(2):
                        h = (oc % 4) * 2 + hh
                        tr.op("dve", lambda e, h=h, hh=hh, G=G: e.tensor_copy(
                            out=kmT[0:64, h, 2 * G:2 * G + 2], in_=ksum[hh * 64:(hh + 1) * 64, :]),
                            r=["ksum"], w=[("kmT", h)])

            def inproj_v(G, tt):
                ts_ = slice(tt * 128, (tt + 1) * 128)
                gt = G * 4 + tt
                bank, bk = gbank()
                for kc in range(8):
                    tr.op("pe", lambda e, kc=kc, bank=bank, ts_=ts_: e.matmul(
                        bank[:, :], lhsT=hT[:, kc, ts_], rhs=win[:, kc, 1024:1536],
                        start=(kc == 0), stop=(kc == 7)), r=["win", ("hT", tt)], w=[bk])
                tr.op("dve", lambda e, bank=bank, gt=gt: e.tensor_copy(
                    out=V[:, gt, :, 0:64], in_=bank[:, :].rearrange("p (h d) -> p h d", h=NH)),
                    r=[bk], w=[("V", gt)])

            def sgu(G, tt):
                ts_ = slice(tt * 128, (tt + 1) * 128)
                for which in range(2):
                    dst = uh if which == 0 else gvg
                    dk = "uh" if which == 0 else "gvg"
                    co = 1536 + which * 512
                    bank, bk = gbank()
                    for kc in range(8):
                        tr.op("pe", lambda e, kc=kc, bank=bank, ts_=ts_, co=co: e.matmul(
                            bank[:, :], lhsT=hT[:, kc, ts_], rhs=win[:, kc, co:co + 512],
                            start=(kc == 0), stop=(kc == 7)), r=["win", ("hT", tt)], w=[bk])
                    tr.op("act", lambda e, bank=bank, dst=dst: e.activation(out=dst[:], in_=bank[:, :], func=AF.Identity,
                                                                            scale=0.5), r=[bk], w=[dk])
                    tr.op("act", lambda e, bank=bank: e.activation(out=x2[:], in_=bank[:, :], func=AF.Square),
                          r=[bk], w=["x2"])
                    tr.op("pool", lambda e: e.tensor_scalar(out=x2[:], in0=x2[:], scalar1=0.044715, scalar2=1.0,
                                                            op0=ALU.mult, op1=ALU.add), r=["x2"], w=["x2"])
                    tr.op("pool", lambda e, dst=dst: e.tensor_tensor(out=x2[:], in0=x2[:], in1=dst[:], op=ALU.mult),
                          r=["x2", dk], w=["x2"])
                    tr.op("act", lambda e: e.activation(out=x2[:], in_=x2[:], func=AF.Tanh, scale=2.0 * GELU_C),
                          r=["x2"], w=["x2"])
                    tr.op("dve", lambda e, dst=dst: e.scalar_tensor_tensor(
                        out=dst[:], in0=x2[:], scalar=1.0, in1=dst[:], op0=ALU.add, op1=ALU.mult),
                        r=["x2", dk], w=[dk])
                for g in range(4):
                    tr.op("dve", lambda e, g=g: e.bn_stats(out=bnst[:, g, :], in_=gvg[:, g * 128:(g + 1) * 128]),
                          r=["gvg"], w=["bnst"])
                for g in range(4):
                    tr.op("dve", lambda e, g=g: e.bn_aggr(out=mv[:, g, :], in_=bnst[:, g, :]), r=["bnst"], w=["mv"])
                tr.op("pool", lambda e: e.tensor_scalar(out=st[:, 4:8], in0=mv[:, :, 1], scalar1=1.0, scalar2=EPS,
                                                        op0=ALU.mult, op1=ALU.add), r=["mv"], w=["st4"])
                tr.op("pool", lambda e: e.tensor_tensor(out=st[:, 4:8], in0=st[:, 4:8], in1=mhalf[:, 0:4], op=ALU.pow),
                      r=["st4"], w=["st4"])
                for g in range(4):
                    tr.op("dve", lambda e, g=g: e.tensor_scalar(
                        out=gvg[:, g * 128:(g + 1) * 128], in0=gvg[:, g * 128:(g + 1) * 128],
                        scalar1=mv[:, g, 0:1], scalar2=st[:, 4 + g:5 + g], op0=ALU.subtract, op1=ALU.mult),
                        r=["gvg", "mv", "st4"], w=["gvg"])
                tr.op("dve", lambda e: e.tensor_tensor(out=vgn[:], in0=gvg[:], in1=gnorm[:], op=ALU.mult),
                      r=["gvg", "gnorm"], w=["vgn"])
                bank, bk = gbank()
                for g in range(4):
                    tr.op("pe", lambda e, g=g, bank=bank: e.matmul(
                        bank[:, g * 128:(g + 1) * 128], lhsT=wsT[:, g, :], rhs=vgn[:, g * 128:(g + 1) * 128],
                        start=True, stop=True), r=["wsT", "vgn"], w=[bk])
                for g in range(4):
                    tr.op("dve", lambda e, g=g, bank=bank: e.scalar_tensor_tensor(
                        out=y2[:, g * 128:(g + 1) * 128], in0=bank[:, g * 128:(g + 1) * 128],
                        scalar=bsgu[:, g:g + 1], in1=uh[:, g * 128:(g + 1) * 128], op0=ALU.add, op1=ALU.mult),
                        r=[bk, "uh", "bsgu"], w=["y2"])
                tr.op("act", lambda e: e.activation(out=junk[:], in_=y2[:], func=AF.Square,
                                                    accum_out=st[:, 8:9]), r=["y2"], w=["junk", "st8"])
                rstd_ops("st8", st[:, 8:9], st[:, 9:10], "st9", 512)
                tr.op("dve", lambda e, tt=tt: e.tensor_scalar(out=sgn[:, tt, :], in0=y2[:], scalar1=st[:, 9:10],
                                                               scalar2=None, op0=ALU.mult),
                      r=["y2", "st9"], w=[("sgn", tt)])

            def gate(G):
                if G < 2:
                    return
                for tt in range(4):
                    j = 2 * G + tt // 2
                    ts_ = slice(tt * 128, (tt + 1) * 128)
                    for h in range(NH):
                        tr.op("pe", lambda e, h=h, ts_=ts_: e.matmul(
                            MF[:, 384 + h * 16:384 + (h + 1) * 16], lhsT=QA[0:64, h, ts_], rhs=kmT[:, h, :],
                            start=True, stop=True), r=[("QA", h), ("kmT", h)], w=["MFg"])
                    tr.op("dve", lambda e, j=j: e.tensor_copy(
                        out=gsb[:, :, 0:j], in_=MF[:, 384:512].rearrange("p (h n) -> p h n", h=NH)[:, :, 0:j]),
                        r=["MFg"], w=["gsb"])
                    for h in range(NH):
                        tr.op("dve", lambda e, h=h: e.max(out=top8[:, h, :], in_=gsb[:, h, :]), r=["gsb"], w=["top8"])
                    for h in range(NH):
                        tr.op("dve", lambda e, h=h, j=j: e.tensor_scalar(
                            out=mb[:, h, 64:64 + j], in0=gsb[:, h, 0:j], scalar1=top8[:, h, 2:3], scalar2=NEGM,
                            op0=ALU.is_lt, op1=ALU.mult), r=["gsb", "top8"], w=["mb"])
                    for h in range(NH):
                        tr.op("pe", lambda e, h=h: e.transpose(out=MBb[0:80, h * 128:(h + 1) * 128],
                                                               in_=mb[:, h, :], identity=ident_b[:]),
                              r=["mb"], w=["MB"])
                    tr.op("dve", lambda e, ts_=ts_: e.tensor_copy(
                        out=QA[64:80, :, ts_], in_=MBb[64:80, :].rearrange("p (h t) -> p h t", h=NH)),
                        r=["MB"], w=["QAm"])

            def attn_items(G):
                return [(G, h, kt) for h in range(NH) for kt in range(4 * G + 4)]

            def qk(G, h, kt):
                c0 = 0 if kt <= 4 * G else (kt - 4 * G) * 128
                si = sctr[0] % 2
                sctr[0] += 1
                Sb = PS[2 + si]
                sk = ("ps", 2 + si)
                tri = kt >= 4 * G
                tr.op("pe", lambda e, h=h, kt=kt, c0=c0, Sb=Sb, tri=tri: e.matmul(
                    Sb[:, c0:512], lhsT=KT[0:80, h, kt * 128:(kt + 1) * 128], rhs=QA[0:80, h, c0:512],
                    start=True, stop=(not tri)),
                    r=[("KT", h, kt // 4), ("KTo", h), ("QA", h), "QAm"], w=[sk])
                if tri:
                    ct0 = (kt - 4 * G) * 128
                    tr.op("pe", lambda e, Sb=Sb, ct0=ct0: e.matmul(
                        Sb[:, ct0:ct0 + 128], lhsT=ident_b[:], rhs=tribias[:], start=False, stop=True),
                        r=[], w=[sk])
                return (Sb, sk, c0)

            def exp_pv(G, h, kt, sinfo, idx):
                Sb, sk, c0 = sinfo
                nkt = 4 * G + 4
                pi = idx % 3
                Ob = PS[4 + h % 2]
                ok = ("ps", 4 + h % 2)
                tr.op("act", lambda e, Sb=Sb, c0=c0, pi=pi: e.activation(
                    out=Pb[pi][:, c0:512], in_=Sb[:, c0:512], func=AF.Exp, scale=0.125),
                    r=[sk], w=[("Pb", pi)])

                def pv():
                    tr.op("pe", lambda e, h=h, kt=kt, c0=c0, pi=pi, Ob=Ob, nkt=nkt: e.matmul(
                        Ob[0:65, c0:512], lhsT=V[:, kt, h, :], rhs=Pb[pi][:, c0:512],
                        start=(kt == 0), stop=(kt == nkt - 1)),
                        r=[("V", kt), "Vones", ("Pb", pi)], w=[ok])
                    if kt == nkt - 1:
                        head_finish(G, h, Ob, ok)
                return pv

            def head_finish(G, h, Ob, ok):
                tr.op("dve", lambda e, Ob=Ob: e.tensor_copy(out=OT[:], in_=Ob[0:65, :]), r=[ok], w=["OT"])
                for tt in range(4):
                    tr.op("pe", lambda e, tt=tt: e.transpose(out=MF[:, tt * 65:(tt + 1) * 65],
                                                             in_=OT[:, tt * 128:(tt + 1) * 128],
                                                             identity=ident_f[0:65, 0:65]),
                          r=["OT"], w=["MFo"])
                MFo = MF[:, 0:260].rearrange("p (t d) -> p t d", t=4)
                tr.op("dve", lambda e, MFo=MFo: e.reciprocal(out=rec[:], in_=MFo[:, :, 64]), r=["MFo"], w=["rec"])
                attn = attn2[G % 2]
                tr.op("dve", lambda e, MFo=MFo, h=h, attn=attn: e.tensor_tensor(
                    out=attn[:, :, h * 64:(h + 1) * 64], in0=MFo[:, :, 0:64],
                    in1=rec[:].unsqueeze(2).to_broadcast([128, 4, 64]), op=ALU.mult),
                    r=["MFo", "rec"], w=[("attn", G % 2)])

            def epilogue(G, tt):
                attn = attn2[G % 2]
                ak = ("attn", G % 2)
                tr.op("act", lambda e, tt=tt, attn=attn: e.activation(out=junk[:], in_=attn[:, tt, :], func=AF.Square,
                                                                      accum_out=st[:, 10:11]), r=[ak], w=["junk", "st10"])
                rstd_ops("st10", st[:, 10:11], st[:, 11:12], "st11", 512)
                tr.op("dve", lambda e, tt=tt, attn=attn: e.tensor_scalar(out=an[:, 0:512], in0=attn[:, tt, :],
                                                                         scalar1=st[:, 11:12], scalar2=None, op0=ALU.mult),
                      r=[ak, "st11"], w=["an"])
                tr.op("dve", lambda e, tt=tt: e.tensor_copy(out=an[:, 512:1024], in_=sgn[:, tt, :]),
                      r=[("sgn", tt)], w=["an"])
                for c in range(8):
                    tr.op("pe", lambda e, c=c: e.transpose(out=MBb[:, c * 128:(c + 1) * 128],
                                                           in_=an[:, c * 128:(c + 1) * 128], identity=ident_b[:]),
                          r=["an"], w=["MB"])
                tr.op("dve", lambda e, tt=tt: e.tensor_tensor(
                    out=mixT[:, :, :], in0=MBb[:, :].rearrange("p (c t) -> p c t", c=8),
                    in1=gmix[:].unsqueeze(2).to_broadcast([128, 8, 128]), op=ALU.mult),
                    r=["MB"], w=["mixT"])
                tr.dma("sp", mix_d[G * 4 + tt].rearrange("p (c t) -> p c t", c=8), mixT[:], r=["mixT"], w=["mixd"])

            def attention(G, side):
                items = attn_items(G)
                n_it = len(items)
                sin = {}
                for i in range(min(LOOK, n_it)):
                    sin[i] = qk(*items[i])
                sops = tr.capture(side)
                per = -(-len(sops) // max(1, n_it - 2))
                for i in range(n_it):
                    if LOOK == 0:
                        sin[i] = qk(*items[i])
                    pv = exp_pv(*items[i], sin.pop(i), i)
                    if LOOK > 0 and i + LOOK < n_it:
                        sin[i + LOOK] = qk(*items[i + LOOK])
                    pv()
                    tr.flush(sops, per)
                tr.flush(sops, len(sops))

            if INTERLEAVE:
                for tt in range(4):
                    prologue_a(0, tt)
                for oc in range(8):
                    inproj_qk(0, oc)
                for tt in range(4):
                    inproj_v(0, tt)
                gate(0)
                for G in range(8):
                    side = []
                    for t in range(4):
                        if G > 0:
                            side.append(lambda t=t, G=G: epilogue(G - 1, t))
                        side.append(lambda t=t, G=G: sgu(G, t))
                    if G + 1 < 8:
                        side += [(lambda t=t, G=G: prologue_a(G + 1, t)) for t in range(4)]
                    attention(G, side)
                    if G + 1 < 8:
                        for oc in range(8):
                            inproj_qk(G + 1, oc)
                        for tt in range(4):
                            inproj_v(G + 1, tt)
                        gate(G + 1)
                for tt in range(4):
                    epilogue(7, tt)
            else:
                for G in range(8):
                    for tt in range(4):
                        prologue_a(G, tt)
                    for oc in range(8):
                        inproj_qk(G, oc)
                    for tt in range(4):
                        inproj_v(G, tt)
                        sgu(G, tt)
                    gate(G)
                    attention(G, [])
                    for tt in range(4):
                        epilogue(G, tt)
            tr.op("sp", None, r=["mixd"])
            tr.emit(nc, p1, "p1")

        with ExitStack() as p2:
            tr = Tr()
            wout = sb(p2, "wout", [128, 8, D], BF16)
            wgu = sb(p2, "wgu", [128, 8, 2 * DFF], BF16)
            wdn = sb(p2, "wdn", [128, NFC, D], BF16)
            actT = sb(p2, "actT", [128, NFC, 256], BF16)
            hbs = [sb(p2, f"hb{i}", [128, 8, 256], BF16) for i in range(2)]
            xt2 = [sb(p2, f"xt2{i}", [128, D]) for i in range(4)]
            xn2 = sb(p2, "xn2", [128, D], BF16)
            junk2 = sb(p2, "junk2", [128, 512], BF16)
            ytmp = sb(p2, "ytmp", [128, 512])
            sg = sb(p2, "sg", [128, 256])
            st2 = sb(p2, "st2", [128, 16])
            GM = sb(p2, "GM", [128, D]); GF = sb(p2, "GF", [128, D])
            tr.dma("sp", GM[:], gpostm_d.partition_broadcast(128), w=["GM"])
            tr.dma("sp", GF[:], gpostf_d.partition_broadcast(128), w=["GF"])

            def sink2(j, mc, mk):
                G_ = GM if j < 6 else GF
                gk = "GM" if j < 6 else "GF"
                hs = slice((j % 2) * 512, (j % 2 + 1) * 512)
                tr.op("dve", lambda e, G_=G_, hs=hs, mc=mc: e.tensor_tensor(
                    out=G_[:, hs], in0=G_[:, hs], in1=mc, op=ALU.mult), r=list(mk) + [gk], w=[gk])
            wa_alias = actT[:, 0:16, :].rearrange("p (k a) t -> p k (a t)", a=2)
            ada_section(tr, p2, [4, 5, 10, 11], 1, sink2, "a2",
                        bufs=(wa_alias, [("actT", f) for f in range(16)], ytmp[:], ["ytmp"],
                              xn2[:].bitcast(F32), ["xn2"]))
            MBb = PS[6][:].bitcast(BF16)

            def rstd2(src_key, src_ap, dst_ap, dst_key, n):
                tr.op("pool", lambda e: e.tensor_scalar(out=dst_ap, in0=src_ap, scalar1=1.0 / n, scalar2=EPS,
                                                        op0=ALU.mult, op1=ALU.add), r=[src_key], w=[dst_key])
                tr.op("pool", lambda e: e.tensor_tensor(out=dst_ap, in0=dst_ap, in1=mhalf[:, 0:1], op=ALU.pow),
                      r=[dst_key], w=[dst_key])

            wout_v = wout_d.rearrange("(kc p) n -> p kc n", p=128)
            wgu_v = wgu_d.rearrange("(kc p) n -> p kc n", p=128)
            wdn_v = wdn_d.rearrange("(fc p) n -> p fc n", p=128)
            tr.dma("pool", wout[:], wout_v, w=["wout"])
            for fc in range(NFC):
                for pt in range(2):
                    co = pt * DFF + fc * 128
                    tr.dma("pool", wgu[:, :, co:co + 128], wgu_v[:, :, co:co + 128], w=[("wgu", fc)])
            for i in range(2):
                tr.dma("pool", wdn[:, i * 11:(i + 1) * 11, :], wdn_v[:, i * 11:(i + 1) * 11, :], w=["wdn"])
            bctr = [0]

            def pbank():
                i = bctr[0] % 4
                bctr[0] += 1
                return PS[i], ("ps", i)

            def prologue2(G, tt):
                hb = hbs[G % 2]
                hk = ("hb", G % 2, tt)
                ts_ = slice(tt * 128, (tt + 1) * 128)
                tok = slice(G * 256 + tt * 128, G * 256 + (tt + 1) * 128)
                xi = (G % 2) * 2 + tt
                xt = xt2[xi]
                xk = ("xt", xi)
                tr.dma("sp", hb[:, :, ts_], mix_d[G * 2 + tt].rearrange("p (c t) -> p c t", c=8), w=[hk])
                tr.dma("sp", xt[:], x_d[tok, :], w=[xk])
                bl = []
                for half in range(2):
                    hs = slice(half * 512, (half + 1) * 512)
                    bank, bk = PS[4 + half], ("ps", 4 + half)
                    bl.append((bank, bk))
                    for kc in range(8):
                        tr.op("pe", lambda e, kc=kc, bank=bank, ts_=ts_, hs=hs, hb=hb: e.matmul(
                            bank[:, :], lhsT=hb[:, kc, ts_], rhs=wout[:, kc, hs], start=(kc == 0), stop=(kc == 7)),
                            r=["wout", hk], w=[bk])
                    tr.op("act", lambda e, bank=bank, half=half: e.activation(
                        out=junk2[:], in_=bank[:, :], func=AF.Square, accum_out=st2[:, half:half + 1]),
                        r=[bk], w=["junk2", ("ssq", half)])
                tr.op("pool", lambda e: e.tensor_tensor(out=st2[:, 2:3], in0=st2[:, 0:1], in1=st2[:, 1:2], op=ALU.add),
                      r=[("ssq", 0), ("ssq", 1)], w=["s2"])
                rstd2("s2", st2[:, 2:3], st2[:, 3:4], "s3", D)
                for half, (bank, bk) in enumerate(bl):
                    hs = slice(half * 512, (half + 1) * 512)
                    tr.op("dve", lambda e, bank=bank, hs=hs: e.scalar_tensor_tensor(
                        out=ytmp[:], in0=bank[:, :], scalar=st2[:, 3:4], in1=GM[:, hs], op0=ALU.mult, op1=ALU.mult),
                        r=[bk, "s3", "GM"], w=["ytmp"])
                    tr.op("dve", lambda e, xt=xt, hs=hs: e.tensor_tensor(out=xt[:, hs], in0=xt[:, hs], in1=ytmp[:],
                                                                         op=ALU.add), r=["ytmp", xk], w=[xk])
                tr.op("act", lambda e, xt=xt: e.activation(out=xn2[:], in_=xt[:], func=AF.Square,
                                                           accum_out=st2[:, 4:5]), r=[xk], w=["xn2", "s4"])
                rstd2("s4", st2[:, 4:5], st2[:, 5:6], "s5", D)
                tr.op("dve", lambda e, xt=xt: e.tensor_scalar(out=xn2[:], in0=xt[:], scalar1=st2[:, 5:6], scalar2=None,
                                                              op0=ALU.mult), r=[xk, "s5"], w=["xn2"])
                for c in range(8):
                    tr.op("pe", lambda e, c=c: e.transpose(out=MBb[:, c * 128:(c + 1) * 128],
                                                           in_=xn2[:, c * 128:(c + 1) * 128], identity=ident_b[:]),
                          r=["xn2"], w=["MB"])
                for c in range(8):
                    tr.op("dve", lambda e, c=c, ts_=ts_, hb=hb: e.tensor_scalar(
                        out=hb[:, c, ts_], in0=MBb[:, c * 128:(c + 1) * 128],
                        scalar1=A_f[:, c:c + 1], scalar2=B_f[:, c:c + 1], op0=ALU.mult, op1=ALU.add),
                        r=["MB"], w=[hk])

            def gate_up(G, fc):
                hb = hbs[G % 2]
                gb, gk = pbank()
                ub, uk = pbank()
                for pt, bank, bk in ((0, gb, gk), (1, ub, uk)):
                    co = pt * DFF + fc * 128
                    for kc in range(8):
                        tr.op("pe", lambda e, kc=kc, bank=bank, co=co, hb=hb: e.matmul(
                            bank[:, 0:256], lhsT=wgu[:, kc, co:co + 128], rhs=hb[:, kc, :],
                            start=(kc == 0), stop=(kc == 7)),
                            r=[("wgu", fc), ("hb", G % 2, 0), ("hb", G % 2, 1)], w=[bk])
                tr.op("act", lambda e, gb=gb: e.activation(out=sg[:], in_=gb[:, 0:256], func=AF.Silu),
                      r=[gk], w=["sg"])
                tr.op("dve", lambda e, ub=ub, fc=fc: e.tensor_tensor(out=actT[:, fc, :], in0=sg[:], in1=ub[:, 0:256],
                                                                     op=ALU.mult), r=["sg", uk], w=[("actT", fc)])

            def down(G, tt):
                ts_ = slice(tt * 128, (tt + 1) * 128)
                tok = slice(G * 256 + tt * 128, G * 256 + (tt + 1) * 128)
                xi = (G % 2) * 2 + tt
                xt = xt2[xi]
                xk = ("xt", xi)
                banks = []
                for half in range(2):
                    hs = slice(half * 512, (half + 1) * 512)
                    bank, bk = pbank()
                    banks.append((bank, bk))
                    for fc in range(NFC):
                        tr.op("pe", lambda e, fc=fc, bank=bank, ts_=ts_, hs=hs: e.matmul(
                            bank[:, :], lhsT=actT[:, fc, ts_], rhs=wdn[:, fc, hs],
                            start=(fc == 0), stop=(fc == NFC - 1)),
                            r=["wdn", ("actT", fc)], w=[bk])
                    tr.op("act", lambda e, bank=bank, half=half: e.activation(
                        out=junk2[:], in_=bank[:, :], func=AF.Square, accum_out=st2[:, 8 + half:9 + half]),
                        r=[bk], w=["junk2", ("ssq2", half)])
                tr.op("pool", lambda e: e.tensor_tensor(out=st2[:, 10:11], in0=st2[:, 8:9], in1=st2[:, 9:10], op=ALU.add),
                      r=[("ssq2", 0), ("ssq2", 1)], w=["s10"])
                rstd2("s10", st2[:, 10:11], st2[:, 11:12], "s11", D)
                for half, (bank, bk) in enumerate(banks):
                    hs = slice(half * 512, (half + 1) * 512)
                    tr.op("dve", lambda e, bank=bank, hs=hs: e.scalar_tensor_tensor(
                        out=ytmp[:], in0=bank[:, :], scalar=st2[:, 11:12], in1=GF[:, hs], op0=ALU.mult, op1=ALU.mult),
                        r=[bk, "s11", "GF"], w=["ytmp"])
                    tr.op("dve", lambda e, xt=xt, hs=hs: e.tensor_tensor(out=xt[:, hs], in0=xt[:, hs], in1=ytmp[:],
                                                                         op=ALU.add), r=["ytmp", xk], w=[xk])
                tr.dma("sp", out_d[tok, :], xt[:], r=[xk], w=["outd"])

            for tt in range(2):
                prologue2(0, tt)
            for G in range(16):
                sops = []
                if G + 1 < 16:
                    sops = tr.capture([lambda G=G: prologue2(G + 1, 0), lambda G=G: prologue2(G + 1, 1)])
                per = -(-len(sops) // (NFC - 2))
                for fc in range(NFC):
                    gate_up(G, fc)
                    tr.flush(sops, per)
                tr.flush(sops, len(sops))
                for tt in range(2):
                    down(G, tt)
            tr.op("sp", None, r=["outd"])
            tr.emit(nc, p2, "p2")
    return nc


def _consts():
    ident = np.eye(128, dtype=np.float32)
    p = np.arange(128)
    tri01 = (p[:, None] <= p[None, :]).astype(np.float32)
    onehot = (np.arange(S)[None, :] // 256 == np.arange(16)[:, None]).astype(np.float32)
    return ident, tri01, onehot


def kernel(x, c, w_ada, b_ada, g_pre_mix, g_post_mix, w_in, g_sgu_norm, w_sgu, b_sgu,
           g_attn_out, g_sgu_out, w_out, g_pre_ffn, g_post_ffn, w_gate_up, w_down):
    f = lambda a: np.ascontiguousarray(np.asarray(a, dtype=np.float32))
    x = f(x); c = f(c)
    ident, tri01, onehot = _consts()
    pp = lambda v: f(np.asarray(v).reshape(-1, 128).T)
    shared = {
        "w_ada": f(w_ada[0]), "b_ada": f(b_ada[0]).reshape(1, -1),
        "gpm_t": pp(g_pre_mix[0]), "gpf_t": pp(g_pre_ffn[0]),
        "gmix_t": f(np.concatenate([pp(g_attn_out[0]), pp(g_sgu_out[0])], axis=1)),
        "g_post_mix": f(g_post_mix[0]).reshape(1, -1), "g_post_ffn": f(g_post_ffn[0]).reshape(1, -1),
        "g_sgu_norm": f(g_sgu_norm[0]).reshape(1, -1),
        "bsgu_t": f(np.asarray(b_sgu[0]).T),
        "wsgu_t": f(np.transpose(np.asarray(w_sgu[0]), (2, 0, 1))),
        "w_in": f(w_in[0]), "w_out": f(w_out[0]), "w_gate_up": f(w_gate_up[0]), "w_down": f(w_down[0]),
        "ident": ident, "tri01": tri01, "onehot": onehot,
    }
    in_maps = []
    for b in range(8):
        m = dict(shared)
        m["x"] = x[b]
        m["ct"] = pp(c[b])
        in_maps.append(m)
    nc = build_program()
    res = run_bass_kernel_spmd(nc, in_maps, core_ids=list(range(8)))
    return np.stack([np.asarray(r["out"], dtype=np.float32) for r in res.results], axis=0)
```
